# Optimizing a Trainium2 kernel written in Bass

```python
import math
import jax, jax.numpy as jnp
from jax import lax
import numpy as np

D_MODEL = 4096
BATCH = 4
SEQ = 2048
DEPTH = 1

A_HEADS = 16
A_HEAD_DIM = 128
IDX_HEADS = 16
IDX_DIM = 64
IDX_TOPK_MAX = 256
A_Q_BLOCK = 32
REL_BUCKETS = 32
REL_MAX_DIST = 128
B_HEADS = 16
B_HEAD_DIM = 128
CONV_WIDTH = 4
DELTA_CHUNK = 64
PEER_HEADS = 8
PEER_QUERY_DIM = 256
PEER_N_KEYS = 128
PEER_TOPK = 16
PEER_N_EXPERTS = PEER_N_KEYS * PEER_N_KEYS
PEER_TOKEN_BLOCK = 32

RMS_EPS = 1e-6
A_WIDTH = A_HEADS * A_HEAD_DIM
B_WIDTH = B_HEADS * B_HEAD_DIM
SPLIT_SIZES = (A_WIDTH, A_WIDTH, A_WIDTH,
               IDX_HEADS * IDX_DIM, IDX_DIM, IDX_HEADS,
               3 * B_WIDTH, B_HEADS, B_HEADS, B_WIDTH,
               D_MODEL, D_MODEL)

kernel_name = 'hybrid_dsa_gdn_peer_block'


def rms_norm(x, g):
    xf = x.astype(jnp.float32)
    y = xf * lax.rsqrt(jnp.mean(xf * xf, axis=-1, keepdims=True) + RMS_EPS)
    return (y * g.astype(jnp.float32)).astype(x.dtype)


def l2_norm(x):
    xf = x.astype(jnp.float32)
    return xf * lax.rsqrt(jnp.sum(xf * xf, axis=-1, keepdims=True) + RMS_EPS)


def split_columns(y):
    parts = []
    start = 0
    for size in SPLIT_SIZES:
        parts.append(y[..., start:start + size])
        start += size
    return parts


def t5_bucket(dist):
    n = jnp.maximum(dist, 0)
    max_exact = REL_BUCKETS // 2
    nf = jnp.maximum(n, 1).astype(jnp.float32)
    large = max_exact + (jnp.log(nf / max_exact) / math.log(REL_MAX_DIST / max_exact)
                         * (REL_BUCKETS - max_exact)).astype(jnp.int32)
    large = jnp.minimum(large, REL_BUCKETS - 1)
    return jnp.where(n < max_exact, n, large)


def causal_depthwise_conv(x, w):
    c = x.shape[-1]
    return lax.conv_general_dilated(
        x, w[:, None, :].astype(x.dtype), window_strides=(1,),
        padding=[(CONV_WIDTH - 1, 0)], dimension_numbers=('NWC', 'WIO', 'NWC'),
        feature_group_count=c)


def dsa_attention(q, k, v, qi, ki, wi, rel_bias):
    bsz, s = q.shape[:2]
    topk = min(IDX_TOPK_MAX, s // 4)
    nblk = s // A_Q_BLOCK
    pos = jnp.arange(s, dtype=jnp.int32)
    idx_scale = (IDX_DIM ** -0.5) * (IDX_HEADS ** -0.5)
    attn_scale = A_HEAD_DIM ** -0.5

    def to_blocks(t):
        return jnp.moveaxis(t.reshape((bsz, nblk, A_Q_BLOCK) + t.shape[2:]), 1, 0)

    def block(args):
        qb, qib, wib, tb = args
        s_idx = jnp.einsum('bqhd,bsd->bqhs', qib, ki).astype(jnp.float32)
        score = jnp.einsum('bqh,bqhs->bqs', wib.astype(jnp.float32), jax.nn.relu(s_idx)) * idx_scale
        causal = pos[None, None, :] <= tb[None, :, None]
        score = jnp.where(causal, score, -jnp.inf)
        _, sel = lax.top_k(score, topk)
        valid = sel <= tb[None, :, None]
        kg = jax.vmap(lambda kk, ii: kk[ii])(k, sel)
        vg = jax.vmap(lambda vv, ii: vv[ii])(v, sel)
        logits = jnp.einsum('bqhd,bqkhd->bqhk', qb, kg).astype(jnp.float32) * attn_scale
        bias = rel_bias[t5_bucket(tb[None, :, None] - sel)]
        logits = logits + jnp.moveaxis(bias.astype(jnp.float32), -1, -2)
        logits = jnp.where(valid[:, :, None, :], logits, -jnp.inf)
        p = jax.nn.softmax(logits, axis=-1).astype(v.dtype)
        return jnp.einsum('bqhk,bqkhd->bqhd', p, vg)

    out = lax.map(block, (to_blocks(q), to_blocks(qi), to_blocks(wi), pos.reshape(nblk, A_Q_BLOCK)))
    return jnp.moveaxis(out, 0, 1).reshape(bsz, s, A_WIDTH)


def gated_delta_rule(q, k, v, g, beta):
    bsz, s, h, dk = q.shape
    dv = v.shape[-1]
    c = DELTA_CHUNK
    nc = s // c

    def chunks(t):
        t = jnp.moveaxis(t.astype(jnp.float32), 2, 1)
        return t.reshape((bsz, h, nc, c) + t.shape[3:])

    q, k, v, g, beta = [chunks(t) for t in (q, k, v, g, beta)]
    q = q * (dk ** -0.5)
    g = jnp.cumsum(g, axis=-1)
    k_beta = k * beta[..., None]
    v_beta = v * beta[..., None]
    lower = jnp.tril(jnp.ones((c, c), dtype=bool))
    strict = jnp.tril(jnp.ones((c, c), dtype=bool), -1)
    diff = g[..., :, None] - g[..., None, :]
    decay = jnp.where(lower, jnp.exp(jnp.where(lower, diff, 0.0)), 0.0)
    eye = jnp.eye(c, dtype=jnp.float32)
    kk = jnp.einsum('bhncd,bhnsd->bhncs', k_beta, k) * decay
    a_mat = eye + jnp.where(strict, kk, 0.0)
    t_mat = lax.linalg.triangular_solve(a_mat, jnp.broadcast_to(eye, a_mat.shape),
                                        left_side=True, lower=True, unit_diagonal=True)
    u = t_mat @ v_beta
    w = t_mat @ (k_beta * jnp.exp(g)[..., None])
    qk = jnp.einsum('bhncd,bhnsd->bhncs', q, k) * decay
    g_last = g[..., -1:]
    k_dec = k * jnp.exp(g_last - g)[..., None]
    q_dec = q * jnp.exp(g)[..., None]

    def step(state, xs):
        u_i, w_i, qk_i, qd_i, kd_i, gl_i = xs
        v_new = u_i - w_i @ state
        o = qd_i @ state + qk_i @ v_new
        state = state * jnp.exp(gl_i)[..., None] + jnp.swapaxes(kd_i, -1, -2) @ v_new
        return state, o

    xs = [jnp.moveaxis(t, 2, 0) for t in (u, w, qk, q_dec, k_dec, g_last)]
    state0 = jnp.zeros((bsz, h, dk, dv), jnp.float32)
    _, o = lax.scan(step, state0, tuple(xs))
    o = jnp.moveaxis(o, 0, 2).reshape(bsz, h, s, dv)
    return jnp.moveaxis(o, 1, 2)


def peer_ffn(xn, wq, k1, k2, u_tab, v_tab):
    bsz, s, d = xn.shape
    xt = xn.reshape(-1, d)
    t = xt.shape[0]
    half = PEER_QUERY_DIM // 2
    q = (xt @ wq).reshape(t, PEER_HEADS, PEER_QUERY_DIM)
    s1 = jnp.einsum('thd,kd->thk', q[..., :half], k1).astype(jnp.float32)
    s2 = jnp.einsum('thd,kd->thk', q[..., half:], k2).astype(jnp.float32)
    v1, i1 = lax.top_k(s1, PEER_TOPK)
    v2, i2 = lax.top_k(s2, PEER_TOPK)
    cand = (v1[..., :, None] + v2[..., None, :]).reshape(t, PEER_HEADS, PEER_TOPK * PEER_TOPK)
    cand_idx = (i1[..., :, None] * PEER_N_KEYS + i2[..., None, :]).reshape(t, PEER_HEADS, PEER_TOPK * PEER_TOPK)
    sc, ci = lax.top_k(cand, PEER_TOPK)
    experts = jnp.take_along_axis(cand_idx, ci, axis=-1)
    gates = jax.nn.softmax(sc, axis=-1).astype(xn.dtype)
    nblk = t // PEER_TOKEN_BLOCK

    def block(args):
        xb, eb, gb = args
        ug = u_tab[eb]
        act = jax.nn.gelu(jnp.einsum('td,thkd->thk', xb, ug), approximate=False)
        vg = v_tab[eb]
        return jnp.einsum('thk,thkd->td', gb * act, vg)

    out = lax.map(block, (xt.reshape(nblk, PEER_TOKEN_BLOCK, d),
                          experts.reshape(nblk, PEER_TOKEN_BLOCK, PEER_HEADS, PEER_TOPK),
                          gates.reshape(nblk, PEER_TOKEN_BLOCK, PEER_HEADS, PEER_TOPK)))
    return out.reshape(bsz, s, d)


def hybrid_layer(x, norm1_g, w_in, conv_w, a_log, dt_bias, gdn_norm_g, q_norm_g, k_norm_g,
                 rel_bias, w_br_a, w_br_b, w_out, norm2_g, peer_wq, peer_k1, peer_k2, peer_u, peer_v):
    bsz, s, _ = x.shape
    xn = rms_norm(x, norm1_g)
    proj = xn @ w_in
    (aq, ak, av, iq, ik, iw, bqkv, ba, bb, bz, gate_a, gate_b) = split_columns(proj)

    aq = rms_norm(aq.reshape(bsz, s, A_HEADS, A_HEAD_DIM), q_norm_g)
    ak = rms_norm(ak.reshape(bsz, s, A_HEADS, A_HEAD_DIM), k_norm_g)
    av = av.reshape(bsz, s, A_HEADS, A_HEAD_DIM)
    iq = iq.reshape(bsz, s, IDX_HEADS, IDX_DIM)
    y_a = dsa_attention(aq, ak, av, iq, ik, iw, rel_bias)

    bqkv = jax.nn.silu(causal_depthwise_conv(bqkv, conv_w))
    bq, bk, bv = bqkv[..., :B_WIDTH], bqkv[..., B_WIDTH:2 * B_WIDTH], bqkv[..., 2 * B_WIDTH:]
    bq = l2_norm(bq.reshape(bsz, s, B_HEADS, B_HEAD_DIM))
    bk = l2_norm(bk.reshape(bsz, s, B_HEADS, B_HEAD_DIM))
    bv = bv.reshape(bsz, s, B_HEADS, B_HEAD_DIM)
    g = -jnp.exp(a_log.astype(jnp.float32)) * jax.nn.softplus(ba.astype(jnp.float32) + dt_bias.astype(jnp.float32))
    beta = jax.nn.sigmoid(bb.astype(jnp.float32))
    o = gated_delta_rule(bq, bk, bv, g, beta)
    z = bz.reshape(bsz, s, B_HEADS, B_HEAD_DIM).astype(jnp.float32)
    y_b = (rms_norm(o, gdn_norm_g) * jax.nn.silu(z)).reshape(bsz, s, B_WIDTH).astype(x.dtype)

    merged = jax.nn.sigmoid(gate_a) * (y_a @ w_br_a) + jax.nn.sigmoid(gate_b) * (y_b @ w_br_b)
    x = x + merged @ w_out

    x = x + peer_ffn(rms_norm(x, norm2_g), peer_wq, peer_k1, peer_k2, peer_u, peer_v)
    return x


def setup_inputs(seed: int = 0) -> dict:
    key = jax.random.key(seed)
    ks = jax.random.split(key, 20)
    f32 = jnp.float32
    in_cols = sum(SPLIT_SIZES)

    def nrm(k, shape, scale):
        return jax.random.normal(k, shape, f32) * scale

    dt = jnp.exp(jax.random.uniform(ks[5], (DEPTH, B_HEADS), f32, math.log(1e-3), math.log(1e-1)))
    return {
        'x': nrm(ks[0], (BATCH, SEQ, D_MODEL), 1.0),
        'norm1_g': 1.0 + nrm(ks[1], (DEPTH, D_MODEL), 0.05),
        'w_in': nrm(ks[2], (DEPTH, D_MODEL, in_cols), D_MODEL ** -0.5),
        'conv_w': nrm(ks[3], (DEPTH, CONV_WIDTH, 3 * B_WIDTH), CONV_WIDTH ** -0.5),
        'a_log': jnp.log(jax.random.uniform(ks[4], (DEPTH, B_HEADS), f32, 1.0, 16.0)),
        'dt_bias': dt + jnp.log(-jnp.expm1(-dt)),
        'gdn_norm_g': 1.0 + nrm(ks[6], (DEPTH, B_HEAD_DIM), 0.05),
        'q_norm_g': 1.0 + nrm(ks[7], (DEPTH, A_HEAD_DIM), 0.05),
        'k_norm_g': 1.0 + nrm(ks[8], (DEPTH, A_HEAD_DIM), 0.05),
        'rel_bias': nrm(ks[9], (REL_BUCKETS, A_HEADS), 0.5),
        'w_br_a': nrm(ks[10], (DEPTH, A_WIDTH, D_MODEL), A_WIDTH ** -0.5),
        'w_br_b': nrm(ks[11], (DEPTH, B_WIDTH, D_MODEL), B_WIDTH ** -0.5),
        'w_out': nrm(ks[12], (DEPTH, D_MODEL, D_MODEL), D_MODEL ** -0.5),
        'norm2_g': 1.0 + nrm(ks[13], (DEPTH, D_MODEL), 0.05),
        'peer_wq': nrm(ks[14], (DEPTH, D_MODEL, PEER_HEADS * PEER_QUERY_DIM), D_MODEL ** -0.5),
        'peer_k1': nrm(ks[15], (DEPTH, PEER_N_KEYS, PEER_QUERY_DIM // 2), (PEER_QUERY_DIM // 2) ** -0.5),
        'peer_k2': nrm(ks[16], (DEPTH, PEER_N_KEYS, PEER_QUERY_DIM // 2), (PEER_QUERY_DIM // 2) ** -0.5),
        'peer_u': nrm(ks[17], (DEPTH, PEER_N_EXPERTS, D_MODEL), D_MODEL ** -0.5),
        'peer_v': nrm(ks[18], (DEPTH, PEER_N_EXPERTS, D_MODEL), PEER_HEADS ** -0.5),
    }


def reference(x, norm1_g, w_in, conv_w, a_log, dt_bias, gdn_norm_g, q_norm_g, k_norm_g, rel_bias,
              w_br_a, w_br_b, w_out, norm2_g, peer_wq, peer_k1, peer_k2, peer_u, peer_v):
    for l in range(DEPTH):
        x = hybrid_layer(x, norm1_g[l], w_in[l], conv_w[l], a_log[l], dt_bias[l], gdn_norm_g[l],
                         q_norm_g[l], k_norm_g[l], rel_bias, w_br_a[l], w_br_b[l], w_out[l],
                         norm2_g[l], peer_wq[l], peer_k1[l], peer_k2[l], peer_u[l], peer_v[l])
    return x
```

```python
import math
import numpy as np
import ml_dtypes
from contextlib import ExitStack
import concourse.bass as bass
import concourse.mybir as mybir
from concourse.bass_utils import run_bass_kernel_spmd

F32 = mybir.dt.float32
BF16 = mybir.dt.bfloat16
AF = mybir.ActivationFunctionType
ALU = mybir.AluOpType
AX = mybir.AxisListType

ENGS = ("tensor", "vector", "scalar", "gpsimd", "sync")

D = 4096
KC = 32
S = 2048
OWN = 1024
NH = 16
EPS = 1e-6
C_AQ, C_AK, C_AV, C_IQ, C_IK, C_IW = 0, 2048, 4096, 6144, 7168, 7232
C_BQ, C_BK, C_BV, C_BA, C_BB, C_BZ = 7248, 9296, 11344, 13392, 13408, 13424
C_GA, C_GB = 15472, 19568
NCOL = 23664


class Prog:
    def __init__(self, nc, es):
        self.nc = nc
        self.es = es
        self.ops = []
        self.cnt = {e: 0 for e in ENGS}
        self.esem = {e: es.enter_context(nc.semaphore("s_" + e)) for e in ENGS}
        self.semobj = {id(s): s for s in self.esem.values()}
        self.known = {e: {} for e in ENGS}
        self.lastw = {}
        self.reads = {}
        self.dsem = {}
        self.free_dsems = []

    def _deps(self, eng, reads, writes):
        deps = []
        for r in reads:
            t = self.lastw.get(r)
            if t is not None:
                deps.append(t)
        for w in writes:
            t = self.lastw.get(w)
            if t is not None:
                deps.append(t)
            deps.extend(self.reads.get(w, ()))
        best = {}
        for (sid, val) in deps:
            if val > best.get(sid, 0):
                best[sid] = val
        waits = []
        kn = self.known[eng]
        for sid, val in best.items():
            if kn.get(sid, 0) >= val:
                continue
            kn[sid] = val
            waits.append((sid, val))
        return waits

    def _commit(self, tok, reads, writes):
        for r in reads:
            self.reads.setdefault(r, []).append(tok)
        for w in writes:
            self.lastw[w] = tok
            self.reads[w] = []

    def op(self, eng, fn, reads=(), writes=(), nosame=None):
        banks = [k for k in reads if isinstance(k, tuple) and k and k[0] == "bank"]
        if banks:
            reads = [k for k in reads if k not in banks]
            writes = list(writes) + [k for k in banks if k not in writes]
        waits = self._deps(eng, reads, writes)
        if nosame is None:
            nosame = (eng == "tensor")
        if nosame:
            waits = [w for w in waits if w[0] != id(self.esem[eng])]
        self.cnt[eng] += 1
        sem = self.esem[eng]
        tok = (id(sem), self.cnt[eng])
        self.ops.append((eng, fn, waits, (sem, 1)))
        self._commit(tok, reads, writes)
        return tok

    def dma(self, eng, fn, reads=(), writes=(), semkey=None):
        assert len(writes) >= 1
        key = semkey if semkey is not None else writes[0]
        if key not in self.dsem:
            if self.free_dsems:
                ent = self.free_dsems.pop()
            else:
                s = self.es.enter_context(self.nc.semaphore("d%d" % len(self.semobj)))
                self.semobj[id(s)] = s
                ent = [s, 0]
            self.dsem[key] = ent
        ent = self.dsem[key]
        waits = self._deps(eng, reads, writes)
        if ent[1] > 0:
            kn = self.known[eng]
            if kn.get(id(ent[0]), 0) < ent[1] * 16:
                kn[id(ent[0])] = ent[1] * 16
                waits.append((id(ent[0]), ent[1] * 16))
        ent[1] += 1
        tok = (id(ent[0]), ent[1] * 16)
        self.ops.append((eng, fn, waits, (ent[0], 16)))
        self._commit(tok, reads, writes)
        return tok

    def barrier(self):
        toks = []
        for e in ENGS:
            if self.cnt[e] > 0:
                toks.append((id(self.esem[e]), self.cnt[e]))
        for key, ent in self.dsem.items():
            if ent[1] > 0:
                toks.append((id(ent[0]), ent[1] * 16))
        for e in ENGS:
            waits = []
            kn = self.known[e]
            for (sid, val) in toks:
                if kn.get(sid, 0) < val:
                    kn[sid] = val
                    waits.append((sid, val))
            if waits:
                self.ops.append((e, None, waits, None))
        self.lastw = {}
        self.reads = {}
        for key, ent in self.dsem.items():
            self.free_dsems.append(ent)
        self.dsem = {}

    def emit(self):
        nc = self.nc
        with nc.Block() as block:
            def mk(ename):
                def body(e):
                    for (eng, fn, waits, inc) in self.ops:
                        if eng != ename:
                            continue
                        for (sid, val) in waits:
                            e.wait_ge(self.semobj[sid], val)
                        if fn is not None:
                            ins = fn(e)
                            ins.then_inc(inc[0], inc[1])
                return body
            block.sync(mk("sync"))
            block.tensor(mk("tensor"))
            block.vector(mk("vector"))
            block.scalar(mk("scalar"))
            block.gpsimd(mk("gpsimd"))


def bc(ap, shape):
    return ap.to_broadcast(list(shape))


def build(stop_after=None, dbg=()):
    nc = bass.Bass("TRN2", target_bir_lowering=False)
    dbg = set(dbg)

    def din(name, shape, dt=F32):
        return nc.dram_tensor(name, list(shape), dt, kind="ExternalInput").ap()

    def dscr(name, shape, dt=F32):
        kind = "ExternalOutput" if name in dbg else "Internal"
        return nc.dram_tensor(name, list(shape), dt, kind=kind).ap()

    xs = din("xs", [S, D])
    w_in = din("w_in", [D, NCOL])
    g1T = din("g1T", [128, KC])
    convT = din("convT", [128, 48, 4])
    qg_col = din("qg_col", [128, 1])
    kg_col = din("kg_col", [128, 1])
    alog_b = din("alog_b", [128, NH])
    dtb_b = din("dtb_b", [128, NH])
    ident_f = din("ident_f", [128, 128])
    ident_b = din("ident_b", [128, 128], BF16)
    keybias = din("keybias", [128, S])
    rel_b = din("rel_b", [32, NH])
    oh_c = din("oh_c", [32, 1280])
    b31_b = din("b31_b", [128, NH])
    J_c = din("J_c", [128, 128])
    um_c = din("um_c", [128, 128])
    ls_c = din("ls_c", [128, 128])
    gg_col = din("gg_col", [128, 1])
    w_bra = din("w_bra", [2048, D])
    w_brb = din("w_brb", [2048, D])
    w_o = din("w_o", [D, D])
    g2T = din("g2T", [128, KC])
    p_wq = din("p_wq", [D, 2048])
    pk1 = din("pk1", [128, 128])
    pk2 = din("pk2", [128, 128])
    p_u = din("p_u", [16384, D])
    p_v = din("p_v", [16384, D])
    out = nc.dram_tensor("out", [OWN, D], F32, kind="ExternalOutput").ap()

    akT = dscr("akT", [NH, 128, S], BF16)
    aqT = dscr("aqT", [NH, 128, OWN], BF16)
    av = dscr("av", [NH, 128, 16, 128], BF16)
    iqT = dscr("iqT", [8, 128, OWN], BF16)
    ikT2 = dscr("ikT2", [128, S], BF16)
    iw = dscr("iw", [OWN, NH])
    bqT = dscr("bqT", [NH, 128, S])
    bkT = dscr("bkT", [NH, 128, S])
    bk = dscr("bk", [NH, 128, 16, 128])
    bv = dscr("bv", [NH, 128, 16, 128])
    glb = dscr("glb", [S, 2 * NH])
    zsT = dscr("zsT", [NH, 128, OWN])
    gsa = dscr("gsa", [KC, 128, OWN], BF16)
    gsb = dscr("gsb", [KC, 128, OWN], BF16)
    mT = dscr("mT", [8, 128, 16, 128], BF16)
    fv = dscr("fv", [NH, 1280])
    yaT = dscr("yaT", [NH, 128, OWN], BF16)
    ybT = dscr("ybT", [NH, 128, OWN], BF16)
    x1 = dscr("x1", [OWN, D])
    GT = dscr("GT", [128, 128, OWN], BF16)
    GaT = dscr("GaT", [128, 128, OWN], BF16)

    with ExitStack() as es:
        P = Prog(nc, es)

        def sb(st, name, shape, dt):
            return st.enter_context(nc.sbuf_tensor(name, list(shape), dt))

        def ps(st, name, shape, dt=F32):
            return st.enter_context(nc.psum_tensor(name, list(shape), dt))

        identF = sb(es, "identF", [128, 128], F32)
        identB = sb(es, "identB", [128, 128], BF16)
        onesF = sb(es, "onesF", [128, 128], F32)
        P.dma("sync", lambda e: e.dma_start(out=identF[:], in_=ident_f), writes=["identF"])
        P.dma("sync", lambda e: e.dma_start(out=identB[:], in_=ident_b), writes=["identB"])
        P.op("vector", lambda e: e.memset(onesF[:], 1.0), writes=["onesF"])

        with ExitStack() as st:
            xnT = sb(st, "xnT", [128, KC, S], BF16)
            g1s = sb(st, "g1s", [128, KC], F32)
            P.dma("sync", lambda e: e.dma_start(out=g1s[:], in_=g1T), writes=["g1s"])
            with ExitStack() as st1:
                xf = [sb(st1, "xf%d" % i, [128, D], F32) for i in range(2)]
                xb = [sb(st1, "xb%d" % i, [128, D], BF16) for i in range(2)]
                junk = sb(st1, "junk", [128, D], BF16)
                ss = sb(st1, "ss", [128, 16], F32)
                rstd = sb(st1, "rstd", [128, 16], F32)
                pt = [ps(st1, "pt%d" % i, [128, 8, 128], BF16) for i in range(2)]
                P.op("vector", lambda e: e.memset(ss[:], 0.0), writes=["ss"])
                for i in range(16):
                    b = i % 2
                    P.dma("sync", lambda e, i=i, b=b: e.dma_start(out=xf[b][:], in_=xs[i * 128:(i + 1) * 128, :]),
                          writes=[("xf", b)])
                    P.op("scalar", lambda e, i=i, b=b: e.activation(out=junk[:], in_=xf[b][:], func=AF.Square,
                                                                   accum_out=ss[:, i:i + 1]),
                         reads=[("xf", b), "ss"], writes=["junk", ("ss", i)])
                    P.op("vector", lambda e, i=i: e.tensor_scalar(out=rstd[:, i:i + 1], in0=ss[:, i:i + 1],
                                                                  scalar1=1.0 / D, scalar2=EPS, op0=ALU.mult, op1=ALU.add),
                         reads=[("ss", i)], writes=[("rstd", i)])
                    P.op("scalar", lambda e, i=i: e.activation(out=rstd[:, i:i + 1], in_=rstd[:, i:i + 1], func=AF.Sqrt),
                         reads=[("rstd", i)], writes=[("rstd", i)])
                    P.op("vector", lambda e, i=i: e.reciprocal(out=rstd[:, i:i + 1], in_=rstd[:, i:i + 1]),
                         reads=[("rstd", i)], writes=[("rstd", i)])
                    P.op("scalar", lambda e, i=i, b=b: e.activation(out=xb[b][:], in_=xf[b][:], func=AF.Copy,
                                                                   scale=rstd[:, i:i + 1]),
                         reads=[("xf", b), ("rstd", i)], writes=[("xb", b)])
                    for g in range(4):
                        pb = g % 2
                        for c in range(8):
                            P.op("tensor", lambda e, b=b, g=g, c=c, pb=pb: e.transpose(
                                out=pt[pb][:, c, :], in_=xb[b][:, (g * 8 + c) * 128:(g * 8 + c + 1) * 128], identity=identB[:]),
                                reads=[("xb", b), "identB"], writes=[("pt", pb)])
                        P.op("vector", lambda e, i=i, g=g, pb=pb: e.tensor_tensor(
                            out=xnT[:, g * 8:(g + 1) * 8, i * 128:(i + 1) * 128], in0=pt[pb][:],
                            in1=bc(g1s[:, g * 8:(g + 1) * 8].unsqueeze(2), [128, 8, 128]), op=ALU.mult),
                            reads=[("pt", pb), "g1s"], writes=[("xnT", i)])
            P.barrier()

            with ExitStack() as st2:
                NWB = 2
                wt = [sb(st2, "wt%d" % i, [128, KC, 256], BF16) for i in range(NWB)]
                raw = sb(st2, "raw", [128, 4 + S], F32)
                cv = [sb(st2, "cv%d" % i, [128, 512], F32) for i in range(2)]
                sl = [sb(st2, "sl%d" % i, [128, 512], F32) for i in range(2)]
                sq = [sb(st2, "sq%d" % i, [128, 512], F32) for i in range(2)]
                rn = [sb(st2, "rn%d" % i, [128, 512], F32) for i in range(2)]
                stf = [sb(st2, "stf%d" % i, [128, 512], F32) for i in range(2)]
                stb = [sb(st2, "stb%d" % i, [128, 512], BF16) for i in range(2)]
                ttm = [sb(st2, "ttm%d" % i, [128, 4, 128], F32) for i in range(2)]
                ttb = [sb(st2, "ttb%d" % i, [128, 4, 128], BF16) for i in range(2)]
                cw = sb(st2, "cw", [128, 48, 4], F32)
                qg = sb(st2, "qg", [128, 1], F32)
                kg = sb(st2, "kg", [128, 1], F32)
                alb = sb(st2, "alb", [128, NH], F32)
                dtb = sb(st2, "dtb", [128, NH], F32)
                sm = [sb(st2, "sm%d" % i, [128, 32], F32) for i in range(6)]
                pA = ps(st2, "pA", [128, 4, 512], F32)
                pB = [ps(st2, "pB%d" % i, [128, 512], F32) for i in range(2)]
                pTf = ps(st2, "pTf", [128, 4, 128], F32)
                pTb = ps(st2, "pTb", [128, 4, 128], BF16)
                P.dma("sync", lambda e: e.dma_start(out=cw[:], in_=convT), writes=["cw"])
                P.dma("sync", lambda e: e.dma_start(out=qg[:], in_=qg_col), writes=["qg"])
                P.dma("sync", lambda e: e.dma_start(out=kg[:], in_=kg_col), writes=["kg"])
                P.dma("sync", lambda e: e.dma_start(out=alb[:], in_=alog_b), writes=["alb"])
                P.dma("sync", lambda e: e.dma_start(out=dtb[:], in_=dtb_b), writes=["dtb"])
                P.op("vector", lambda e: e.memset(raw[:, 0:4], 0.0), writes=["rawpad"])
                P.op("scalar", lambda e: e.activation(out=alb[:], in_=alb[:], func=AF.Exp), reads=["alb"], writes=["alb"])
                P.op("vector", lambda e: e.tensor_scalar(out=alb[:], in0=alb[:], scalar1=-1.0, scalar2=None, op0=ALU.mult),
                     reads=["alb"], writes=["alb"])

                plan = []
                for h in range(NH):
                    plan.append(dict(kind="ak", segs=[(C_AK + h * 128, 128)], own=False, h=h))
                for h in range(NH):
                    plan.append(dict(kind="aq", segs=[(C_AQ + h * 128, 128)], own=True, h=h))
                for h in range(NH):
                    plan.append(dict(kind="av", segs=[(C_AV + h * 128, 128)], own=False, h=h))
                for j in range(8):
                    plan.append(dict(kind="iq", segs=[(C_IQ + j * 128, 128)], own=True, h=j))
                plan.append(dict(kind="ik", segs=[(C_IK, 64), (C_IK, 64)], own=False, h=0))
                plan.append(dict(kind="iw", segs=[(C_IW, 16)], own=True, h=0))
                for wh, nm, c0 in ((0, "bq", C_BQ), (1, "bk", C_BK), (2, "bv", C_BV)):
                    for h in range(NH):
                        plan.append(dict(kind=nm, segs=[(c0 + h * 128, 128)], own=False, h=h, cb=wh * 16 + h))
                plan.append(dict(kind="bab", segs=[(C_BA, 32)], own=False, h=0))
                for h in range(NH):
                    plan.append(dict(kind="bz", segs=[(C_BZ + h * 128, 128)], own=True, h=h))
                for j in range(KC):
                    plan.append(dict(kind="ga", segs=[(C_GA + j * 128, 128)], own=True, h=j))
                for j in range(KC):
                    plan.append(dict(kind="gb", segs=[(C_GB + j * 128, 128)], own=True, h=j))
                if stop_after == "p2small":
                    plan = [p for p in plan if p["h"] == 0 or p["kind"] in ("ik", "iw", "bab")]

                tiles = []
                i = 0
                while i < len(plan):
                    a = plan[i]
                    if (i + 1 < len(plan) and len(a["segs"]) == 1 and a["segs"][0][1] == 128
                            and len(plan[i + 1]["segs"]) == 1 and plan[i + 1]["segs"][0][1] == 128
                            and plan[i + 1]["segs"][0][0] == a["segs"][0][0] + 128):
                        tiles.append([a, plan[i + 1]])
                        i += 2
                    else:
                        tiles.append([a])
                        i += 1

                w_v = w_in.rearrange("(c p) n -> p c n", p=128)

                def load_tile(ti):
                    t = tiles[ti]
                    wb = ti % NWB
                    off = 0
                    parts = []
                    for blk in t:
                        for (c0, n) in blk["segs"]:
                            parts.append((off, c0, n))
                            off += n
                    merged = []
                    for (o, c0, n) in parts:
                        if merged and merged[-1][1] + merged[-1][2] == c0 and merged[-1][0] + merged[-1][2] == o:
                            merged[-1] = (merged[-1][0], merged[-1][1], merged[-1][2] + n)
                        else:
                            merged.append((o, c0, n))
                    for q in range(4):
                        for (o, c0, n) in merged:
                            P.dma("gpsimd", lambda e, wb=wb, q=q, o=o, c0=c0, n=n: e.dma_start(
                                out=wt[wb][:, q * 8:(q + 1) * 8, o:o + n], in_=w_v[:, q * 8:(q + 1) * 8, c0:c0 + n]),
                                writes=[("wt", wb, q, o)], semkey=("wt", wb, q, o))

                def wt_res(wb):
                    return [k for k in list(P.lastw.keys()) if isinstance(k, tuple) and k[0] == "wt" and k[1] == wb]

                cnt2 = [0]

                def nxt():
                    cnt2[0] += 1
                    return cnt2[0] % 2

                for ti in range(min(NWB - 1, len(tiles))):
                    load_tile(ti)
                for ti, t in enumerate(tiles):
                    if ti + NWB - 1 < len(tiles):
                        load_tile(ti + NWB - 1)
                    wb = ti % NWB
                    off = 0
                    for blk in t:
                        M = sum(n for (_, n) in blk["segs"])
                        own = blk["own"]
                        sbl = [2, 3] if own else [0, 1, 2, 3]
                        kind = blk["kind"]
                        h = blk["h"]
                        wres = wt_res(wb)
                        for c in range(KC):
                            for j in sbl:
                                P.op("tensor", lambda e, wb=wb, c=c, j=j, off=off, M=M: e.matmul(
                                    pA[0:M, j, :], lhsT=wt[wb][:, c, off:off + M], rhs=xnT[:, c, j * 512:(j + 1) * 512],
                                    start=(c == 0), stop=(c == KC - 1)),
                                    reads=wres if (c == 0 and j == sbl[0]) or (c == KC - 1 and j == sbl[-1]) else [], writes=[("pA", j)])
                        if kind in ("bq", "bk", "bv"):
                            for j in sbl:
                                P.op("scalar", lambda e, j=j: e.activation(out=raw[:, 4 + j * 512:4 + (j + 1) * 512], in_=pA[:, j, :],
                                                                          func=AF.Copy),
                                     reads=[("pA", j), "rawpad"], writes=[("raw", j)])
                            cb = blk["cb"]
                            for j in sbl:
                                k = nxt()
                                rr = [("raw", j)] + ([("raw", j - 1)] if j > 0 else [])
                                P.op("vector", lambda e, j=j, k=k, cb=cb: e.tensor_scalar(
                                    out=cv[k][:], in0=raw[:, 1 + j * 512:1 + (j + 1) * 512], scalar1=cw[:, cb, 0:1], scalar2=None,
                                    op0=ALU.mult), reads=rr + ["cw"], writes=[("cv", k)])
                                for jj in (1, 2, 3):
                                    P.op("vector", lambda e, j=j, k=k, cb=cb, jj=jj: e.scalar_tensor_tensor(
                                        out=cv[k][:], in0=raw[:, 1 + jj + j * 512:1 + jj + (j + 1) * 512], scalar=cw[:, cb, jj:jj + 1],
                                        in1=cv[k][:], op0=ALU.mult, op1=ALU.add), reads=rr + [("cv", k)], writes=[("cv", k)])
                                P.op("scalar", lambda e, k=k: e.activation(out=sl[k][:], in_=cv[k][:], func=AF.Silu),
                                     reads=[("cv", k)], writes=[("sl", k)])
                                src = sl[k]
                                srckey = ("sl", k)
                                if kind in ("bq", "bk"):
                                    P.op("scalar", lambda e, k=k: e.activation(out=sq[k][:], in_=sl[k][:], func=AF.Square),
                                         reads=[("sl", k)], writes=[("sq", k)])
                                    P.op("tensor", lambda e, k=k: e.matmul(pB[k][:], lhsT=onesF[:], rhs=sq[k][:], start=True, stop=True),
                                         reads=[("sq", k), "onesF"], writes=[("pB", k)])
                                    P.op("scalar", lambda e, k=k: e.activation(out=rn[k][:], in_=pB[k][:], func=AF.Sqrt, bias=EPS,
                                                                              scale=1.0),
                                         reads=[("pB", k)], writes=[("rn", k)])
                                    P.op("vector", lambda e, k=k: e.reciprocal(out=rn[k][:], in_=rn[k][:]),
                                         reads=[("rn", k)], writes=[("rn", k)])
                                    scl = (128.0 ** -0.5) if kind == "bq" else 1.0
                                    P.op("vector", lambda e, k=k, scl=scl: e.scalar_tensor_tensor(
                                        out=stf[k][:], in0=sl[k][:], scalar=scl, in1=rn[k][:], op0=ALU.mult, op1=ALU.mult),
                                        reads=[("sl", k), ("rn", k)], writes=[("stf", k)])
                                    dst = bqT if kind == "bq" else bkT
                                    P.dma("sync", lambda e, k=k, h=h, j=j, dst=dst: e.dma_start(
                                        out=dst[h, :, j * 512:(j + 1) * 512], in_=stf[k][:]),
                                        reads=[("stf", k)], writes=[(kind, h, j)], semkey=("stfo", k))
                                    src = stf[k]
                                    srckey = ("stf", k)
                                if kind in ("bk", "bv"):
                                    for q in range(4):
                                        P.op("tensor", lambda e, q=q, src=src: e.transpose(out=pTf[:, q, :], in_=src[:, q * 128:(q + 1) * 128],
                                                                                           identity=identF[:]),
                                             reads=[srckey, "identF"], writes=["pTf"])
                                    P.op("vector", lambda e, k=k: e.tensor_copy(out=ttm[k][:], in_=pTf[:]),
                                         reads=["pTf"], writes=[("ttm", k)])
                                    dst = bk if kind == "bk" else bv
                                    P.dma("sync", lambda e, k=k, h=h, j=j, dst=dst: e.dma_start(
                                        out=dst[h, :, j * 4:(j + 1) * 4, :], in_=ttm[k][:]),
                                        reads=[("ttm", k)], writes=[(kind + "t", h, j)], semkey=("ttmo", k))
                        elif kind in ("ak", "aq"):
                            gcol = kg if kind == "ak" else qg
                            for j in sbl:
                                k = nxt()
                                P.op("scalar", lambda e, j=j, k=k: e.activation(out=sl[k][:], in_=pA[:, j, :], func=AF.Copy),
                                     reads=[("pA", j)], writes=[("sl", k)])
                                P.op("scalar", lambda e, k=k: e.activation(out=sq[k][:], in_=sl[k][:], func=AF.Square),
                                     reads=[("sl", k)], writes=[("sq", k)])
                                P.op("tensor", lambda e, k=k: e.matmul(pB[k][:], lhsT=onesF[:], rhs=sq[k][:], start=True, stop=True),
                                     reads=[("sq", k), "onesF"], writes=[("pB", k)])
                                P.op("scalar", lambda e, k=k: e.activation(out=rn[k][:], in_=pB[k][:], func=AF.Sqrt, bias=EPS,
                                                                          scale=1.0 / 128.0),
                                     reads=[("pB", k)], writes=[("rn", k)])
                                P.op("vector", lambda e, k=k: e.reciprocal(out=rn[k][:], in_=rn[k][:]),
                                     reads=[("rn", k)], writes=[("rn", k)])
                                P.op("vector", lambda e, k=k, gcol=gcol: e.scalar_tensor_tensor(
                                    out=stb[k][:], in0=sl[k][:], scalar=gcol[:, 0:1], in1=rn[k][:], op0=ALU.mult, op1=ALU.mult),
                                    reads=[("sl", k), ("rn", k), "qg", "kg"], writes=[("stb", k)])
                                if kind == "ak":
                                    P.dma("sync", lambda e, k=k, h=h, j=j: e.dma_start(out=akT[h, :, j * 512:(j + 1) * 512], in_=stb[k][:]),
                                          reads=[("stb", k)], writes=[("akT", h, j)], semkey=("stbo", k))
                                else:
                                    P.dma("sync", lambda e, k=k, h=h, j=j: e.dma_start(out=aqT[h, :, (j - 2) * 512:(j - 1) * 512], in_=stb[k][:]),
                                          reads=[("stb", k)], writes=[("aqT", h, j)], semkey=("stbo", k))
                        elif kind == "av":
                            for j in sbl:
                                k = nxt()
                                P.op("scalar", lambda e, j=j, k=k: e.activation(out=stb[k][:], in_=pA[:, j, :], func=AF.Copy),
                                     reads=[("pA", j)], writes=[("stb", k)])
                                for q in range(4):
                                    P.op("tensor", lambda e, q=q, k=k: e.transpose(out=pTb[:, q, :], in_=stb[k][:, q * 128:(q + 1) * 128],
                                                                                   identity=identB[:]),
                                         reads=[("stb", k), "identB"], writes=["pTb"])
                                P.op("vector", lambda e, k=k: e.tensor_copy(out=ttb[k][:], in_=pTb[:]),
                                     reads=["pTb"], writes=[("ttb", k)])
                                P.dma("sync", lambda e, k=k, h=h, j=j: e.dma_start(
                                    out=av[h, :, j * 4:(j + 1) * 4, :], in_=ttb[k][:]),
                                    reads=[("ttb", k)], writes=[("av", h, j)], semkey=("ttbo", k))
                        elif kind in ("iq", "ik", "ga", "gb"):
                            func = AF.Sigmoid if kind in ("ga", "gb") else AF.Copy
                            for j in sbl:
                                k = nxt()
                                P.op("scalar", lambda e, j=j, k=k, func=func: e.activation(out=stb[k][:], in_=pA[:, j, :], func=func),
                                     reads=[("pA", j)], writes=[("stb", k)])
                                if kind == "ik":
                                    dsto = ikT2[:, j * 512:(j + 1) * 512]
                                elif kind == "iq":
                                    dsto = iqT[h, :, (j - 2) * 512:(j - 1) * 512]
                                elif kind == "ga":
                                    dsto = gsa[h, :, (j - 2) * 512:(j - 1) * 512]
                                else:
                                    dsto = gsb[h, :, (j - 2) * 512:(j - 1) * 512]
                                P.dma("sync", lambda e, k=k, dsto=dsto: e.dma_start(out=dsto, in_=stb[k][:]),
                                      reads=[("stb", k)], writes=[(kind, h, j)], semkey=("stbo", k))
                        elif kind == "bz":
                            for j in sbl:
                                k = nxt()
                                P.op("scalar", lambda e, j=j, k=k: e.activation(out=stf[k][:], in_=pA[:, j, :], func=AF.Silu),
                                     reads=[("pA", j)], writes=[("stf", k)])
                                P.dma("sync", lambda e, k=k, h=h, j=j: e.dma_start(out=zsT[h, :, (j - 2) * 512:(j - 1) * 512], in_=stf[k][:]),
                                      reads=[("stf", k)], writes=[("zsT", h, j)], semkey=("stfo", k))
                        elif kind in ("iw", "bab"):
                            for j in sbl:
                                k = nxt()
                                P.op("scalar", lambda e, j=j, k=k, M=M: e.activation(out=sl[k][0:M, :], in_=pA[0:M, j, :], func=AF.Copy),
                                     reads=[("pA", j)], writes=[("sl", k)])
                                for q in range(4):
                                    P.op("tensor", lambda e, q=q, k=k, M=M: e.transpose(out=pTf[:, q, 0:M], in_=sl[k][0:M, q * 128:(q + 1) * 128],
                                                                                        identity=identF[0:M, 0:M]),
                                         reads=[("sl", k), "identF"], writes=["pTf"])
                                P.op("vector", lambda e, k=k, M=M: e.tensor_copy(out=ttm[k][:, :, 0:M], in_=pTf[:, :, 0:M]),
                                     reads=["pTf"], writes=[("ttm", k)])
                                if kind == "iw":
                                    P.dma("sync", lambda e, k=k, j=j: e.dma_start(
                                        out=iw[(j - 2) * 512:(j - 1) * 512, :].rearrange("(q p) d -> p q d", p=128), in_=ttm[k][:, :, 0:16]),
                                        reads=[("ttm", k)], writes=[("iw", j)], semkey=("ttmo", k))
                                else:
                                    xa, ax_, ee = sm[0], sm[1], sm[2]
                                    for q in range(4):
                                        P.op("vector", lambda e, k=k, q=q: e.tensor_tensor(out=ttm[k][:, q, 0:16], in0=ttm[k][:, q, 0:16], in1=dtb[:],
                                                                                          op=ALU.add),
                                             reads=[("ttm", k), "dtb"], writes=[("ttm", k)])
                                    xv = ttm[k][:, :, 0:16]
                                    tmpa = ttm[k][:, :, 32:48]
                                    tmpb = ttm[k][:, :, 48:64]
                                    P.op("vector", lambda e, xv=xv, tmpa=tmpa: e.tensor_scalar(out=tmpa, in0=xv, scalar1=60.0, scalar2=None, op0=ALU.min),
                                         reads=[("ttm", k)], writes=[("ttm", k)])
                                    P.op("scalar", lambda e, tmpa=tmpa: e.activation(out=tmpa, in_=tmpa, func=AF.Exp),
                                         reads=[("ttm", k)], writes=[("ttm", k)])
                                    P.op("scalar", lambda e, tmpa=tmpa: e.activation(out=tmpa, in_=tmpa, func=AF.Ln, bias=1.0, scale=1.0),
                                         reads=[("ttm", k)], writes=[("ttm", k)])
                                    for q in range(4):
                                        P.op("vector", lambda e, k=k, q=q: e.tensor_tensor(out=ttm[k][:, q, 0:16], in0=ttm[k][:, q, 32:48], in1=alb[:],
                                                                                          op=ALU.mult),
                                             reads=[("ttm", k), "alb"], writes=[("ttm", k)])
                                    P.op("scalar", lambda e, k=k: e.activation(out=ttm[k][:, :, 16:32], in_=ttm[k][:, :, 16:32], func=AF.Sigmoid),
                                         reads=[("ttm", k)], writes=[("ttm", k)])
                                    P.dma("sync", lambda e, k=k, j=j: e.dma_start(
                                        out=glb[j * 512:(j + 1) * 512, :].rearrange("(q p) d -> p q d", p=128), in_=ttm[k][:, :, 0:32]),
                                        reads=[("ttm", k)], writes=[("glb", j)], semkey=("ttmo", k))
                        off += M
            P.barrier()

        if stop_after in ("p2", "p2small"):
            P.emit()
            return nc

        import os as _os
        LIM = [int(_os.environ.get("OPLIM", "1000000000")), 0, False]

        def _lim():
            if LIM[2]:
                LIM[1] += 1
                return LIM[1] > LIM[0]
            return False

        REC = [None]

        def _op(eng, fn, r, w):
            if REC[0] is not None:
                REC[0].append(("op", eng, fn, list(r), list(w), None))
                return None
            return P.op(eng, fn, reads=r, writes=w)

        def V(fn, r=(), w=()):
            return _op("vector", fn, r, w)

        def A(fn, r=(), w=()):
            return _op("scalar", fn, r, w)

        def G(fn, r=(), w=()):
            return _op("gpsimd", fn, r, w)

        def T(fn, r=(), w=()):
            return _op("tensor", fn, r, w)

        def DMA(fn, r=(), w=(), key=None, eng="sync"):
            if REC[0] is not None:
                REC[0].append(("dma", eng, fn, list(r), list(w), key))
                return None
            return P.dma(eng, fn, reads=r, writes=w, semkey=key)

        def emit_rec(o):
            kind_, eng, fn, r, w, key = o
            if kind_ == "op":
                P.op(eng, fn, reads=r, writes=w)
            else:
                P.dma(eng, fn, reads=r, writes=w, semkey=key)

        with ExitStack() as st:
            ikS = sb(st, "ikS", [128, S], BF16)
            kbS = sb(st, "kbS", [128, S], F32)
            iqA = sb(st, "iqA", [128, 8, OWN], BF16)
            iwS = [sb(st, "iwS%d" % i, [128, 16], F32) for i in range(2)]
            rl = [sb(st, "rl%d" % i, [128, 2, 512], F32) for i in range(3)]
            acc = sb(st, "acc", [128, S], F32)
            sc = sb(st, "sc", [128, S], F32)
            wk = sb(st, "wk", [128, S], F32)
            mx = sb(st, "mx", [128, 8], F32)
            mk = sb(st, "mk", [128, S], BF16)
            mts = [sb(st, "mts%d" % i, [128, 16, 128], BF16) for i in range(2)]
            pS = [ps(st, "pS%d" % i, [128, 2, 512]) for i in range(3)]
            pT = [ps(st, "pT%d" % i, [128, 8, 128], BF16) for i in range(2)]
            DMA(lambda e: e.dma_start(out=ikS[:], in_=ikT2), w=["ikS"])
            DMA(lambda e: e.dma_start(out=kbS[:], in_=keybias), w=["kbS"])
            DMA(lambda e: e.dma_start(out=iqA[:], in_=iqT.rearrange("j p t -> p j t")), w=["iqA"])
            for qt in range(8):
                b = qt % 2
                DMA(lambda e, b=b, qt=qt: e.dma_start(out=iwS[b][:], in_=iw[qt * 128:(qt + 1) * 128, :]), w=[("iwS", b)])
                for hi in range(16):
                    blk, sub = hi // 2, hi % 2
                    for half in range(2):
                        r = (hi * 2 + half) % 3
                        for n in range(2):
                            c0 = half * 1024 + n * 512
                            T(lambda e, r=r, n=n, qt=qt, sub=sub, blk=blk, c0=c0: e.matmul(
                                pS[r][:, n, :], lhsT=iqA[sub * 64:(sub + 1) * 64, blk, qt * 128:(qt + 1) * 128], rhs=ikS[sub * 64:(sub + 1) * 64, c0:c0 + 512],
                                start=True, stop=True), r=["iqA", "ikS"], w=[("pS", r)])
                        A(lambda e, r=r: e.activation(out=rl[r][:], in_=pS[r][:], func=AF.Relu), r=[("pS", r)], w=[("rl", r)])
                        acch = acc[:, half * 1024:(half + 1) * 1024]
                        rlf = rl[r][:].rearrange("p a b -> p (a b)")
                        if hi == 0:
                            V(lambda e, b=b, acch=acch, rlf=rlf: e.tensor_scalar(out=acch, in0=rlf, scalar1=iwS[b][:, 0:1], scalar2=None, op0=ALU.mult),
                              r=[("rl", r), ("iwS", b)], w=[("acc", half)])
                        else:
                            V(lambda e, b=b, acch=acch, rlf=rlf, hi=hi: e.scalar_tensor_tensor(
                                out=acch, in0=rlf, scalar=iwS[b][:, hi:hi + 1], in1=acch, op0=ALU.mult, op1=ALU.add),
                              r=[("rl", r), ("iwS", b), ("acc", half)], w=[("acc", half)])
                V(lambda e: e.tensor_tensor(out=acc[:], in0=acc[:], in1=kbS[:], op=ALU.add), r=[("acc", 0), ("acc", 1), "kbS"], w=[("acc", 0), ("acc", 1)])
                G(lambda e, qt=qt: e.affine_select(out=sc[:], in_=acc[:], pattern=[[-1, S]], compare_op=ALU.is_ge, fill=-3.0e38,
                                                   base=OWN + qt * 128, channel_multiplier=1),
                  r=[("acc", 0), ("acc", 1)], w=["sc"])
                for rr in range(32):
                    srcb = sc if rr == 0 else wk
                    srck = "sc" if rr == 0 else "wk"
                    V(lambda e, srcb=srcb: e.max(out=mx[:], in_=srcb[:]), r=[srck], w=["mx"])
                    if rr < 31:
                        V(lambda e, srcb=srcb: e.match_replace(out=wk[:], in_to_replace=mx[:], in_values=srcb[:], imm_value=-1.0e30),
                          r=[srck, "mx"], w=["wk"])
                V(lambda e: e.tensor_scalar(out=mk[:], in0=sc[:], scalar1=mx[:, 7:8], scalar2=None, op0=ALU.is_ge), r=["sc", "mx"], w=["mk"])
                for g2 in range(2):
                    for c in range(8):
                        T(lambda e, g2=g2, c=c: e.transpose(out=pT[g2][:, c, :], in_=mk[:, (g2 * 8 + c) * 128:(g2 * 8 + c + 1) * 128], identity=identB[:]),
                          r=["mk", "identB"], w=[("pT", g2)])
                    A(lambda e, g2=g2, b=b: e.activation(out=mts[b][:, g2 * 8:(g2 + 1) * 8, :], in_=pT[g2][:], func=AF.Copy),
                      r=[("pT", g2)], w=[("mts", b)])
                DMA(lambda e, b=b, qt=qt: e.dma_start(out=mT[qt], in_=mts[b][:]),
                    r=[("mts", b)], w=[("mT", qt)], key=("mtso", b))
        P.barrier()
        if stop_after == "p3":
            P.emit()
            return nc

        with ExitStack() as st:
            mTS = sb(st, "mTS", [128, 8, 16, 128], BF16)
            rbS = sb(st, "rbS", [32, NH], F32)
            ohS = sb(st, "ohS", [32, 1280], F32)
            fvS = sb(st, "fvS", [NH, 1280], F32)
            b31S = sb(st, "b31S", [128, NH], F32)
            JS = sb(st, "JS", [128, 128], F32)
            onesB = sb(st, "onesB", [128, 128], BF16)
            hk = [sb(st, "hk%d" % i, [128, 512], F32) for i in range(2)]
            corr = [sb(st, "corr%d" % i, [128, 5, 512], BF16) for i in range(2)]
            kTS = [sb(st, "kTS%d" % i, [128, S], BF16) for i in range(2)]
            qTS = [sb(st, "qTS%d" % i, [128, OWN], BF16) for i in range(2)]
            vS = [sb(st, "vS%d" % i, [128, 16, 128], BF16) for i in range(2)]
            Eb = [sb(st, "Eb%d" % i, [128, 512], BF16) for i in range(3)]
            Pm = [sb(st, "Pm%d" % i, [128, 512], BF16) for i in range(3)]
            rinv = [sb(st, "rinv%d" % i, [128, 512], F32) for i in range(2)]
            yab = [sb(st, "yab%d" % i, [128, 512], BF16) for i in range(2)]
            pS4 = [ps(st, "pS4%d" % i, [128, 512]) for i in range(3)]
            pO = [ps(st, "pO%d" % i, [128, 512]) for i in range(2)]
            pR = [ps(st, "pR%d" % i, [128, 512]) for i in range(2)]
            pC = ps(st, "pC", [128, 512])
            DMA(lambda e: e.dma_start(out=mTS[:], in_=mT.rearrange("q p s t -> p q s t")), w=["mTS"])
            DMA(lambda e: e.dma_start(out=rbS[:], in_=rel_b), w=["rbS"])
            DMA(lambda e: e.dma_start(out=ohS[:], in_=oh_c), w=["ohS"])
            DMA(lambda e: e.dma_start(out=b31S[:], in_=b31_b), w=["b31S"])
            DMA(lambda e: e.dma_start(out=JS[:], in_=J_c), w=["JS"])
            V(lambda e: e.memset(onesB[:], 1.0), w=["onesB"])
            tg = [pO[0], pO[1], pR[0]]
            for n3, (c0, cn) in enumerate(((0, 512), (512, 512), (1024, 256))):
                T(lambda e, n3=n3, c0=c0, cn=cn: e.matmul(tg[n3][0:NH, 0:cn], lhsT=rbS[:], rhs=ohS[:, c0:c0 + cn], start=True, stop=True),
                  r=["rbS", "ohS"], w=[("tg", n3)])
                A(lambda e, n3=n3, c0=c0, cn=cn: e.activation(out=fvS[:, c0:c0 + cn], in_=tg[n3][0:NH, 0:cn], func=AF.Exp),
                  r=[("tg", n3)], w=["fvS"])
            DMA(lambda e: e.dma_start(out=fv, in_=fvS[:]), r=["fvS"], w=["fv"])
            P.barrier()
            OFFS = (128, 0, -128, -256, -384)
            import os
            P4H = int(os.environ.get("P4H", NH))
            LA = 2

            def p4_loads(h):
                b = h % 2
                DMA(lambda e, b=b, h=h: e.dma_start(out=kTS[b][:], in_=akT[h]), w=[("kTS", b)])
                DMA(lambda e, b=b, h=h: e.dma_start(out=qTS[b][:], in_=aqT[h]), w=[("qTS", b)])
                DMA(lambda e, b=b, h=h: e.dma_start(out=vS[b][:], in_=av[h]), w=[("vS", b)])

            def p4_corr(h, oi):
                b = h % 2
                o = OFFS[oi]
                hb = oi % 2
                src_ap = bass.AP(fv.tensor, h * 1280 + o + 513, [[1, 128], [1, 512]])
                DMA(lambda e, hb=hb, src_ap=src_ap: e.dma_start(out=hk[hb][:], in_=src_ap), w=[("hk", hb)])
                T(lambda e, hb=hb: e.matmul(pC[:], lhsT=JS[:], rhs=hk[hb][:], start=True, stop=True), r=[("hk", hb), "JS"], w=["pC"])
                A(lambda e, b=b, oi=oi: e.activation(out=corr[b][:, oi, :], in_=pC[:], func=AF.Copy), r=["pC"], w=[("corr", b)])

            if P4H > 0:
                p4_loads(0)
                for oi in range(5):
                    p4_corr(0, oi)
            for h in range(P4H):
                b = h % 2
                if h + 1 < P4H:
                    p4_loads(h + 1)
                pairs = [(tb, sti) for tb in range(2) for sti in range(12 if tb == 0 else 16)]
                npairs = len(pairs)

                def s_stage(p, h=h, b=b):
                    tb, sti = pairs[p]
                    k2 = p % 3
                    o = OWN + 512 * tb - 128 * sti
                    T(lambda e, k2=k2, b=b, sti=sti, tb=tb: e.matmul(pS4[k2][:], lhsT=kTS[b][:, sti * 128:(sti + 1) * 128],
                                                                  rhs=qTS[b][:, tb * 512:(tb + 1) * 512], start=True, stop=True),
                      r=[("kTS", b), ("qTS", b)], w=[("pS4", k2)])
                    A(lambda e, k2=k2, h=h: e.activation(out=Eb[k2][:], in_=pS4[k2][:], func=AF.Exp, bias=b31S[:, h:h + 1],
                                                        scale=128.0 ** -0.5), r=[("pS4", k2), "b31S"], w=[("Eb", k2)])
                    V(lambda e, k2=k2, sti=sti, tb=tb: e.tensor_tensor(out=Pm[k2][:].rearrange("p (a b) -> p a b", a=4),
                                                                      in0=Eb[k2][:].rearrange("p (a b) -> p a b", a=4),
                                                                      in1=mTS[:, tb * 4:(tb + 1) * 4, sti, :],
                                                                      op=ALU.mult), r=[("Eb", k2), "mTS"], w=[("Pm", k2)])
                    if o in OFFS:
                        oi = OFFS.index(o)
                        G(lambda e, k2=k2, b=b, oi=oi: e.tensor_tensor(out=Pm[k2][:], in0=Pm[k2][:], in1=corr[b][:, oi, :], op=ALU.mult),
                          r=[("Pm", k2), ("corr", b)], w=[("Pm", k2)])

                def pv_stage(p, h=h, b=b):
                    tb, sti = pairs[p]
                    k2 = p % 3
                    ob = tb
                    nst = 12 if tb == 0 else 16
                    T(lambda e, ob=ob, b=b, sti=sti, k2=k2, nst=nst: e.matmul(pO[ob][:], lhsT=vS[b][:, sti, :], rhs=Pm[k2][:],
                                                                          start=(sti == 0), stop=(sti == nst - 1)),
                      r=[("Pm", k2), ("vS", b)], w=[("pO", ob)])
                    T(lambda e, ob=ob, k2=k2, sti=sti, nst=nst: e.matmul(pR[ob][:], lhsT=onesB[:], rhs=Pm[k2][:],
                                                                     start=(sti == 0), stop=(sti == nst - 1)),
                      r=[("Pm", k2), "onesB"], w=[("pR", ob)])
                    if sti == nst - 1:
                        V(lambda e, ob=ob: e.reciprocal(out=rinv[ob][:], in_=pR[ob][:]), r=[("pR", ob)], w=[("rinv", ob)])
                        V(lambda e, ob=ob: e.tensor_tensor(out=yab[ob][:], in0=pO[ob][:], in1=rinv[ob][:], op=ALU.mult),
                          r=[("pO", ob), ("rinv", ob)], w=[("yab", ob)])
                        DMA(lambda e, ob=ob, h=h, tb=tb: e.dma_start(out=yaT[h, :, tb * 512:(tb + 1) * 512], in_=yab[ob][:]),
                            r=[("yab", ob)], w=[("yaT", h, tb)], key=("yabo", ob))

                for p in range(npairs + LA):
                    if p < npairs:
                        s_stage(p)
                    if p - LA >= 0:
                        pv_stage(p - LA)
                    if h + 1 < P4H and p in (4, 8, 12, 16, 20):
                        p4_corr(h + 1, (p - 4) // 4)
        P.barrier()
        if stop_after == "p4":
            P.emit()
            return nc

        with ExitStack() as st:
            UmS = sb(st, "UmS", [128, 128], F32)
            LsS = sb(st, "LsS", [128, 128], F32)
            ggS = sb(st, "ggS", [128, 1], F32)
            glS = sb(st, "glS", [128, 16, 32], F32)
            gcol = sb(st, "gcol", [128, 16, NH], F32)
            glast = sb(st, "glast", [128, 16, NH], F32)
            bgS = sb(st, "bgS", [128, 16, NH], F32)
            ekd = sb(st, "ekd", [128, 16, NH], F32)
            egl = sb(st, "egl", [128, 16, NH], F32)
            kTh = [sb(st, "kTh%d" % i, [128, S], F32) for i in range(2)]
            qTh = [sb(st, "qTh%d" % i, [128, OWN], F32) for i in range(2)]
            kth = [sb(st, "kth%d" % i, [128, 16, 128], F32) for i in range(2)]
            vth = [sb(st, "vth%d" % i, [128, 16, 128], F32) for i in range(2)]
            zsS = [sb(st, "zsS%d" % i, [128, OWN], F32) for i in range(2)]
            uS = [sb(st, "uS%d" % i, [128, 16, 128], F32) for i in range(2)]
            wTS = [sb(st, "wTS%d" % i, [128, 16, 128], F32) for i in range(2)]
            kdS = [sb(st, "kdS%d" % i, [128, 16, 128], F32) for i in range(2)]
            qdS = [sb(st, "qdS%d" % i, [128, 8, 128], F32) for i in range(2)]
            qkS = [sb(st, "qkS%d" % i, [128, 8, 128], F32) for i in range(2)]
            Sst = [sb(st, "Sst%d" % i, [128, 128], F32) for i in range(2)]
            ybst = [sb(st, "ybst%d" % i, [128, OWN], BF16) for i in range(2)]
            ssq = sb(st, "ssq5", [128, 8], F32)
            rs5 = sb(st, "rs5", [128, 8], F32)
            junk5 = sb(st, "junk5", [128, 128], F32)
            on5 = [sb(st, "on5%d" % i, [128, 128], F32) for i in range(2)]
            vn5 = [sb(st, "vn5%d" % i, [128, 128], F32) for i in range(2)]
            TN = ("Ug", "dA", "dec", "dB", "decT", "egb", "N", "M0", "M1", "N0", "N1", "P", "Q", "vb", "kbg")
            NCH = 6
            tmp = {nm: [sb(st, "t5%s%d" % (nm, i), [128, 128], F32) for i in range(NCH)] for nm in TN}
            pbk = [ps(st, "p5b%d" % i, [128, 4, 128]) for i in range(7)]
            def pslot(pb, k):
                return pbk[pb][:, k, :]
            PSN = {"Grow": 0, "KK": 1, "NT": 2, "M2": 0, "N2": 1, "pP": 2, "pQ": 3, "pu": 0, "pw": 1, "pqk": 2}

            DMA(lambda e: e.dma_start(out=UmS[:], in_=um_c), w=["UmS"])
            DMA(lambda e: e.dma_start(out=LsS[:], in_=ls_c), w=["LsS"])
            DMA(lambda e: e.dma_start(out=ggS[:], in_=gg_col), w=["ggS"])
            DMA(lambda e: e.dma_start(out=glS[:], in_=glb.rearrange("(i p) d -> p i d", p=128)), w=["glS"])
            for i in range(16):
                pg = pbk[i % 2][:, 0, 0:NH]
                pl = pbk[2 + i % 2][:, 0, 0:NH]
                T(lambda e, pg=pg, i=i: e.matmul(pg, lhsT=UmS[:], rhs=glS[:, i, 0:NH], start=True, stop=True), r=["UmS", "glS"], w=[("bank", i % 2)])
                T(lambda e, pl=pl, i=i: e.matmul(pl, lhsT=onesF[:], rhs=glS[:, i, 0:NH], start=True, stop=True), r=["onesF", "glS"], w=[("bank", 2 + i % 2)])
                A(lambda e, pg=pg, i=i: e.activation(out=gcol[:, i, :], in_=pg, func=AF.Copy), w=["gcol", ("bank", i % 2)])
                A(lambda e, pl=pl, i=i: e.activation(out=glast[:, i, :], in_=pl, func=AF.Copy), w=["glast", ("bank", 2 + i % 2)])
            A(lambda e: e.activation(out=bgS[:], in_=gcol[:], func=AF.Exp), r=["gcol"], w=["bgS"])
            V(lambda e: e.tensor_tensor(out=bgS[:], in0=bgS[:], in1=glS[:, :, NH:2 * NH], op=ALU.mult), r=["bgS", "glS"], w=["bgS"])
            V(lambda e: e.tensor_tensor(out=ekd[:], in0=glast[:], in1=gcol[:], op=ALU.subtract), r=["glast", "gcol"], w=["ekd"])
            A(lambda e: e.activation(out=ekd[:], in_=ekd[:], func=AF.Exp), r=["ekd"], w=["ekd"])
            A(lambda e: e.activation(out=egl[:], in_=glast[:], func=AF.Exp), r=["glast"], w=["egl"])

            def prep(h, i, pb):
                hb = h % 2
                own = i >= 8
                io = i - 8
                t = {nm: tmp[nm][pb] for nm in TN}
                K_ = lambda nm: ("t5", nm, pb)
                PK = lambda nm: ("bank", pb)
                psl = {nm: pslot(pb, k) for nm, k in PSN.items()}
                glc = glS[:, i, h:h + 1]
                btc = glS[:, i, NH + h:NH + h + 1]
                gcc = gcol[:, i, h:h + 1]
                kTc = kTh[hb][:, i * 128:(i + 1) * 128]
                V(lambda e: e.tensor_scalar(out=t["Ug"][:], in0=UmS[:], scalar1=glc, scalar2=None, op0=ALU.mult), r=["UmS", "glS"], w=[K_("Ug")])
                T(lambda e: e.matmul(psl["Grow"], lhsT=onesF[:], rhs=t["Ug"][:], start=True, stop=True), r=[K_("Ug"), "onesF"], w=[PK("Grow")])
                V(lambda e: e.tensor_scalar(out=t["dA"][:], in0=psl["Grow"], scalar1=gcc, scalar2=0.0, op0=ALU.subtract, op1=ALU.max),
                  r=[PK("Grow"), "gcol"], w=[K_("dA")])
                A(lambda e: e.activation(out=t["dec"][:], in_=t["dA"][:], func=AF.Exp, scale=-1.0), r=[K_("dA")], w=[K_("dec")])
                G(lambda e: e.tensor_tensor(out=t["dec"][:], in0=t["dec"][:], in1=LsS[:], op=ALU.mult), r=[K_("dec"), "LsS"], w=[K_("dec")])
                if own:
                    V(lambda e: e.tensor_scalar(out=t["dB"][:], in0=psl["Grow"], scalar1=gcc, scalar2=0.0, op0=ALU.subtract, op1=ALU.min),
                      r=[PK("Grow"), "gcol"], w=[K_("dB")])
                    A(lambda e: e.activation(out=t["decT"][:], in_=t["dB"][:], func=AF.Exp), r=[K_("dB")], w=[K_("decT")])
                    G(lambda e: e.tensor_tensor(out=t["decT"][:], in0=t["decT"][:], in1=UmS[:], op=ALU.mult), r=[K_("decT"), "UmS"], w=[K_("decT")])
                    A(lambda e: e.activation(out=t["egb"][:], in_=psl["Grow"], func=AF.Exp), r=[PK("Grow")], w=[K_("egb")])
                T(lambda e: e.matmul(psl["KK"], lhsT=kTc, rhs=kTc, start=True, stop=True), r=[("kTh", hb)], w=[PK("KK")])
                V(lambda e: e.scalar_tensor_tensor(out=t["N"][:], in0=psl["KK"], scalar=btc, in1=t["dec"][:], op0=ALU.mult, op1=ALU.mult),
                  r=[PK("KK"), K_("dec"), "glS"], w=[K_("N")])
                T(lambda e: e.transpose(out=psl["NT"], in_=t["N"][:], identity=identF[:]), r=[K_("N"), "identF"], w=[PK("NT")])
                A(lambda e: e.activation(out=t["M0"][:], in_=psl["NT"], func=AF.Copy), r=[PK("NT")], w=[K_("M0")])
                G(lambda e: e.tensor_copy(out=t["N0"][:], in_=t["N"][:]), r=[K_("N")], w=[K_("N0")])
                V(lambda e: e.tensor_tensor(out=t["P"][:], in0=identF[:], in1=t["M0"][:], op=ALU.subtract), r=["identF", K_("M0")], w=[K_("P")])
                G(lambda e: e.tensor_tensor(out=t["Q"][:], in0=identF[:], in1=t["N"][:], op=ALU.subtract), r=["identF", K_("N")], w=[K_("Q")])
                cur = 0
                for lv in range(1, 7):
                    last = lv == 6
                    Mc, Nc = "M%d" % cur, "N%d" % cur
                    Mn, Nn = "M%d" % (1 - cur), "N%d" % (1 - cur)
                    T(lambda e, Mc=Mc, Nc=Nc: e.matmul(psl["M2"], lhsT=t[Nc][:], rhs=t[Mc][:], start=True, stop=True),
                      r=[K_(Mc), K_(Nc)], w=[PK("M2")])
                    A(lambda e, Mn=Mn: e.activation(out=t[Mn][:], in_=psl["M2"], func=AF.Copy), r=[PK("M2")], w=[K_(Mn)])
                    if not last:
                        T(lambda e, Mc=Mc, Nc=Nc: e.matmul(psl["N2"], lhsT=t[Mc][:], rhs=t[Nc][:], start=True, stop=True),
                          r=[K_(Mc), K_(Nc)], w=[PK("N2")])
                        A(lambda e, Nn=Nn: e.activation(out=t[Nn][:], in_=psl["N2"], func=AF.Copy), r=[PK("N2")], w=[K_(Nn)])
                    T(lambda e, Mn=Mn: e.matmul(psl["pP"], lhsT=t["Q"][:], rhs=t[Mn][:], start=True, stop=True), r=[K_("Q"), K_(Mn)], w=[PK("pP")])
                    if not last:
                        T(lambda e, Nn=Nn: e.matmul(psl["pQ"], lhsT=t["P"][:], rhs=t[Nn][:], start=True, stop=True), r=[K_("P"), K_(Nn)], w=[PK("pQ")])
                    V(lambda e: e.tensor_tensor(out=t["P"][:], in0=t["P"][:], in1=psl["pP"], op=ALU.add), r=[K_("P"), PK("pP")], w=[K_("P")])
                    if not last:
                        V(lambda e: e.tensor_tensor(out=t["Q"][:], in0=t["Q"][:], in1=psl["pQ"], op=ALU.add), r=[K_("Q"), PK("pQ")], w=[K_("Q")])
                    cur = 1 - cur
                G(lambda e: e.tensor_scalar(out=t["vb"][:], in0=vth[hb][:, i, :], scalar1=btc, scalar2=None, op0=ALU.mult),
                  r=[("vth", hb), "glS"], w=[K_("vb")])
                T(lambda e: e.matmul(psl["pu"], lhsT=t["P"][:], rhs=t["vb"][:], start=True, stop=True), r=[K_("P"), K_("vb")], w=[PK("pu")])
                A(lambda e: e.activation(out=uS[hb][:, i, :], in_=psl["pu"], func=AF.Copy), r=[PK("pu")], w=[("uS", hb, i)])
                G(lambda e: e.tensor_scalar(out=t["kbg"][:], in0=kth[hb][:, i, :], scalar1=bgS[:, i, h:h + 1], scalar2=None, op0=ALU.mult),
                  r=[("kth", hb), "bgS"], w=[K_("kbg")])
                T(lambda e: e.matmul(psl["pw"], lhsT=t["kbg"][:], rhs=t["P"][:], start=True, stop=True), r=[K_("P"), K_("kbg")], w=[PK("pw")])
                A(lambda e: e.activation(out=wTS[hb][:, i, :], in_=psl["pw"], func=AF.Copy), r=[PK("pw")], w=[("wTS", hb, i)])
                G(lambda e: e.tensor_scalar(out=kdS[hb][:, i, :], in0=kth[hb][:, i, :], scalar1=ekd[:, i, h:h + 1], scalar2=None, op0=ALU.mult),
                  r=[("kth", hb), "ekd"], w=[("kdS", hb, i)])
                if own:
                    qTc = qTh[hb][:, io * 128:(io + 1) * 128]
                    T(lambda e: e.matmul(psl["pqk"], lhsT=kTc, rhs=qTc, start=True, stop=True), r=[("kTh", hb), ("qTh", hb)], w=[PK("pqk")])
                    V(lambda e: e.tensor_tensor(out=qkS[hb][:, io, :], in0=psl["pqk"], in1=t["decT"][:], op=ALU.mult),
                      r=[PK("pqk"), K_("decT")], w=[("qkS", hb, io)])
                    G(lambda e: e.tensor_tensor(out=qdS[hb][:, io, :], in0=qTc, in1=t["egb"][:], op=ALU.mult),
                      r=[("qTh", hb), K_("egb")], w=[("qdS", hb, io)])

            pW, pSs, pOo, pTr = (pbk[6][:, k, :] for k in range(4))

            def step(h, i):
                hb = h % 2
                own = i >= 8
                io = i - 8
                vb_ = i % 2
                T(lambda e: e.matmul(pW, lhsT=wTS[hb][:, i, :], rhs=Sst[hb][:], start=True, stop=True), r=[("wTS", hb, i), ("Sst", hb)], w=[("bank", 6)])
                V(lambda e: e.tensor_tensor(out=vn5[vb_][:], in0=uS[hb][:, i, :], in1=pW, op=ALU.subtract), r=[("uS", hb, i), ("bank", 6)], w=[("vn5", vb_)])
                if own:
                    T(lambda e: e.matmul(pOo, lhsT=qdS[hb][:, io, :], rhs=Sst[hb][:], start=True, stop=False), r=[("qdS", hb, io), ("Sst", hb)], w=[("bank", 6)])
                    T(lambda e: e.matmul(pOo, lhsT=qkS[hb][:, io, :], rhs=vn5[vb_][:], start=False, stop=True), r=[("qkS", hb, io), ("vn5", vb_)], w=[("bank", 6)])
                T(lambda e: e.matmul(pSs, lhsT=kdS[hb][:, i, :], rhs=vn5[vb_][:], start=True, stop=True), r=[("kdS", hb, i), ("vn5", vb_)], w=[("bank", 6)])
                V(lambda e: e.scalar_tensor_tensor(out=Sst[hb][:], in0=Sst[hb][:], scalar=egl[:, i, h:h + 1], in1=pSs, op0=ALU.mult, op1=ALU.add),
                  r=[("Sst", hb), "egl", ("bank", 6)], w=[("Sst", hb)])
                if own:
                    ob_ = io % 2
                    A(lambda e: e.activation(out=junk5[:], in_=pOo, func=AF.Square, accum_out=ssq[:, io:io + 1]), r=[("bank", 6), "ssq"], w=["junk5", ("ssq", io)])
                    V(lambda e: e.tensor_scalar(out=rs5[:, io:io + 1], in0=ssq[:, io:io + 1], scalar1=1.0 / 128.0, scalar2=EPS, op0=ALU.mult, op1=ALU.add),
                      r=[("ssq", io)], w=[("rs5", io)])
                    A(lambda e: e.activation(out=rs5[:, io:io + 1], in_=rs5[:, io:io + 1], func=AF.Sqrt), r=[("rs5", io)], w=[("rs5", io)])
                    V(lambda e: e.reciprocal(out=rs5[:, io:io + 1], in_=rs5[:, io:io + 1]), r=[("rs5", io)], w=[("rs5", io)])
                    A(lambda e: e.activation(out=on5[ob_][:], in_=pOo, func=AF.Copy, scale=rs5[:, io:io + 1]), r=[("bank", 6), ("rs5", io)], w=[("on5", ob_)])
                    T(lambda e: e.transpose(out=pTr, in_=on5[ob_][:], identity=identF[:]), r=[("on5", ob_), "identF"], w=[("bank", 6)])
                    V(lambda e: e.scalar_tensor_tensor(out=ybst[hb][:, io * 128:(io + 1) * 128], in0=pTr, scalar=ggS[:, 0:1],
                                                       in1=zsS[hb][:, io * 128:(io + 1) * 128], op0=ALU.mult, op1=ALU.mult),
                      r=[("bank", 6), "ggS", ("zsS", hb)], w=[("ybst", hb)])
                    if i == 15:
                        DMA(lambda e: e.dma_start(out=ybT[h], in_=ybst[hb][:]), r=[("ybst", hb)], w=[("ybT", h)], key=("ybo", hb))

            def loads(h):
                hb = h % 2
                DMA(lambda e: e.dma_start(out=kTh[hb][:], in_=bkT[h]), w=[("kTh", hb)])
                DMA(lambda e: e.dma_start(out=qTh[hb][:], in_=bqT[h][:, OWN:S]), w=[("qTh", hb)])
                DMA(lambda e: e.dma_start(out=kth[hb][:], in_=bk[h]), w=[("kth", hb)])
                DMA(lambda e: e.dma_start(out=vth[hb][:], in_=bv[h]), w=[("vth", hb)])
                DMA(lambda e: e.dma_start(out=zsS[hb][:], in_=zsT[h]), w=[("zsS", hb)])

            import os
            from collections import deque
            P5H = int(os.environ.get("P5H", NH))
            chunks = [(h, i) for h in range(P5H) for i in range(16)]
            steps = deque(chunks)
            active = {}
            prep_done = set()
            step_emitted = set()
            nxt_chunk = 0
            cur_step = None
            while nxt_chunk < len(chunks) or active or steps or cur_step:
                for slot in range(NCH):
                    if slot in active or nxt_chunk >= len(chunks):
                        continue
                    h, i = chunks[nxt_chunk]
                    if h >= 2 and (h - 2, i) not in step_emitted:
                        continue
                    if i == 0:
                        loads(h)
                    REC[0] = []
                    prep(h, i, slot)
                    active[slot] = (deque(REC[0]), (h, i))
                    REC[0] = None
                    nxt_chunk += 1
                for slot in sorted(active):
                    ops_, hi_ = active[slot]
                    emit_rec(ops_.popleft())
                    if not ops_:
                        prep_done.add(hi_)
                        del active[slot]
                if cur_step is None and steps and steps[0] in prep_done:
                    h, i = steps.popleft()
                    if i == 0:
                        hb1 = h % 2
                        V(lambda e, hb1=hb1: e.memset(Sst[hb1][:], 0.0), w=[("Sst", hb1)])
                        V(lambda e: e.memset(ssq[:], 0.0), w=["ssq"] + [("ssq", k) for k in range(8)])
                    REC[0] = []
                    step(h, i)
                    cur_step = (deque(REC[0]), (h, i))
                    REC[0] = None
                if cur_step is not None:
                    for _ in range(2):
                        if cur_step[0]:
                            emit_rec(cur_step[0].popleft())
                    if not cur_step[0]:
                        step_emitted.add(cur_step[1])
                        cur_step = None
        P.barrier()
        if stop_after == "p5":
            P.emit()
            return nc

        with ExitStack() as stX:
            ssq2 = sb(stX, "ssq2", [128, 8, 16], F32)
            with ExitStack() as st:
                mgT = sb(st, "mgT", [128, KC, OWN], BF16)
                with ExitStack() as st6a:
                    yaS = sb(st6a, "yaS", [128, NH, OWN], BF16)
                    ybS = sb(st6a, "ybS", [128, NH, OWN], BF16)
                    wa = [sb(st6a, "wa%d" % i, [128, NH, 256], BF16) for i in range(2)]
                    wb_ = [sb(st6a, "wb%d" % i, [128, NH, 256], BF16) for i in range(2)]
                    gaS = [sb(st6a, "gaS%d" % i, [128, OWN], BF16) for i in range(2)]
                    gbS = [sb(st6a, "gbS%d" % i, [128, OWN], BF16) for i in range(2)]
                    t1 = [sb(st6a, "t1%d" % i, [128, 512], F32) for i in range(2)]
                    t2 = [sb(st6a, "t2%d" % i, [128, 512], F32) for i in range(2)]
                    pMa = [ps(st6a, "pMa%d" % i, [128, 2, 512]) for i in range(2)]
                    pMb = [ps(st6a, "pMb%d" % i, [128, 2, 512]) for i in range(2)]
                    DMA(lambda e: e.dma_start(out=yaS[:], in_=yaT.rearrange("h p t -> p h t")), w=["yaS"])
                    DMA(lambda e: e.dma_start(out=ybS[:], in_=ybT.rearrange("h p t -> p h t")), w=["ybS"])
                    wa_v = w_bra.rearrange("(c p) n -> p c n", p=128)
                    wb_v = w_brb.rearrange("(c p) n -> p c n", p=128)
                    k6 = 0
                    for ti in range(16):
                        wbuf = ti % 2
                        for q in range(2):
                            DMA(lambda e, wbuf=wbuf, ti=ti, q=q: e.dma_start(out=wa[wbuf][:, q * 8:(q + 1) * 8, :],
                                                                            in_=wa_v[:, q * 8:(q + 1) * 8, ti * 256:(ti + 1) * 256]),
                                w=[("wa", wbuf, q)], eng="gpsimd")
                            DMA(lambda e, wbuf=wbuf, ti=ti, q=q: e.dma_start(out=wb_[wbuf][:, q * 8:(q + 1) * 8, :],
                                                                            in_=wb_v[:, q * 8:(q + 1) * 8, ti * 256:(ti + 1) * 256]),
                                w=[("wb", wbuf, q)], eng="gpsimd")
                        for sub in range(2):
                            cb = ti * 2 + sub
                            s6 = cb % 2
                            DMA(lambda e, s6=s6, cb=cb: e.dma_start(out=gaS[s6][:], in_=gsa[cb]), w=[("gaS", s6)])
                            DMA(lambda e, s6=s6, cb=cb: e.dma_start(out=gbS[s6][:], in_=gsb[cb]), w=[("gbS", s6)])
                            for (pM, wt_, yS, wk_, yk) in ((pMa, wa, yaS, "wa", "yaS"), (pMb, wb_, ybS, "wb", "ybS")):
                                for c in range(NH):
                                    for tb in range(2):
                                        T(lambda e, pM=pM, wt_=wt_, yS=yS, s6=s6, wbuf=wbuf, c=c, tb=tb, sub=sub: e.matmul(
                                            pM[s6][:, tb, :], lhsT=wt_[wbuf][:, c, sub * 128:(sub + 1) * 128], rhs=yS[:, c, tb * 512:(tb + 1) * 512],
                                            start=(c == 0), stop=(c == NH - 1)),
                                          r=[(wk_, wbuf, 0), (wk_, wbuf, 1), yk], w=[(wk_ + "p", s6, tb)])
                            for tb in range(2):
                                k6 = 1 - k6
                                V(lambda e, s6=s6, tb=tb, k6=k6: e.tensor_tensor(out=t1[k6][:], in0=pMa[s6][:, tb, :], in1=gaS[s6][:, tb * 512:(tb + 1) * 512],
                                                                              op=ALU.mult), r=[("wap", s6, tb), ("gaS", s6)], w=[("t1", k6)])
                                V(lambda e, s6=s6, tb=tb, k6=k6: e.tensor_tensor(out=t2[k6][:], in0=pMb[s6][:, tb, :], in1=gbS[s6][:, tb * 512:(tb + 1) * 512],
                                                                              op=ALU.mult), r=[("wbp", s6, tb), ("gbS", s6)], w=[("t2", k6)])
                                G(lambda e, cb=cb, tb=tb, k6=k6: e.tensor_tensor(out=mgT[:, cb, tb * 512:(tb + 1) * 512], in0=t1[k6][:], in1=t2[k6][:], op=ALU.add),
                                  r=[("t1", k6), ("t2", k6)], w=[("mgT", cb)])
                P.barrier()
                with ExitStack() as st6b:
                    wo = [sb(st6b, "wo%d" % i, [128, KC, 256], BF16) for i in range(2)]
                    xr = [sb(st6b, "xr%d" % i, [128, 256], F32) for i in range(4)]
                    x1t = [sb(st6b, "x1t%d" % i, [128, 256], F32) for i in range(4)]
                    junk6 = sb(st6b, "junk6", [128, 256], F32)
                    pX = [ps(st6b, "pX%d" % i, [128, 512]) for i in range(4)]
                    V(lambda e: e.memset(ssq2[:], 0.0), w=["ssq2"])
                    wo_v = w_o.rearrange("(c p) n -> p c n", p=128)
                    for ct in range(16):
                        wbuf = ct % 2
                        for q in range(4):
                            DMA(lambda e, wbuf=wbuf, ct=ct, q=q: e.dma_start(out=wo[wbuf][:, q * 8:(q + 1) * 8, :],
                                                                            in_=wo_v[:, q * 8:(q + 1) * 8, ct * 256:(ct + 1) * 256]),
                                w=[("wo", wbuf, q)], eng="gpsimd")
                        for tt in range(8):
                            k4 = (ct * 8 + tt) % 4
                            DMA(lambda e, k4=k4, tt=tt, ct=ct: e.dma_start(out=xr[k4][:], in_=xs[OWN + tt * 128:OWN + (tt + 1) * 128, ct * 256:(ct + 1) * 256]),
                                w=[("xr", k4)])
                            for c in range(KC):
                                T(lambda e, k4=k4, c=c, tt=tt, wbuf=wbuf: e.matmul(pX[k4][:, 0:256], lhsT=mgT[:, c, tt * 128:(tt + 1) * 128], rhs=wo[wbuf][:, c, :],
                                                                                start=(c == 0), stop=(c == KC - 1)),
                                  r=[("wo", wbuf, c // 8)], w=[("pX", k4)])
                            V(lambda e, k4=k4: e.tensor_tensor(out=x1t[k4][:], in0=pX[k4][:, 0:256], in1=xr[k4][:], op=ALU.add),
                              r=[("pX", k4), ("xr", k4)], w=[("x1t", k4)])
                            A(lambda e, k4=k4, tt=tt, ct=ct: e.activation(out=junk6[:], in_=x1t[k4][:], func=AF.Square, accum_out=ssq2[:, tt, ct:ct + 1]),
                              r=[("x1t", k4), "ssq2"], w=["junk6", ("ssq2", tt, ct)])
                            DMA(lambda e, k4=k4, tt=tt, ct=ct: e.dma_start(out=x1[tt * 128:(tt + 1) * 128, ct * 256:(ct + 1) * 256], in_=x1t[k4][:]),
                                r=[("x1t", k4)], w=[("x1", tt, ct)], key=("x1o", k4))
                P.barrier()
            xn2T = sb(stX, "xn2T", [128, KC, OWN], BF16)
            with ExitStack() as st1:
                xf6 = [sb(st1, "xf6%d" % i, [128, D], F32) for i in range(2)]
                xb6 = [sb(st1, "xb6%d" % i, [128, D], BF16) for i in range(2)]
                g2s = sb(st1, "g2s", [128, KC], F32)
                ss6v = sb(st1, "ss6", [128, 8], F32)
                pt6 = [ps(st1, "pt6%d" % i, [128, 8, 128], BF16) for i in range(2)]
                DMA(lambda e: e.dma_start(out=g2s[:], in_=g2T), w=["g2s"])
                V(lambda e: e.reduce_sum(out=ss6v[:], in_=ssq2[:], axis=AX.X), w=["ss6"])
                V(lambda e: e.tensor_scalar(out=ss6v[:], in0=ss6v[:], scalar1=1.0 / D, scalar2=EPS, op0=ALU.mult, op1=ALU.add), r=["ss6"], w=["ss6"])
                A(lambda e: e.activation(out=ss6v[:], in_=ss6v[:], func=AF.Sqrt), r=["ss6"], w=["ss6"])
                V(lambda e: e.reciprocal(out=ss6v[:], in_=ss6v[:]), r=["ss6"], w=["ss6"])
                for i in range(8):
                    b = i % 2
                    DMA(lambda e, i=i, b=b: e.dma_start(out=xf6[b][:], in_=x1[i * 128:(i + 1) * 128, :]), w=[("xf6", b)])
                    A(lambda e, i=i, b=b: e.activation(out=xb6[b][:], in_=xf6[b][:], func=AF.Copy, scale=ss6v[:, i:i + 1]),
                      r=[("xf6", b), "ss6"], w=[("xb6", b)])
                    for g4 in range(4):
                        pb = g4 % 2
                        for c in range(8):
                            T(lambda e, b=b, g4=g4, c=c, pb=pb: e.transpose(out=pt6[pb][:, c, :], in_=xb6[b][:, (g4 * 8 + c) * 128:(g4 * 8 + c + 1) * 128],
                                                                         identity=identB[:]), r=[("xb6", b), "identB"], w=[("pt6", pb)])
                        V(lambda e, i=i, g4=g4, pb=pb: e.tensor_tensor(out=xn2T[:, g4 * 8:(g4 + 1) * 8, i * 128:(i + 1) * 128], in0=pt6[pb][:],
                                                                     in1=bc(g2s[:, g4 * 8:(g4 + 1) * 8].unsqueeze(2), [128, 8, 128]), op=ALU.mult),
                          r=[("pt6", pb), "g2s"], w=[("xn2T", i)])
            P.barrier()
            if stop_after == "p6":
                P.emit()
                return nc

            with ExitStack() as st7:
                s12 = sb(st7, "s12", [128, 8, 16, 128], F32)
                with ExitStack() as st:
                    wq = [sb(st, "wq%d" % i, [128, KC, 256], BF16) for i in range(2)]
                    qpS = [sb(st, "qpS%d" % i, [128, OWN], F32) for i in range(2)]
                    kraw = sb(st, "kraw", [128, 2, 128], F32)
                    kT2 = sb(st, "kT2", [128, 2, 128], F32)
                    pQ = [ps(st, "pQ%d" % i, [128, 2, 512]) for i in range(2)]
                    pSc = [ps(st, "pSc%d" % i, [128, 512]) for i in range(2)]
                    pK = ps(st, "pK7", [128, 512])
                    DMA(lambda e: e.dma_start(out=kraw[:, 0, :], in_=pk1), w=[("kraw", 0)])
                    DMA(lambda e: e.dma_start(out=kraw[:, 1, :], in_=pk2), w=[("kraw", 1)])
                    for j in range(2):
                        T(lambda e, j=j: e.transpose(out=pK[:, j * 128:(j + 1) * 128], in_=kraw[:, j, :], identity=identF[:]),
                          r=[("kraw", j), "identF"], w=["pK"])
                    A(lambda e: e.activation(out=kT2[:].rearrange("p a b -> p (a b)"), in_=pK[:, 0:256], func=AF.Copy), r=["pK"], w=["kT2"])
                    wq_v = p_wq.rearrange("(c p) n -> p c n", p=128)
                    for ti in range(8):
                        wbuf = ti % 2
                        for q in range(4):
                            DMA(lambda e, wbuf=wbuf, ti=ti, q=q: e.dma_start(out=wq[wbuf][:, q * 8:(q + 1) * 8, :],
                                                                            in_=wq_v[:, q * 8:(q + 1) * 8, ti * 256:(ti + 1) * 256]),
                                w=[("wq", wbuf, q)], eng="gpsimd")
                        for sub in range(2):
                            blk = ti * 2 + sub
                            s7 = blk % 2
                            for c in range(KC):
                                for tb in range(2):
                                    T(lambda e, s7=s7, wbuf=wbuf, c=c, tb=tb, sub=sub: e.matmul(
                                        pQ[s7][:, tb, :], lhsT=wq[wbuf][:, c, sub * 128:(sub + 1) * 128], rhs=xn2T[:, c, tb * 512:(tb + 1) * 512],
                                        start=(c == 0), stop=(c == KC - 1)), r=[("wq", wbuf, c // 8)], w=[("pQ", s7)])
                            A(lambda e, s7=s7: e.activation(out=qpS[s7][:], in_=pQ[s7][:].rearrange("p a b -> p (a b)"), func=AF.Copy),
                              r=[("pQ", s7)], w=[("qpS", s7)])
                            for tt in range(8):
                                k2 = tt % 2
                                T(lambda e, k2=k2, s7=s7, tt=tt, blk=blk: e.matmul(pSc[k2][:, 0:128], lhsT=qpS[s7][:, tt * 128:(tt + 1) * 128],
                                                                                rhs=kT2[:, blk % 2, :], start=True, stop=True),
                                  r=[("qpS", s7), "kT2"], w=[("pSc", k2)])
                                V(lambda e, k2=k2, tt=tt, blk=blk: e.tensor_copy(out=s12[:, tt, blk, :], in_=pSc[k2][:, 0:128]),
                                  r=[("pSc", k2)], w=[("s12", tt, blk)])
                P.barrier()
                with ExitStack() as st:
                    thr = sb(st, "thr7", [128, 8, 8], F32)
                    nb7 = sb(st, "nb7", [128, 8, 8], F32)
                    v12 = sb(st, "v12", [128, 2, 16], F32)
                    wk7 = sb(st, "wk7", [128, 128], F32)
                    cand = sb(st, "cand", [128, 16, 16], F32)
                    cwk = sb(st, "cwk", [128, 256], F32)
                    c24 = sb(st, "c24", [128, 24], F32)
                    z7 = sb(st, "z7", [128, 8, 8], F32)
                    j16 = sb(st, "j16", [128, 16], F32)
                    sum7 = [sb(st, "sum7%d" % i, [128, 8, 128], F32) for i in range(2)]
                    E7 = [sb(st, "E7%d" % i, [128, 8, 128], BF16) for i in range(2)]
                    Gh = [[sb(st, "Gh%d_%d" % (i, h), [128, 8, 128], BF16) for h in range(8)] for i in range(2)]
                    GTs = sb(st, "GTs", [128, 8, OWN], BF16)
                    pGn = [ps(st, "pGn%d" % i, [128, 2, 512]) for i in range(2)]
                    pGt = [ps(st, "pGt%d" % i, [128, 8, 128], BF16) for i in range(2)]
                    Gn = [sb(st, "Gn%d" % i, [128, 8, 128], BF16) for i in range(2)]
                    V(lambda e: e.memset(z7[:], 0.0), w=["z7"])
                    for tt in range(8):
                        for h in range(8):
                            for half in range(2):
                                sv = s12[:, tt, 2 * h + half, :]
                                V(lambda e, sv=sv, half=half: e.max(out=v12[:, half, 0:8], in_=sv), w=["v12"])
                                V(lambda e, sv=sv, half=half: e.match_replace(out=wk7[:], in_to_replace=v12[:, half, 0:8], in_values=sv, imm_value=-1.0e30),
                                  r=["v12"], w=["wk7"])
                                V(lambda e, half=half: e.max(out=v12[:, half, 8:16], in_=wk7[:]), r=["wk7"], w=["v12"])
                            V(lambda e: e.tensor_tensor(out=cand[:], in0=bc(v12[:, 0, :].unsqueeze(2), [128, 16, 16]),
                                                        in1=bc(v12[:, 1, :].unsqueeze(1), [128, 16, 16]), op=ALU.add), r=["v12"], w=["cand"])
                            cf = cand[:].rearrange("p a b -> p (a b)")
                            V(lambda e, cf=cf: e.max(out=c24[:, 0:8], in_=cf), r=["cand"], w=["c24"])
                            V(lambda e, cf=cf: e.match_replace(out=cwk[:], in_to_replace=c24[:, 0:8], in_values=cf, imm_value=-1.0e30), r=["cand", "c24"], w=["cwk"])
                            V(lambda e: e.max(out=c24[:, 8:16], in_=cwk[:]), r=["cwk"], w=["c24"])
                            V(lambda e: e.match_replace(out=cwk[:], in_to_replace=c24[:, 8:16], in_values=cwk[:], imm_value=-1.0e30), r=["cwk", "c24"], w=["cwk"])
                            V(lambda e: e.max(out=c24[:, 16:24], in_=cwk[:]), r=["cwk"], w=["c24"])
                            tc_ = thr[:, tt, h:h + 1]
                            nc_ = nb7[:, tt, h:h + 1]
                            zc_ = z7[:, tt, h:h + 1]
                            V(lambda e, tc_=tc_: e.tensor_tensor(out=tc_, in0=c24[:, 15:16], in1=c24[:, 16:17], op=ALU.add), r=["c24"], w=["thr"])
                            V(lambda e, tc_=tc_: e.tensor_scalar(out=tc_, in0=tc_, scalar1=0.5, scalar2=None, op0=ALU.mult), r=["thr"], w=["thr"])
                            V(lambda e, tc_=tc_, nc_=nc_: e.tensor_scalar(out=nc_, in0=tc_, scalar1=-1.0, scalar2=None, op0=ALU.mult), r=["thr"], w=["nb7"])
                            A(lambda e, nc_=nc_, zc_=zc_: e.activation(out=j16[:], in_=c24[:, 0:16], func=AF.Exp, bias=nc_, accum_out=zc_),
                              r=["c24", "nb7", "z7"], w=["j16", "z7"])
                            A(lambda e, zc_=zc_: e.activation(out=zc_, in_=zc_, func=AF.Ln), r=["z7"], w=["z7"])
                            V(lambda e, nc_=nc_, zc_=zc_: e.tensor_tensor(out=nc_, in0=nc_, in1=zc_, op=ALU.subtract), r=["nb7", "z7"], w=["nb7"])
                    for ic in range(16):
                        for tt in range(8):
                            gs = tt % 2
                            for h in range(8):
                                k2 = h % 2
                                s1c = s12[:, tt, 2 * h, ic * 8:(ic + 1) * 8]
                                s2a = s12[:, tt, 2 * h + 1, :]
                                G(lambda e, k2=k2, s1c=s1c, s2a=s2a: e.tensor_tensor(out=sum7[k2][:], in0=bc(s1c.unsqueeze(2), [128, 8, 128]),
                                                                                     in1=bc(s2a.unsqueeze(1), [128, 8, 128]), op=ALU.add),
                                  w=[("sum7", k2)])
                                A(lambda e, k2=k2, tt=tt, h=h: e.activation(out=E7[k2][:], in_=sum7[k2][:], func=AF.Exp, bias=nb7[:, tt, h:h + 1]),
                                  r=[("sum7", k2), "nb7"], w=[("E7", k2)])
                                V(lambda e, k2=k2, gs=gs, h=h, tt=tt: e.scalar_tensor_tensor(out=Gh[gs][h][:], in0=sum7[k2][:], scalar=thr[:, tt, h:h + 1],
                                                                                             in1=E7[k2][:], op0=ALU.is_ge, op1=ALU.mult),
                                  r=[("sum7", k2), ("E7", k2), "thr"], w=[("Gh", gs, h)])
                            for half in range(2):
                                for h in range(8):
                                    T(lambda e, gs=gs, half=half, h=h: e.matmul(pGn[gs][:, half, :], lhsT=identB[:],
                                                                              rhs=Gh[gs][h][:, half * 4:(half + 1) * 4, :].rearrange("p a b -> p (a b)"),
                                                                              start=(h == 0), stop=(h == 7)),
                                      r=[("Gh", gs, h), "identB"], w=[("pGn", gs, half)])
                            A(lambda e, gs=gs: e.activation(out=Gn[gs][:].rearrange("p a b -> p (a b)"), in_=pGn[gs][:].rearrange("p a b -> p (a b)"), func=AF.Copy),
                              r=[("pGn", gs, 0), ("pGn", gs, 1)], w=[("Gn", gs)])
                            for i1l in range(8):
                                T(lambda e, gs=gs, i1l=i1l: e.transpose(out=pGt[gs][:, i1l, :], in_=Gn[gs][:, i1l, :], identity=identB[:]),
                                  r=[("Gn", gs), "identB"], w=[("pGt", gs)])
                            V(lambda e, gs=gs, tt=tt: e.tensor_copy(out=GTs[:, :, tt * 128:(tt + 1) * 128], in_=pGt[gs][:]),
                              r=[("pGt", gs)], w=["GTs"])
                        DMA(lambda e, ic=ic: e.dma_start(out=GT[ic * 8:(ic + 1) * 8].rearrange("a p t -> p a t"), in_=GTs[:]),
                            r=["GTs"], w=[("GT", ic)], key="gto")
                P.barrier()
            with ExitStack() as st:
                Ub = [sb(st, "Ub%d" % i, [128, D], BF16) for i in range(2)]
                UT = [sb(st, "UT%d" % i, [128, KC, 128], BF16) for i in range(2)]
                actT = [sb(st, "actT%d" % i, [128, OWN], BF16) for i in range(2)]
                GTt = [sb(st, "GTt%d" % i, [128, OWN], BF16) for i in range(2)]
                Gat = [sb(st, "Gat%d" % i, [128, OWN], BF16) for i in range(2)]
                pUT = [ps(st, "pUT%d" % i, [128, 8, 128], BF16) for i in range(2)]
                pA7 = [ps(st, "pA7%d" % i, [128, 2, 512]) for i in range(2)]
                import os
                NE = int(os.environ.get("P7E", 128))
                for i1 in range(NE):
                    b = i1 % 2
                    for q in range(2):
                        DMA(lambda e, b=b, i1=i1, q=q: e.dma_start(out=Ub[b][:, q * 2048:(q + 1) * 2048], in_=p_u[i1 * 128:(i1 + 1) * 128, q * 2048:(q + 1) * 2048]),
                            w=[("Ub", b, q)], eng="gpsimd")
                    DMA(lambda e, b=b, i1=i1: e.dma_start(out=GTt[b][:], in_=GT[i1]), w=[("GTt", b)])
                    for g4 in range(4):
                        pb = g4 % 2
                        for c in range(8):
                            T(lambda e, b=b, g4=g4, c=c, pb=pb: e.transpose(out=pUT[pb][:, c, :], in_=Ub[b][:, (g4 * 8 + c) * 128:(g4 * 8 + c + 1) * 128],
                                                                         identity=identB[:]), r=[("Ub", b, g4 // 2), "identB"], w=[("pUT", pb)])
                        V(lambda e, b=b, g4=g4, pb=pb: e.tensor_copy(out=UT[b][:, g4 * 8:(g4 + 1) * 8, :], in_=pUT[pb][:]), r=[("pUT", pb)], w=[("UT", b, g4)])
                    for c in range(KC):
                        for tb in range(2):
                            T(lambda e, b=b, c=c, tb=tb: e.matmul(pA7[b][:, tb, :], lhsT=UT[b][:, c, :], rhs=xn2T[:, c, tb * 512:(tb + 1) * 512],
                                                               start=(c == 0), stop=(c == KC - 1)), r=[("UT", b, c // 8)], w=[("pA7", b)])
                    A(lambda e, b=b: e.activation(out=actT[b][:], in_=pA7[b][:].rearrange("p a b -> p (a b)"), func=AF.Gelu), r=[("pA7", b)], w=[("actT", b)])
                    V(lambda e, b=b: e.tensor_tensor(out=Gat[b][:], in0=actT[b][:], in1=GTt[b][:], op=ALU.mult), r=[("actT", b), ("GTt", b)], w=[("Gat", b)])
                    DMA(lambda e, b=b, i1=i1: e.dma_start(out=GaT[i1], in_=Gat[b][:]), r=[("Gat", b)], w=[("GaT", i1)], key=("gato", b))
            P.barrier()
        with ExitStack() as st:
            Vb = [sb(st, "Vb%d" % i, [128, 4, 512], BF16) for i in range(3)]
            Gg = [sb(st, "Gg%d" % i, [128, 4, OWN], BF16) for i in range(3)]
            x1r = [sb(st, "x1r%d" % i, [128, 512], F32) for i in range(2)]
            ot = [sb(st, "ot%d" % i, [128, 512], F32) for i in range(2)]
            pO7 = [ps(st, "pO7%d" % i, [128, 512]) for i in range(8)]
            NG = NE // 4
            for db in range(8):
                for ig in range(NG):
                    b3 = (db * NG + ig) % 3
                    DMA(lambda e, b3=b3, ig=ig, db=db: e.dma_start(
                        out=Vb[b3][:], in_=p_v[ig * 512:(ig + 1) * 512, db * 512:(db + 1) * 512].rearrange("(q p) n -> p q n", p=128)),
                        w=[("Vb", b3)], eng="gpsimd")
                    DMA(lambda e, b3=b3, ig=ig: e.dma_start(out=Gg[b3][:], in_=GaT[ig * 4:(ig + 1) * 4].rearrange("q p t -> p q t")), w=[("Gg", b3)])
                    for q in range(4):
                        i1 = ig * 4 + q
                        for tt in range(8):
                            T(lambda e, b3=b3, q=q, tt=tt, i1=i1: e.matmul(pO7[tt][:], lhsT=Gg[b3][:, q, tt * 128:(tt + 1) * 128], rhs=Vb[b3][:, q, :],
                                                                        start=(i1 == 0), stop=(i1 == NE - 1)),
                              r=[("Vb", b3), ("Gg", b3)], w=[("pO7", tt)])
                for tt in range(8):
                    k2 = tt % 2
                    DMA(lambda e, k2=k2, tt=tt, db=db: e.dma_start(out=x1r[k2][:], in_=x1[tt * 128:(tt + 1) * 128, db * 512:(db + 1) * 512]), w=[("x1r", k2)])
                    V(lambda e, k2=k2, tt=tt: e.tensor_tensor(out=ot[k2][:], in0=pO7[tt][:], in1=x1r[k2][:], op=ALU.add),
                      r=[("pO7", tt), ("x1r", k2)], w=[("ot", k2)])
                    DMA(lambda e, k2=k2, tt=tt, db=db: e.dma_start(out=out[tt * 128:(tt + 1) * 128, db * 512:(db + 1) * 512], in_=ot[k2][:]),
                        r=[("ot", k2)], w=[("out", tt, db)], key=("oto", k2))
        P.barrier()

        P.emit()
    return nc


def prep_shared(inp):
    m = {}
    f = lambda k: np.asarray(inp[k], np.float32)
    m["w_in"] = np.ascontiguousarray(f("w_in")[0])
    m["g1T"] = np.ascontiguousarray(f("norm1_g")[0].reshape(KC, 128).T)
    cw = f("conv_w")[0]
    m["convT"] = np.ascontiguousarray(cw.reshape(4, 48, 128).transpose(2, 1, 0))
    m["qg_col"] = np.ascontiguousarray(f("q_norm_g")[0].reshape(128, 1))
    m["kg_col"] = np.ascontiguousarray(f("k_norm_g")[0].reshape(128, 1))
    m["alog_b"] = np.ascontiguousarray(np.broadcast_to(f("a_log")[0][None, :], (128, NH)))
    m["dtb_b"] = np.ascontiguousarray(np.broadcast_to(f("dt_bias")[0][None, :], (128, NH)))
    m["ident_f"] = np.eye(128, dtype=np.float32)
    m["ident_b"] = np.eye(128, dtype=np.float32).astype(ml_dtypes.bfloat16)
    rb = f("rel_bias")
    m["rel_b"] = np.ascontiguousarray(rb)
    m["oh_c"] = _t5_onehot()
    m["b31_b"] = np.ascontiguousarray(np.broadcast_to(rb[31][None, :], (128, NH)))
    m["J_c"] = np.ascontiguousarray(np.eye(128, dtype=np.float32)[::-1])
    m["um_c"] = np.ascontiguousarray(np.triu(np.ones((128, 128), np.float32)))
    m["ls_c"] = np.ascontiguousarray(np.tril(np.ones((128, 128), np.float32), -1))
    m["gg_col"] = np.ascontiguousarray(f("gdn_norm_g")[0].reshape(128, 1))
    m["w_bra"] = np.ascontiguousarray(f("w_br_a")[0])
    m["w_brb"] = np.ascontiguousarray(f("w_br_b")[0])
    m["w_o"] = np.ascontiguousarray(f("w_out")[0])
    m["g2T"] = np.ascontiguousarray(f("norm2_g")[0].reshape(KC, 128).T)
    m["p_wq"] = np.ascontiguousarray(f("peer_wq")[0])
    m["pk1"] = np.ascontiguousarray(f("peer_k1")[0])
    m["pk2"] = np.ascontiguousarray(f("peer_k2")[0])
    m["p_u"] = np.ascontiguousarray(f("peer_u")[0])
    m["p_v"] = np.ascontiguousarray(f("peer_v")[0])
    return m


def prep_core(inp, c, shared=None):
    b, g = c // 2, c % 2
    m = dict(shared if shared is not None else prep_shared(inp))
    x = np.asarray(inp["x"], np.float32)
    xs = np.zeros((S, D), np.float32)
    kb = np.zeros((128, S), np.float32)
    if g == 1:
        xs[:] = x[b]
    else:
        xs[OWN:] = x[b, :OWN]
        kb[:, :OWN] = -3.0e38
    m["xs"] = xs
    m["keybias"] = kb
    return m


def kernel(**inputs):
    nc = build()
    shared = prep_shared(inputs)
    in_maps = [prep_core(inputs, c, shared) for c in range(8)]
    res = run_bass_kernel_spmd(nc, in_maps, core_ids=list(range(8)))
    out = np.zeros((4, S, D), np.float32)
    for c in range(8):
        b, g = c // 2, c % 2
        out[b, g * OWN:(g + 1) * OWN] = np.asarray(res.results[c]["out"], np.float32)
    return out


def _t5_onehot():
    oh = np.zeros((32, 1280), np.float32)
    for d in range(128):
        n = d
        if n < 16:
            bk_ = n
        else:
            bk_ = 16 + int(np.float32(np.log(np.float32(n) / np.float32(16)) / np.float32(math.log(128 / 16)) * np.float32(16)))
            bk_ = min(bk_, 31)
        oh[bk_, d + 640] += 1.0
        oh[31, d + 640] -= 1.0
    return oh
```

```python
import math
import numpy as np
import ml_dtypes
from contextlib import ExitStack
import concourse.bass as bass
import concourse.mybir as mybir
from concourse.bass_utils import run_bass_kernel_spmd

F32 = mybir.dt.float32
BF16 = mybir.dt.bfloat16
AF = mybir.ActivationFunctionType
ALU = mybir.AluOpType
AX = mybir.AxisListType

ENGS = ("tensor", "vector", "scalar", "gpsimd", "sync")

D = 4096
KC = 32
S = 2048
OWN = 1024
NH = 16
EPS = 1e-6
C_AQ, C_AK, C_AV, C_IQ, C_IK, C_IW = 0, 2048, 4096, 6144, 7168, 7232
C_BQ, C_BK, C_BV, C_BA, C_BB, C_BZ = 7248, 9296, 11344, 13392, 13408, 13424
C_GA, C_GB = 15472, 19568
NCOL = 23664


class Prog:
    def __init__(self, nc, es):
        self.nc = nc
        self.es = es
        self.ops = []
        self.cnt = {e: 0 for e in ENGS}
        self.esem = {e: es.enter_context(nc.semaphore("s_" + e)) for e in ENGS}
        self.semobj = {id(s): s for s in self.esem.values()}
        self.known = {e: {} for e in ENGS}
        self.lastw = {}
        self.reads = {}
        self.dsem = {}
        self.free_dsems = []

    def _deps(self, eng, reads, writes):
        deps = []
        for r in reads:
            t = self.lastw.get(r)
            if t is not None:
                deps.append(t)
        for w in writes:
            t = self.lastw.get(w)
            if t is not None:
                deps.append(t)
            deps.extend(self.reads.get(w, ()))
        best = {}
        for (sid, val) in deps:
            if val > best.get(sid, 0):
                best[sid] = val
        waits = []
        kn = self.known[eng]
        for sid, val in best.items():
            if kn.get(sid, 0) >= val:
                continue
            kn[sid] = val
            waits.append((sid, val))
        return waits

    def _commit(self, tok, reads, writes):
        for r in reads:
            self.reads.setdefault(r, []).append(tok)
        for w in writes:
            self.lastw[w] = tok
            self.reads[w] = []

    def op(self, eng, fn, reads=(), writes=(), nosame=None):
        banks = [k for k in reads if isinstance(k, tuple) and k and k[0] == "bank"]
        if banks:
            reads = [k for k in reads if k not in banks]
            writes = list(writes) + [k for k in banks if k not in writes]
        waits = self._deps(eng, reads, writes)
        if nosame is None:
            nosame = (eng == "tensor")
        if nosame:
            waits = [w for w in waits if w[0] != id(self.esem[eng])]
        self.cnt[eng] += 1
        sem = self.esem[eng]
        tok = (id(sem), self.cnt[eng])
        self.ops.append((eng, fn, waits, (sem, 1)))
        self._commit(tok, reads, writes)
        return tok

    def dma(self, eng, fn, reads=(), writes=(), semkey=None):
        assert len(writes) >= 1
        key = semkey if semkey is not None else writes[0]
        if key not in self.dsem:
            if self.free_dsems:
                ent = self.free_dsems.pop()
            else:
                s = self.es.enter_context(self.nc.semaphore("d%d" % len(self.semobj)))
                self.semobj[id(s)] = s
                ent = [s, 0]
            self.dsem[key] = ent
        ent = self.dsem[key]
        waits = self._deps(eng, reads, writes)
        if ent[1] > 0:
            kn = self.known[eng]
            if kn.get(id(ent[0]), 0) < ent[1] * 16:
                kn[id(ent[0])] = ent[1] * 16
                waits.append((id(ent[0]), ent[1] * 16))
        ent[1] += 1
        tok = (id(ent[0]), ent[1] * 16)
        self.ops.append((eng, fn, waits, (ent[0], 16)))
        self._commit(tok, reads, writes)
        return tok

    def barrier(self):
        toks = []
        for e in ENGS:
            if self.cnt[e] > 0:
                toks.append((id(self.esem[e]), self.cnt[e]))
        for key, ent in self.dsem.items():
            if ent[1] > 0:
                toks.append((id(ent[0]), ent[1] * 16))
        for e in ENGS:
            waits = []
            kn = self.known[e]
            for (sid, val) in toks:
                if kn.get(sid, 0) < val:
                    kn[sid] = val
                    waits.append((sid, val))
            if waits:
                self.ops.append((e, None, waits, None))
        self.lastw = {}
        self.reads = {}
        for key, ent in self.dsem.items():
            self.free_dsems.append(ent)
        self.dsem = {}

    def emit(self):
        nc = self.nc
        with nc.Block() as block:
            def mk(ename):
                def body(e):
                    for (eng, fn, waits, inc) in self.ops:
                        if eng != ename:
                            continue
                        for (sid, val) in waits:
                            e.wait_ge(self.semobj[sid], val)
                        if fn is not None:
                            ins = fn(e)
                            ins.then_inc(inc[0], inc[1])
                return body
            block.sync(mk("sync"))
            block.tensor(mk("tensor"))
            block.vector(mk("vector"))
            block.scalar(mk("scalar"))
            block.gpsimd(mk("gpsimd"))


def bc(ap, shape):
    return ap.to_broadcast(list(shape))


def build(stop_after=None, dbg=()):
    nc = bass.Bass("TRN2", target_bir_lowering=False)
    dbg = set(dbg)

    def din(name, shape, dt=F32):
        return nc.dram_tensor(name, list(shape), dt, kind="ExternalInput").ap()

    def dscr(name, shape, dt=F32):
        kind = "ExternalOutput" if name in dbg else "Internal"
        return nc.dram_tensor(name, list(shape), dt, kind=kind).ap()

    xs = din("xs", [S, D])
    w_in = din("w_in", [D, NCOL])
    g1T = din("g1T", [128, KC])
    convT = din("convT", [128, 48, 4])
    qg_col = din("qg_col", [128, 1])
    kg_col = din("kg_col", [128, 1])
    alog_b = din("alog_b", [128, NH])
    dtb_b = din("dtb_b", [128, NH])
    ident_f = din("ident_f", [128, 128])
    ident_b = din("ident_b", [128, 128], BF16)
    keybias = din("keybias", [128, S])
    rel_b = din("rel_b", [32, NH])
    oh_c = din("oh_c", [32, 1280])
    b31_b = din("b31_b", [128, NH])
    J_c = din("J_c", [128, 128])
    um_c = din("um_c", [128, 128])
    ls_c = din("ls_c", [128, 128])
    gg_col = din("gg_col", [128, 1])
    w_bra = din("w_bra", [2048, D])
    w_brb = din("w_brb", [2048, D])
    w_o = din("w_o", [D, D])
    g2T = din("g2T", [128, KC])
    p_wq = din("p_wq", [D, 2048])
    pk1 = din("pk1", [128, 128])
    pk2 = din("pk2", [128, 128])
    p_u = din("p_u", [16384, D])
    p_v = din("p_v", [16384, D])
    out = nc.dram_tensor("out", [OWN, D], F32, kind="ExternalOutput").ap()

    akT = dscr("akT", [NH, 128, S], BF16)
    aqT = dscr("aqT", [NH, 128, OWN], BF16)
    av = dscr("av", [NH, 128, 16, 128], BF16)
    iqT = dscr("iqT", [8, 128, OWN], BF16)
    ikT2 = dscr("ikT2", [128, S], BF16)
    iw = dscr("iw", [OWN, NH])
    bqT = dscr("bqT", [NH, 128, S])
    bkT = dscr("bkT", [NH, 128, S])
    bk = dscr("bk", [NH, 128, 16, 128])
    bv = dscr("bv", [NH, 128, 16, 128])
    glb = dscr("glb", [S, 2 * NH])
    zsT = dscr("zsT", [NH, 128, OWN])
    gsa = dscr("gsa", [KC, 128, OWN], BF16)
    gsb = dscr("gsb", [KC, 128, OWN], BF16)
    mT = dscr("mT", [8, 128, 16, 128], BF16)
    fv = dscr("fv", [NH, 1280])
    yaT = dscr("yaT", [NH, 128, OWN], BF16)
    ybT = dscr("ybT", [NH, 128, OWN], BF16)
    x1 = dscr("x1", [OWN, D])
    xn2D = dscr("xn2D", [128, KC, OWN], BF16)
    GT = dscr("GT", [128, 128, OWN], BF16)
    GaT = dscr("GaT", [128, 128, OWN], BF16)

    with ExitStack() as es:
        P = Prog(nc, es)

        def sb(st, name, shape, dt):
            return st.enter_context(nc.sbuf_tensor(name, list(shape), dt))

        def ps(st, name, shape, dt=F32):
            return st.enter_context(nc.psum_tensor(name, list(shape), dt))

        identF = sb(es, "identF", [128, 128], F32)
        identB = sb(es, "identB", [128, 128], BF16)
        onesF = sb(es, "onesF", [128, 128], F32)
        P.dma("sync", lambda e: e.dma_start(out=identF[:], in_=ident_f), writes=["identF"])
        P.dma("sync", lambda e: e.dma_start(out=identB[:], in_=ident_b), writes=["identB"])
        P.op("vector", lambda e: e.memset(onesF[:], 1.0), writes=["onesF"])

        with ExitStack() as st:
            xnT = sb(st, "xnT", [128, KC, S], BF16)
            g1s = sb(st, "g1s", [128, KC], F32)
            P.dma("sync", lambda e: e.dma_start(out=g1s[:], in_=g1T), writes=["g1s"])
            with ExitStack() as st1:
                xf = [sb(st1, "xf%d" % i, [128, D], F32) for i in range(2)]
                xb = [sb(st1, "xb%d" % i, [128, D], BF16) for i in range(2)]
                junk = sb(st1, "junk", [128, D], BF16)
                ss = sb(st1, "ss", [128, 16], F32)
                rstd = sb(st1, "rstd", [128, 16], F32)
                pt = [ps(st1, "pt%d" % i, [128, 8, 128], BF16) for i in range(2)]
                P.op("vector", lambda e: e.memset(ss[:], 0.0), writes=["ss"])
                for i in range(16):
                    b = i % 2
                    P.dma("sync", lambda e, i=i, b=b: e.dma_start(out=xf[b][:], in_=xs[i * 128:(i + 1) * 128, :]),
                          writes=[("xf", b)])
                    P.op("scalar", lambda e, i=i, b=b: e.activation(out=junk[:], in_=xf[b][:], func=AF.Square,
                                                                   accum_out=ss[:, i:i + 1]),
                         reads=[("xf", b), "ss"], writes=["junk", ("ss", i)])
                    P.op("vector", lambda e, i=i: e.tensor_scalar(out=rstd[:, i:i + 1], in0=ss[:, i:i + 1],
                                                                  scalar1=1.0 / D, scalar2=EPS, op0=ALU.mult, op1=ALU.add),
                         reads=[("ss", i)], writes=[("rstd", i)])
                    P.op("scalar", lambda e, i=i: e.activation(out=rstd[:, i:i + 1], in_=rstd[:, i:i + 1], func=AF.Sqrt),
                         reads=[("rstd", i)], writes=[("rstd", i)])
                    P.op("vector", lambda e, i=i: e.reciprocal(out=rstd[:, i:i + 1], in_=rstd[:, i:i + 1]),
                         reads=[("rstd", i)], writes=[("rstd", i)])
                    P.op("scalar", lambda e, i=i, b=b: e.activation(out=xb[b][:], in_=xf[b][:], func=AF.Copy,
                                                                   scale=rstd[:, i:i + 1]),
                         reads=[("xf", b), ("rstd", i)], writes=[("xb", b)])
                    for g in range(4):
                        pb = g % 2
                        for c in range(8):
                            P.op("tensor", lambda e, b=b, g=g, c=c, pb=pb: e.transpose(
                                out=pt[pb][:, c, :], in_=xb[b][:, (g * 8 + c) * 128:(g * 8 + c + 1) * 128], identity=identB[:]),
                                reads=[("xb", b), "identB"], writes=[("pt", pb)])
                        P.op("vector", lambda e, i=i, g=g, pb=pb: e.tensor_tensor(
                            out=xnT[:, g * 8:(g + 1) * 8, i * 128:(i + 1) * 128], in0=pt[pb][:],
                            in1=bc(g1s[:, g * 8:(g + 1) * 8].unsqueeze(2), [128, 8, 128]), op=ALU.mult),
                            reads=[("pt", pb), "g1s"], writes=[("xnT", i)])
            P.barrier()

            with ExitStack() as st2:
                NWB = 2
                wt = [sb(st2, "wt%d" % i, [128, KC, 256], BF16) for i in range(NWB)]
                raw = sb(st2, "raw", [128, 4 + S], F32)
                cv = [sb(st2, "cv%d" % i, [128, 512], F32) for i in range(2)]
                sl = [sb(st2, "sl%d" % i, [128, 512], F32) for i in range(2)]
                sq = [sb(st2, "sq%d" % i, [128, 512], F32) for i in range(2)]
                rn = [sb(st2, "rn%d" % i, [128, 512], F32) for i in range(2)]
                stf = [sb(st2, "stf%d" % i, [128, 512], F32) for i in range(2)]
                stb = [sb(st2, "stb%d" % i, [128, 512], BF16) for i in range(2)]
                ttm = [sb(st2, "ttm%d" % i, [128, 4, 128], F32) for i in range(2)]
                ttb = [sb(st2, "ttb%d" % i, [128, 4, 128], BF16) for i in range(2)]
                cw = sb(st2, "cw", [128, 48, 4], F32)
                qg = sb(st2, "qg", [128, 1], F32)
                kg = sb(st2, "kg", [128, 1], F32)
                alb = sb(st2, "alb", [128, NH], F32)
                dtb = sb(st2, "dtb", [128, NH], F32)
                sm = [sb(st2, "sm%d" % i, [128, 32], F32) for i in range(6)]
                pA = ps(st2, "pA", [128, 4, 512], F32)
                pB = [ps(st2, "pB%d" % i, [128, 512], F32) for i in range(2)]
                pTf = ps(st2, "pTf", [128, 4, 128], F32)
                pTb = ps(st2, "pTb", [128, 4, 128], BF16)
                P.dma("sync", lambda e: e.dma_start(out=cw[:], in_=convT), writes=["cw"])
                P.dma("sync", lambda e: e.dma_start(out=qg[:], in_=qg_col), writes=["qg"])
                P.dma("sync", lambda e: e.dma_start(out=kg[:], in_=kg_col), writes=["kg"])
                P.dma("sync", lambda e: e.dma_start(out=alb[:], in_=alog_b), writes=["alb"])
                P.dma("sync", lambda e: e.dma_start(out=dtb[:], in_=dtb_b), writes=["dtb"])
                P.op("vector", lambda e: e.memset(raw[:, 0:4], 0.0), writes=["rawpad"])
                P.op("scalar", lambda e: e.activation(out=alb[:], in_=alb[:], func=AF.Exp), reads=["alb"], writes=["alb"])
                P.op("vector", lambda e: e.tensor_scalar(out=alb[:], in0=alb[:], scalar1=-1.0, scalar2=None, op0=ALU.mult),
                     reads=["alb"], writes=["alb"])

                plan = []
                for h in range(NH):
                    plan.append(dict(kind="ak", segs=[(C_AK + h * 128, 128)], own=False, h=h))
                for h in range(NH):
                    plan.append(dict(kind="aq", segs=[(C_AQ + h * 128, 128)], own=True, h=h))
                for h in range(NH):
                    plan.append(dict(kind="av", segs=[(C_AV + h * 128, 128)], own=False, h=h))
                for j in range(8):
                    plan.append(dict(kind="iq", segs=[(C_IQ + j * 128, 128)], own=True, h=j))
                plan.append(dict(kind="ik", segs=[(C_IK, 64), (C_IK, 64)], own=False, h=0))
                plan.append(dict(kind="iw", segs=[(C_IW, 16)], own=True, h=0))
                for wh, nm, c0 in ((0, "bq", C_BQ), (1, "bk", C_BK), (2, "bv", C_BV)):
                    for h in range(NH):
                        plan.append(dict(kind=nm, segs=[(c0 + h * 128, 128)], own=False, h=h, cb=wh * 16 + h))
                plan.append(dict(kind="bab", segs=[(C_BA, 32)], own=False, h=0))
                for h in range(NH):
                    plan.append(dict(kind="bz", segs=[(C_BZ + h * 128, 128)], own=True, h=h))
                for j in range(KC):
                    plan.append(dict(kind="ga", segs=[(C_GA + j * 128, 128)], own=True, h=j))
                for j in range(KC):
                    plan.append(dict(kind="gb", segs=[(C_GB + j * 128, 128)], own=True, h=j))
                if stop_after == "p2small":
                    plan = [p for p in plan if p["h"] == 0 or p["kind"] in ("ik", "iw", "bab")]

                tiles = []
                i = 0
                while i < len(plan):
                    a = plan[i]
                    if (i + 1 < len(plan) and len(a["segs"]) == 1 and a["segs"][0][1] == 128
                            and len(plan[i + 1]["segs"]) == 1 and plan[i + 1]["segs"][0][1] == 128
                            and plan[i + 1]["segs"][0][0] == a["segs"][0][0] + 128):
                        tiles.append([a, plan[i + 1]])
                        i += 2
                    else:
                        tiles.append([a])
                        i += 1

                w_v = w_in.rearrange("(c p) n -> p c n", p=128)

                def load_tile(ti):
                    t = tiles[ti]
                    wb = ti % NWB
                    off = 0
                    parts = []
                    for blk in t:
                        for (c0, n) in blk["segs"]:
                            parts.append((off, c0, n))
                            off += n
                    merged = []
                    for (o, c0, n) in parts:
                        if merged and merged[-1][1] + merged[-1][2] == c0 and merged[-1][0] + merged[-1][2] == o:
                            merged[-1] = (merged[-1][0], merged[-1][1], merged[-1][2] + n)
                        else:
                            merged.append((o, c0, n))
                    for q in range(4):
                        for (o, c0, n) in merged:
                            P.dma("gpsimd", lambda e, wb=wb, q=q, o=o, c0=c0, n=n: e.dma_start(
                                out=wt[wb][:, q * 8:(q + 1) * 8, o:o + n], in_=w_v[:, q * 8:(q + 1) * 8, c0:c0 + n]),
                                writes=[("wt", wb, q, o)], semkey=("wt", wb, q, o))

                def wt_res(wb):
                    return [k for k in list(P.lastw.keys()) if isinstance(k, tuple) and k[0] == "wt" and k[1] == wb]

                cnt2 = [0]

                def nxt():
                    cnt2[0] += 1
                    return cnt2[0] % 2

                for ti in range(min(NWB - 1, len(tiles))):
                    load_tile(ti)
                for ti, t in enumerate(tiles):
                    if ti + NWB - 1 < len(tiles):
                        load_tile(ti + NWB - 1)
                    wb = ti % NWB
                    off = 0
                    for blk in t:
                        M = sum(n for (_, n) in blk["segs"])
                        own = blk["own"]
                        sbl = [2, 3] if own else [0, 1, 2, 3]
                        kind = blk["kind"]
                        h = blk["h"]
                        wres = wt_res(wb)
                        for c in range(KC):
                            for j in sbl:
                                P.op("tensor", lambda e, wb=wb, c=c, j=j, off=off, M=M: e.matmul(
                                    pA[0:M, j, :], lhsT=wt[wb][:, c, off:off + M], rhs=xnT[:, c, j * 512:(j + 1) * 512],
                                    start=(c == 0), stop=(c == KC - 1)),
                                    reads=wres if (c == 0 and j == sbl[0]) or (c == KC - 1 and j == sbl[-1]) else [], writes=[("pA", j)])
                        if kind in ("bq", "bk", "bv"):
                            for j in sbl:
                                P.op("scalar", lambda e, j=j: e.activation(out=raw[:, 4 + j * 512:4 + (j + 1) * 512], in_=pA[:, j, :],
                                                                          func=AF.Copy),
                                     reads=[("pA", j), "rawpad"], writes=[("raw", j)])
                            cb = blk["cb"]
                            for j in sbl:
                                k = nxt()
                                rr = [("raw", j)] + ([("raw", j - 1)] if j > 0 else [])
                                P.op("vector", lambda e, j=j, k=k, cb=cb: e.tensor_scalar(
                                    out=cv[k][:], in0=raw[:, 1 + j * 512:1 + (j + 1) * 512], scalar1=cw[:, cb, 0:1], scalar2=None,
                                    op0=ALU.mult), reads=rr + ["cw"], writes=[("cv", k)])
                                for jj in (1, 2, 3):
                                    P.op("vector", lambda e, j=j, k=k, cb=cb, jj=jj: e.scalar_tensor_tensor(
                                        out=cv[k][:], in0=raw[:, 1 + jj + j * 512:1 + jj + (j + 1) * 512], scalar=cw[:, cb, jj:jj + 1],
                                        in1=cv[k][:], op0=ALU.mult, op1=ALU.add), reads=rr + [("cv", k)], writes=[("cv", k)])
                                P.op("scalar", lambda e, k=k: e.activation(out=sl[k][:], in_=cv[k][:], func=AF.Silu),
                                     reads=[("cv", k)], writes=[("sl", k)])
                                src = sl[k]
                                srckey = ("sl", k)
                                if kind in ("bq", "bk"):
                                    P.op("scalar", lambda e, k=k: e.activation(out=sq[k][:], in_=sl[k][:], func=AF.Square),
                                         reads=[("sl", k)], writes=[("sq", k)])
                                    P.op("tensor", lambda e, k=k: e.matmul(pB[k][:], lhsT=onesF[:], rhs=sq[k][:], start=True, stop=True),
                                         reads=[("sq", k), "onesF"], writes=[("pB", k)])
                                    P.op("scalar", lambda e, k=k: e.activation(out=rn[k][:], in_=pB[k][:], func=AF.Sqrt, bias=EPS,
                                                                              scale=1.0),
                                         reads=[("pB", k)], writes=[("rn", k)])
                                    P.op("vector", lambda e, k=k: e.reciprocal(out=rn[k][:], in_=rn[k][:]),
                                         reads=[("rn", k)], writes=[("rn", k)])
                                    scl = (128.0 ** -0.5) if kind == "bq" else 1.0
                                    P.op("vector", lambda e, k=k, scl=scl: e.scalar_tensor_tensor(
                                        out=stf[k][:], in0=sl[k][:], scalar=scl, in1=rn[k][:], op0=ALU.mult, op1=ALU.mult),
                                        reads=[("sl", k), ("rn", k)], writes=[("stf", k)])
                                    dst = bqT if kind == "bq" else bkT
                                    P.dma("sync", lambda e, k=k, h=h, j=j, dst=dst: e.dma_start(
                                        out=dst[h, :, j * 512:(j + 1) * 512], in_=stf[k][:]),
                                        reads=[("stf", k)], writes=[(kind, h, j)], semkey=("stfo", k))
                                    src = stf[k]
                                    srckey = ("stf", k)
                                if kind in ("bk", "bv"):
                                    for q in range(4):
                                        P.op("tensor", lambda e, q=q, src=src: e.transpose(out=pTf[:, q, :], in_=src[:, q * 128:(q + 1) * 128],
                                                                                           identity=identF[:]),
                                             reads=[srckey, "identF"], writes=["pTf"])
                                    P.op("vector", lambda e, k=k: e.tensor_copy(out=ttm[k][:], in_=pTf[:]),
                                         reads=["pTf"], writes=[("ttm", k)])
                                    dst = bk if kind == "bk" else bv
                                    P.dma("sync", lambda e, k=k, h=h, j=j, dst=dst: e.dma_start(
                                        out=dst[h, :, j * 4:(j + 1) * 4, :], in_=ttm[k][:]),
                                        reads=[("ttm", k)], writes=[(kind + "t", h, j)], semkey=("ttmo", k))
                        elif kind in ("ak", "aq"):
                            gcol = kg if kind == "ak" else qg
                            for j in sbl:
                                k = nxt()
                                P.op("scalar", lambda e, j=j, k=k: e.activation(out=sl[k][:], in_=pA[:, j, :], func=AF.Copy),
                                     reads=[("pA", j)], writes=[("sl", k)])
                                P.op("scalar", lambda e, k=k: e.activation(out=sq[k][:], in_=sl[k][:], func=AF.Square),
                                     reads=[("sl", k)], writes=[("sq", k)])
                                P.op("tensor", lambda e, k=k: e.matmul(pB[k][:], lhsT=onesF[:], rhs=sq[k][:], start=True, stop=True),
                                     reads=[("sq", k), "onesF"], writes=[("pB", k)])
                                P.op("scalar", lambda e, k=k: e.activation(out=rn[k][:], in_=pB[k][:], func=AF.Sqrt, bias=EPS,
                                                                          scale=1.0 / 128.0),
                                     reads=[("pB", k)], writes=[("rn", k)])
                                P.op("vector", lambda e, k=k: e.reciprocal(out=rn[k][:], in_=rn[k][:]),
                                     reads=[("rn", k)], writes=[("rn", k)])
                                P.op("vector", lambda e, k=k, gcol=gcol: e.scalar_tensor_tensor(
                                    out=stb[k][:], in0=sl[k][:], scalar=gcol[:, 0:1], in1=rn[k][:], op0=ALU.mult, op1=ALU.mult),
                                    reads=[("sl", k), ("rn", k), "qg", "kg"], writes=[("stb", k)])
                                if kind == "ak":
                                    P.dma("sync", lambda e, k=k, h=h, j=j: e.dma_start(out=akT[h, :, j * 512:(j + 1) * 512], in_=stb[k][:]),
                                          reads=[("stb", k)], writes=[("akT", h, j)], semkey=("stbo", k))
                                else:
                                    P.dma("sync", lambda e, k=k, h=h, j=j: e.dma_start(out=aqT[h, :, (j - 2) * 512:(j - 1) * 512], in_=stb[k][:]),
                                          reads=[("stb", k)], writes=[("aqT", h, j)], semkey=("stbo", k))
                        elif kind == "av":
                            for j in sbl:
                                k = nxt()
                                P.op("scalar", lambda e, j=j, k=k: e.activation(out=stb[k][:], in_=pA[:, j, :], func=AF.Copy),
                                     reads=[("pA", j)], writes=[("stb", k)])
                                for q in range(4):
                                    P.op("tensor", lambda e, q=q, k=k: e.transpose(out=pTb[:, q, :], in_=stb[k][:, q * 128:(q + 1) * 128],
                                                                                   identity=identB[:]),
                                         reads=[("stb", k), "identB"], writes=["pTb"])
                                P.op("vector", lambda e, k=k: e.tensor_copy(out=ttb[k][:], in_=pTb[:]),
                                     reads=["pTb"], writes=[("ttb", k)])
                                P.dma("sync", lambda e, k=k, h=h, j=j: e.dma_start(
                                    out=av[h, :, j * 4:(j + 1) * 4, :], in_=ttb[k][:]),
                                    reads=[("ttb", k)], writes=[("av", h, j)], semkey=("ttbo", k))
                        elif kind in ("iq", "ik", "ga", "gb"):
                            func = AF.Sigmoid if kind in ("ga", "gb") else AF.Copy
                            for j in sbl:
                                k = nxt()
                                P.op("scalar", lambda e, j=j, k=k, func=func: e.activation(out=stb[k][:], in_=pA[:, j, :], func=func),
                                     reads=[("pA", j)], writes=[("stb", k)])
                                if kind == "ik":
                                    dsto = ikT2[:, j * 512:(j + 1) * 512]
                                elif kind == "iq":
                                    dsto = iqT[h, :, (j - 2) * 512:(j - 1) * 512]
                                elif kind == "ga":
                                    dsto = gsa[h, :, (j - 2) * 512:(j - 1) * 512]
                                else:
                                    dsto = gsb[h, :, (j - 2) * 512:(j - 1) * 512]
                                P.dma("sync", lambda e, k=k, dsto=dsto: e.dma_start(out=dsto, in_=stb[k][:]),
                                      reads=[("stb", k)], writes=[(kind, h, j)], semkey=("stbo", k))
                        elif kind == "bz":
                            for j in sbl:
                                k = nxt()
                                P.op("scalar", lambda e, j=j, k=k: e.activation(out=stf[k][:], in_=pA[:, j, :], func=AF.Silu),
                                     reads=[("pA", j)], writes=[("stf", k)])
                                P.dma("sync", lambda e, k=k, h=h, j=j: e.dma_start(out=zsT[h, :, (j - 2) * 512:(j - 1) * 512], in_=stf[k][:]),
                                      reads=[("stf", k)], writes=[("zsT", h, j)], semkey=("stfo", k))
                        elif kind in ("iw", "bab"):
                            for j in sbl:
                                k = nxt()
                                P.op("scalar", lambda e, j=j, k=k, M=M: e.activation(out=sl[k][0:M, :], in_=pA[0:M, j, :], func=AF.Copy),
                                     reads=[("pA", j)], writes=[("sl", k)])
                                for q in range(4):
                                    P.op("tensor", lambda e, q=q, k=k, M=M: e.transpose(out=pTf[:, q, 0:M], in_=sl[k][0:M, q * 128:(q + 1) * 128],
                                                                                        identity=identF[0:M, 0:M]),
                                         reads=[("sl", k), "identF"], writes=["pTf"])
                                P.op("vector", lambda e, k=k, M=M: e.tensor_copy(out=ttm[k][:, :, 0:M], in_=pTf[:, :, 0:M]),
                                     reads=["pTf"], writes=[("ttm", k)])
                                if kind == "iw":
                                    P.dma("sync", lambda e, k=k, j=j: e.dma_start(
                                        out=iw[(j - 2) * 512:(j - 1) * 512, :].rearrange("(q p) d -> p q d", p=128), in_=ttm[k][:, :, 0:16]),
                                        reads=[("ttm", k)], writes=[("iw", j)], semkey=("ttmo", k))
                                else:
                                    xa, ax_, ee = sm[0], sm[1], sm[2]
                                    for q in range(4):
                                        P.op("vector", lambda e, k=k, q=q: e.tensor_tensor(out=ttm[k][:, q, 0:16], in0=ttm[k][:, q, 0:16], in1=dtb[:],
                                                                                          op=ALU.add),
                                             reads=[("ttm", k), "dtb"], writes=[("ttm", k)])
                                    xv = ttm[k][:, :, 0:16]
                                    tmpa = ttm[k][:, :, 32:48]
                                    tmpb = ttm[k][:, :, 48:64]
                                    P.op("vector", lambda e, xv=xv, tmpa=tmpa: e.tensor_scalar(out=tmpa, in0=xv, scalar1=60.0, scalar2=None, op0=ALU.min),
                                         reads=[("ttm", k)], writes=[("ttm", k)])
                                    P.op("scalar", lambda e, tmpa=tmpa: e.activation(out=tmpa, in_=tmpa, func=AF.Exp),
                                         reads=[("ttm", k)], writes=[("ttm", k)])
                                    P.op("scalar", lambda e, tmpa=tmpa: e.activation(out=tmpa, in_=tmpa, func=AF.Ln, bias=1.0, scale=1.0),
                                         reads=[("ttm", k)], writes=[("ttm", k)])
                                    for q in range(4):
                                        P.op("vector", lambda e, k=k, q=q: e.tensor_tensor(out=ttm[k][:, q, 0:16], in0=ttm[k][:, q, 32:48], in1=alb[:],
                                                                                          op=ALU.mult),
                                             reads=[("ttm", k), "alb"], writes=[("ttm", k)])
                                    P.op("scalar", lambda e, k=k: e.activation(out=ttm[k][:, :, 16:32], in_=ttm[k][:, :, 16:32], func=AF.Sigmoid),
                                         reads=[("ttm", k)], writes=[("ttm", k)])
                                    P.dma("sync", lambda e, k=k, j=j: e.dma_start(
                                        out=glb[j * 512:(j + 1) * 512, :].rearrange("(q p) d -> p q d", p=128), in_=ttm[k][:, :, 0:32]),
                                        reads=[("ttm", k)], writes=[("glb", j)], semkey=("ttmo", k))
                        off += M
            P.barrier()

        if stop_after in ("p2", "p2small"):
            P.emit()
            return nc

        import os as _os
        LIM = [int(_os.environ.get("OPLIM", "1000000000")), 0, False]

        def _lim():
            if LIM[2]:
                LIM[1] += 1
                return LIM[1] > LIM[0]
            return False

        REC = [None]

        def _op(eng, fn, r, w):
            if REC[0] is not None:
                REC[0].append(("op", eng, fn, list(r), list(w), None))
                return None
            return P.op(eng, fn, reads=r, writes=w)

        def V(fn, r=(), w=()):
            return _op("vector", fn, r, w)

        def A(fn, r=(), w=()):
            return _op("scalar", fn, r, w)

        def G(fn, r=(), w=()):
            return _op("gpsimd", fn, r, w)

        def T(fn, r=(), w=()):
            return _op("tensor", fn, r, w)

        def DMA(fn, r=(), w=(), key=None, eng="sync"):
            if REC[0] is not None:
                REC[0].append(("dma", eng, fn, list(r), list(w), key))
                return None
            return P.dma(eng, fn, reads=r, writes=w, semkey=key)

        def emit_rec(o):
            kind_, eng, fn, r, w, key = o
            if kind_ == "op":
                P.op(eng, fn, reads=r, writes=w)
            else:
                P.dma(eng, fn, reads=r, writes=w, semkey=key)

        with ExitStack() as st:
            ikS = sb(st, "ikS", [128, S], BF16)
            kbS = sb(st, "kbS", [128, S], F32)
            iqA = sb(st, "iqA", [128, 8, OWN], BF16)
            iwS = [sb(st, "iwS%d" % i, [128, 16], F32) for i in range(2)]
            rl = [sb(st, "rl%d" % i, [128, 2, 512], F32) for i in range(3)]
            acc = sb(st, "acc", [128, S], F32)
            sc = sb(st, "sc", [128, S], F32)
            wk = sb(st, "wk", [128, S], F32)
            mx = sb(st, "mx", [128, 8], F32)
            mk = sb(st, "mk", [128, S], BF16)
            mts = [sb(st, "mts%d" % i, [128, 16, 128], BF16) for i in range(2)]
            pS = [ps(st, "pS%d" % i, [128, 2, 512]) for i in range(3)]
            pT = [ps(st, "pT%d" % i, [128, 8, 128], BF16) for i in range(2)]
            DMA(lambda e: e.dma_start(out=ikS[:], in_=ikT2), w=["ikS"])
            DMA(lambda e: e.dma_start(out=kbS[:], in_=keybias), w=["kbS"])
            DMA(lambda e: e.dma_start(out=iqA[:], in_=iqT.rearrange("j p t -> p j t")), w=["iqA"])
            for qt in range(8):
                b = qt % 2
                DMA(lambda e, b=b, qt=qt: e.dma_start(out=iwS[b][:], in_=iw[qt * 128:(qt + 1) * 128, :]), w=[("iwS", b)])
                for hi in range(16):
                    blk, sub = hi // 2, hi % 2
                    for half in range(2):
                        r = (hi * 2 + half) % 3
                        for n in range(2):
                            c0 = half * 1024 + n * 512
                            T(lambda e, r=r, n=n, qt=qt, sub=sub, blk=blk, c0=c0: e.matmul(
                                pS[r][:, n, :], lhsT=iqA[sub * 64:(sub + 1) * 64, blk, qt * 128:(qt + 1) * 128], rhs=ikS[sub * 64:(sub + 1) * 64, c0:c0 + 512],
                                start=True, stop=True), r=["iqA", "ikS"], w=[("pS", r)])
                        A(lambda e, r=r: e.activation(out=rl[r][:], in_=pS[r][:], func=AF.Relu), r=[("pS", r)], w=[("rl", r)])
                        acch = acc[:, half * 1024:(half + 1) * 1024]
                        rlf = rl[r][:].rearrange("p a b -> p (a b)")
                        if hi == 0:
                            V(lambda e, b=b, acch=acch, rlf=rlf: e.tensor_scalar(out=acch, in0=rlf, scalar1=iwS[b][:, 0:1], scalar2=None, op0=ALU.mult),
                              r=[("rl", r), ("iwS", b)], w=[("acc", half)])
                        else:
                            V(lambda e, b=b, acch=acch, rlf=rlf, hi=hi: e.scalar_tensor_tensor(
                                out=acch, in0=rlf, scalar=iwS[b][:, hi:hi + 1], in1=acch, op0=ALU.mult, op1=ALU.add),
                              r=[("rl", r), ("iwS", b), ("acc", half)], w=[("acc", half)])
                V(lambda e: e.tensor_tensor(out=acc[:], in0=acc[:], in1=kbS[:], op=ALU.add), r=[("acc", 0), ("acc", 1), "kbS"], w=[("acc", 0), ("acc", 1)])
                G(lambda e, qt=qt: e.affine_select(out=sc[:], in_=acc[:], pattern=[[-1, S]], compare_op=ALU.is_ge, fill=-3.0e38,
                                                   base=OWN + qt * 128, channel_multiplier=1),
                  r=[("acc", 0), ("acc", 1)], w=["sc"])
                for rr in range(32):
                    srcb = sc if rr == 0 else wk
                    srck = "sc" if rr == 0 else "wk"
                    V(lambda e, srcb=srcb: e.max(out=mx[:], in_=srcb[:]), r=[srck], w=["mx"])
                    if rr < 31:
                        V(lambda e, srcb=srcb: e.match_replace(out=wk[:], in_to_replace=mx[:], in_values=srcb[:], imm_value=-1.0e30),
                          r=[srck, "mx"], w=["wk"])
                V(lambda e: e.tensor_scalar(out=mk[:], in0=sc[:], scalar1=mx[:, 7:8], scalar2=None, op0=ALU.is_ge), r=["sc", "mx"], w=["mk"])
                for g2 in range(2):
                    for c in range(8):
                        T(lambda e, g2=g2, c=c: e.transpose(out=pT[g2][:, c, :], in_=mk[:, (g2 * 8 + c) * 128:(g2 * 8 + c + 1) * 128], identity=identB[:]),
                          r=["mk", "identB"], w=[("pT", g2)])
                    A(lambda e, g2=g2, b=b: e.activation(out=mts[b][:, g2 * 8:(g2 + 1) * 8, :], in_=pT[g2][:], func=AF.Copy),
                      r=[("pT", g2)], w=[("mts", b)])
                DMA(lambda e, b=b, qt=qt: e.dma_start(out=mT[qt], in_=mts[b][:]),
                    r=[("mts", b)], w=[("mT", qt)], key=("mtso", b))
        P.barrier()
        if stop_after == "p3":
            P.emit()
            return nc

        with ExitStack() as st:
            mTS = sb(st, "mTS", [128, 8, 16, 128], BF16)
            rbS = sb(st, "rbS", [32, NH], F32)
            ohS = sb(st, "ohS", [32, 1280], F32)
            fvS = sb(st, "fvS", [NH, 1280], F32)
            b31S = sb(st, "b31S", [128, NH], F32)
            JS = sb(st, "JS", [128, 128], F32)
            onesB = sb(st, "onesB", [128, 128], BF16)
            hk = [sb(st, "hk%d" % i, [128, 512], F32) for i in range(2)]
            corr = [sb(st, "corr%d" % i, [128, 5, 512], BF16) for i in range(2)]
            kTS = [sb(st, "kTS%d" % i, [128, S], BF16) for i in range(2)]
            qTS = [sb(st, "qTS%d" % i, [128, OWN], BF16) for i in range(2)]
            vS = [sb(st, "vS%d" % i, [128, 16, 128], BF16) for i in range(2)]
            Eb = [sb(st, "Eb%d" % i, [128, 512], BF16) for i in range(3)]
            Pm = [sb(st, "Pm%d" % i, [128, 512], BF16) for i in range(3)]
            rinv = [sb(st, "rinv%d" % i, [128, 512], F32) for i in range(2)]
            yab = [sb(st, "yab%d" % i, [128, 512], BF16) for i in range(2)]
            pS4 = [ps(st, "pS4%d" % i, [128, 512]) for i in range(3)]
            pO = [ps(st, "pO%d" % i, [128, 512]) for i in range(2)]
            pR = [ps(st, "pR%d" % i, [128, 512]) for i in range(2)]
            pC = ps(st, "pC", [128, 512])
            DMA(lambda e: e.dma_start(out=mTS[:], in_=mT.rearrange("q p s t -> p q s t")), w=["mTS"])
            DMA(lambda e: e.dma_start(out=rbS[:], in_=rel_b), w=["rbS"])
            DMA(lambda e: e.dma_start(out=ohS[:], in_=oh_c), w=["ohS"])
            DMA(lambda e: e.dma_start(out=b31S[:], in_=b31_b), w=["b31S"])
            DMA(lambda e: e.dma_start(out=JS[:], in_=J_c), w=["JS"])
            V(lambda e: e.memset(onesB[:], 1.0), w=["onesB"])
            tg = [pO[0], pO[1], pR[0]]
            for n3, (c0, cn) in enumerate(((0, 512), (512, 512), (1024, 256))):
                T(lambda e, n3=n3, c0=c0, cn=cn: e.matmul(tg[n3][0:NH, 0:cn], lhsT=rbS[:], rhs=ohS[:, c0:c0 + cn], start=True, stop=True),
                  r=["rbS", "ohS"], w=[("tg", n3)])
                A(lambda e, n3=n3, c0=c0, cn=cn: e.activation(out=fvS[:, c0:c0 + cn], in_=tg[n3][0:NH, 0:cn], func=AF.Exp),
                  r=[("tg", n3)], w=["fvS"])
            DMA(lambda e: e.dma_start(out=fv, in_=fvS[:]), r=["fvS"], w=["fv"])
            P.barrier()
            OFFS = (128, 0, -128, -256, -384)
            import os
            P4H = int(os.environ.get("P4H", NH))
            LA = 2

            def p4_loads(h):
                b = h % 2
                DMA(lambda e, b=b, h=h: e.dma_start(out=kTS[b][:], in_=akT[h]), w=[("kTS", b)])
                DMA(lambda e, b=b, h=h: e.dma_start(out=qTS[b][:], in_=aqT[h]), w=[("qTS", b)])
                DMA(lambda e, b=b, h=h: e.dma_start(out=vS[b][:], in_=av[h]), w=[("vS", b)])

            def p4_corr(h, oi):
                b = h % 2
                o = OFFS[oi]
                hb = oi % 2
                src_ap = bass.AP(fv.tensor, h * 1280 + o + 513, [[1, 128], [1, 512]])
                DMA(lambda e, hb=hb, src_ap=src_ap: e.dma_start(out=hk[hb][:], in_=src_ap), w=[("hk", hb)])
                T(lambda e, hb=hb: e.matmul(pC[:], lhsT=JS[:], rhs=hk[hb][:], start=True, stop=True), r=[("hk", hb), "JS"], w=["pC"])
                A(lambda e, b=b, oi=oi: e.activation(out=corr[b][:, oi, :], in_=pC[:], func=AF.Copy), r=["pC"], w=[("corr", b)])

            if P4H > 0:
                p4_loads(0)
                for oi in range(5):
                    p4_corr(0, oi)
            for h in range(P4H):
                b = h % 2
                if h + 1 < P4H:
                    p4_loads(h + 1)
                pairs = [(tb, sti) for tb in range(2) for sti in range(12 if tb == 0 else 16)]
                npairs = len(pairs)

                def s_stage(p, h=h, b=b):
                    tb, sti = pairs[p]
                    k2 = p % 3
                    o = OWN + 512 * tb - 128 * sti
                    T(lambda e, k2=k2, b=b, sti=sti, tb=tb: e.matmul(pS4[k2][:], lhsT=kTS[b][:, sti * 128:(sti + 1) * 128],
                                                                  rhs=qTS[b][:, tb * 512:(tb + 1) * 512], start=True, stop=True),
                      r=[("kTS", b), ("qTS", b)], w=[("pS4", k2)])
                    A(lambda e, k2=k2, h=h: e.activation(out=Eb[k2][:], in_=pS4[k2][:], func=AF.Exp, bias=b31S[:, h:h + 1],
                                                        scale=128.0 ** -0.5), r=[("pS4", k2), "b31S"], w=[("Eb", k2)])
                    V(lambda e, k2=k2, sti=sti, tb=tb: e.tensor_tensor(out=Pm[k2][:].rearrange("p (a b) -> p a b", a=4),
                                                                      in0=Eb[k2][:].rearrange("p (a b) -> p a b", a=4),
                                                                      in1=mTS[:, tb * 4:(tb + 1) * 4, sti, :],
                                                                      op=ALU.mult), r=[("Eb", k2), "mTS"], w=[("Pm", k2)])
                    if o in OFFS:
                        oi = OFFS.index(o)
                        G(lambda e, k2=k2, b=b, oi=oi: e.tensor_tensor(out=Pm[k2][:], in0=Pm[k2][:], in1=corr[b][:, oi, :], op=ALU.mult),
                          r=[("Pm", k2), ("corr", b)], w=[("Pm", k2)])

                def pv_stage(p, h=h, b=b):
                    tb, sti = pairs[p]
                    k2 = p % 3
                    ob = tb
                    nst = 12 if tb == 0 else 16
                    T(lambda e, ob=ob, b=b, sti=sti, k2=k2, nst=nst: e.matmul(pO[ob][:], lhsT=vS[b][:, sti, :], rhs=Pm[k2][:],
                                                                          start=(sti == 0), stop=(sti == nst - 1)),
                      r=[("Pm", k2), ("vS", b)], w=[("pO", ob)])
                    T(lambda e, ob=ob, k2=k2, sti=sti, nst=nst: e.matmul(pR[ob][:], lhsT=onesB[:], rhs=Pm[k2][:],
                                                                     start=(sti == 0), stop=(sti == nst - 1)),
                      r=[("Pm", k2), "onesB"], w=[("pR", ob)])
                    if sti == nst - 1:
                        V(lambda e, ob=ob: e.reciprocal(out=rinv[ob][:], in_=pR[ob][:]), r=[("pR", ob)], w=[("rinv", ob)])
                        V(lambda e, ob=ob: e.tensor_tensor(out=yab[ob][:], in0=pO[ob][:], in1=rinv[ob][:], op=ALU.mult),
                          r=[("pO", ob), ("rinv", ob)], w=[("yab", ob)])
                        DMA(lambda e, ob=ob, h=h, tb=tb: e.dma_start(out=yaT[h, :, tb * 512:(tb + 1) * 512], in_=yab[ob][:]),
                            r=[("yab", ob)], w=[("yaT", h, tb)], key=("yabo", ob))

                for p in range(npairs + LA):
                    if p < npairs:
                        s_stage(p)
                    if p - LA >= 0:
                        pv_stage(p - LA)
                    if h + 1 < P4H and p in (4, 8, 12, 16, 20):
                        p4_corr(h + 1, (p - 4) // 4)
        P.barrier()
        if stop_after == "p4":
            P.emit()
            return nc

        with ExitStack() as st:
            UmS = sb(st, "UmS", [128, 128], F32)
            LsS = sb(st, "LsS", [128, 128], F32)
            ggS = sb(st, "ggS", [128, 1], F32)
            glS = sb(st, "glS", [128, 16, 32], F32)
            gcol = sb(st, "gcol", [128, 16, NH], F32)
            glast = sb(st, "glast", [128, 16, NH], F32)
            bgS = sb(st, "bgS", [128, 16, NH], F32)
            ekd = sb(st, "ekd", [128, 16, NH], F32)
            egl = sb(st, "egl", [128, 16, NH], F32)
            kTh = [sb(st, "kTh%d" % i, [128, S], F32) for i in range(2)]
            qTh = [sb(st, "qTh%d" % i, [128, OWN], F32) for i in range(2)]
            kth = [sb(st, "kth%d" % i, [128, 16, 128], F32) for i in range(2)]
            vth = [sb(st, "vth%d" % i, [128, 16, 128], F32) for i in range(2)]
            zsS = [sb(st, "zsS%d" % i, [128, OWN], F32) for i in range(2)]
            uS = [sb(st, "uS%d" % i, [128, 16, 128], F32) for i in range(2)]
            wTS = [sb(st, "wTS%d" % i, [128, 16, 128], F32) for i in range(2)]
            kdS = [sb(st, "kdS%d" % i, [128, 16, 128], F32) for i in range(2)]
            qdS = [sb(st, "qdS%d" % i, [128, 8, 128], F32) for i in range(2)]
            qkS = [sb(st, "qkS%d" % i, [128, 8, 128], F32) for i in range(2)]
            Sst = [sb(st, "Sst%d" % i, [128, 128], F32) for i in range(2)]
            ybst = [sb(st, "ybst%d" % i, [128, OWN], BF16) for i in range(2)]
            ssq = sb(st, "ssq5", [128, 8], F32)
            rs5 = sb(st, "rs5", [128, 8], F32)
            junk5 = sb(st, "junk5", [128, 128], F32)
            on5 = [sb(st, "on5%d" % i, [128, 128], F32) for i in range(2)]
            vn5 = [sb(st, "vn5%d" % i, [128, 128], F32) for i in range(2)]
            TN = ("Ug", "dA", "dec", "dB", "decT", "egb", "N", "M0", "M1", "N0", "N1", "P", "Q", "vb", "kbg")
            NCH = 6
            tmp = {nm: [sb(st, "t5%s%d" % (nm, i), [128, 128], F32) for i in range(NCH)] for nm in TN}
            pbk = [ps(st, "p5b%d" % i, [128, 4, 128]) for i in range(7)]
            def pslot(pb, k):
                return pbk[pb][:, k, :]
            PSN = {"Grow": 0, "KK": 1, "NT": 2, "M2": 0, "N2": 1, "pP": 2, "pQ": 3, "pu": 0, "pw": 1, "pqk": 2}

            DMA(lambda e: e.dma_start(out=UmS[:], in_=um_c), w=["UmS"])
            DMA(lambda e: e.dma_start(out=LsS[:], in_=ls_c), w=["LsS"])
            DMA(lambda e: e.dma_start(out=ggS[:], in_=gg_col), w=["ggS"])
            DMA(lambda e: e.dma_start(out=glS[:], in_=glb.rearrange("(i p) d -> p i d", p=128)), w=["glS"])
            for i in range(16):
                pg = pbk[i % 2][:, 0, 0:NH]
                pl = pbk[2 + i % 2][:, 0, 0:NH]
                T(lambda e, pg=pg, i=i: e.matmul(pg, lhsT=UmS[:], rhs=glS[:, i, 0:NH], start=True, stop=True), r=["UmS", "glS"], w=[("bank", i % 2)])
                T(lambda e, pl=pl, i=i: e.matmul(pl, lhsT=onesF[:], rhs=glS[:, i, 0:NH], start=True, stop=True), r=["onesF", "glS"], w=[("bank", 2 + i % 2)])
                A(lambda e, pg=pg, i=i: e.activation(out=gcol[:, i, :], in_=pg, func=AF.Copy), w=["gcol", ("bank", i % 2)])
                A(lambda e, pl=pl, i=i: e.activation(out=glast[:, i, :], in_=pl, func=AF.Copy), w=["glast", ("bank", 2 + i % 2)])
            A(lambda e: e.activation(out=bgS[:], in_=gcol[:], func=AF.Exp), r=["gcol"], w=["bgS"])
            V(lambda e: e.tensor_tensor(out=bgS[:], in0=bgS[:], in1=glS[:, :, NH:2 * NH], op=ALU.mult), r=["bgS", "glS"], w=["bgS"])
            V(lambda e: e.tensor_tensor(out=ekd[:], in0=glast[:], in1=gcol[:], op=ALU.subtract), r=["glast", "gcol"], w=["ekd"])
            A(lambda e: e.activation(out=ekd[:], in_=ekd[:], func=AF.Exp), r=["ekd"], w=["ekd"])
            A(lambda e: e.activation(out=egl[:], in_=glast[:], func=AF.Exp), r=["glast"], w=["egl"])

            def prep(h, i, pb):
                hb = h % 2
                own = i >= 8
                io = i - 8
                t = {nm: tmp[nm][pb] for nm in TN}
                K_ = lambda nm: ("t5", nm, pb)
                PK = lambda nm: ("bank", pb)
                psl = {nm: pslot(pb, k) for nm, k in PSN.items()}
                glc = glS[:, i, h:h + 1]
                btc = glS[:, i, NH + h:NH + h + 1]
                gcc = gcol[:, i, h:h + 1]
                kTc = kTh[hb][:, i * 128:(i + 1) * 128]
                V(lambda e: e.tensor_scalar(out=t["Ug"][:], in0=UmS[:], scalar1=glc, scalar2=None, op0=ALU.mult), r=["UmS", "glS"], w=[K_("Ug")])
                T(lambda e: e.matmul(psl["Grow"], lhsT=onesF[:], rhs=t["Ug"][:], start=True, stop=True), r=[K_("Ug"), "onesF"], w=[PK("Grow")])
                V(lambda e: e.tensor_scalar(out=t["dA"][:], in0=psl["Grow"], scalar1=gcc, scalar2=0.0, op0=ALU.subtract, op1=ALU.max),
                  r=[PK("Grow"), "gcol"], w=[K_("dA")])
                A(lambda e: e.activation(out=t["dec"][:], in_=t["dA"][:], func=AF.Exp, scale=-1.0), r=[K_("dA")], w=[K_("dec")])
                G(lambda e: e.tensor_tensor(out=t["dec"][:], in0=t["dec"][:], in1=LsS[:], op=ALU.mult), r=[K_("dec"), "LsS"], w=[K_("dec")])
                if own:
                    V(lambda e: e.tensor_scalar(out=t["dB"][:], in0=psl["Grow"], scalar1=gcc, scalar2=0.0, op0=ALU.subtract, op1=ALU.min),
                      r=[PK("Grow"), "gcol"], w=[K_("dB")])
                    A(lambda e: e.activation(out=t["decT"][:], in_=t["dB"][:], func=AF.Exp), r=[K_("dB")], w=[K_("decT")])
                    G(lambda e: e.tensor_tensor(out=t["decT"][:], in0=t["decT"][:], in1=UmS[:], op=ALU.mult), r=[K_("decT"), "UmS"], w=[K_("decT")])
                    A(lambda e: e.activation(out=t["egb"][:], in_=psl["Grow"], func=AF.Exp), r=[PK("Grow")], w=[K_("egb")])
                T(lambda e: e.matmul(psl["KK"], lhsT=kTc, rhs=kTc, start=True, stop=True), r=[("kTh", hb)], w=[PK("KK")])
                V(lambda e: e.scalar_tensor_tensor(out=t["N"][:], in0=psl["KK"], scalar=btc, in1=t["dec"][:], op0=ALU.mult, op1=ALU.mult),
                  r=[PK("KK"), K_("dec"), "glS"], w=[K_("N")])
                T(lambda e: e.transpose(out=psl["NT"], in_=t["N"][:], identity=identF[:]), r=[K_("N"), "identF"], w=[PK("NT")])
                A(lambda e: e.activation(out=t["M0"][:], in_=psl["NT"], func=AF.Copy), r=[PK("NT")], w=[K_("M0")])
                G(lambda e: e.tensor_copy(out=t["N0"][:], in_=t["N"][:]), r=[K_("N")], w=[K_("N0")])
                V(lambda e: e.tensor_tensor(out=t["P"][:], in0=identF[:], in1=t["M0"][:], op=ALU.subtract), r=["identF", K_("M0")], w=[K_("P")])
                G(lambda e: e.tensor_tensor(out=t["Q"][:], in0=identF[:], in1=t["N"][:], op=ALU.subtract), r=["identF", K_("N")], w=[K_("Q")])
                cur = 0
                for lv in range(1, 7):
                    last = lv == 6
                    Mc, Nc = "M%d" % cur, "N%d" % cur
                    Mn, Nn = "M%d" % (1 - cur), "N%d" % (1 - cur)
                    T(lambda e, Mc=Mc, Nc=Nc: e.matmul(psl["M2"], lhsT=t[Nc][:], rhs=t[Mc][:], start=True, stop=True),
                      r=[K_(Mc), K_(Nc)], w=[PK("M2")])
                    A(lambda e, Mn=Mn: e.activation(out=t[Mn][:], in_=psl["M2"], func=AF.Copy), r=[PK("M2")], w=[K_(Mn)])
                    if not last:
                        T(lambda e, Mc=Mc, Nc=Nc: e.matmul(psl["N2"], lhsT=t[Mc][:], rhs=t[Nc][:], start=True, stop=True),
                          r=[K_(Mc), K_(Nc)], w=[PK("N2")])
                        A(lambda e, Nn=Nn: e.activation(out=t[Nn][:], in_=psl["N2"], func=AF.Copy), r=[PK("N2")], w=[K_(Nn)])
                    T(lambda e, Mn=Mn: e.matmul(psl["pP"], lhsT=t["Q"][:], rhs=t[Mn][:], start=True, stop=True), r=[K_("Q"), K_(Mn)], w=[PK("pP")])
                    if not last:
                        T(lambda e, Nn=Nn: e.matmul(psl["pQ"], lhsT=t["P"][:], rhs=t[Nn][:], start=True, stop=True), r=[K_("P"), K_(Nn)], w=[PK("pQ")])
                    V(lambda e: e.tensor_tensor(out=t["P"][:], in0=t["P"][:], in1=psl["pP"], op=ALU.add), r=[K_("P"), PK("pP")], w=[K_("P")])
                    if not last:
                        V(lambda e: e.tensor_tensor(out=t["Q"][:], in0=t["Q"][:], in1=psl["pQ"], op=ALU.add), r=[K_("Q"), PK("pQ")], w=[K_("Q")])
                    cur = 1 - cur
                G(lambda e: e.tensor_scalar(out=t["vb"][:], in0=vth[hb][:, i, :], scalar1=btc, scalar2=None, op0=ALU.mult),
                  r=[("vth", hb), "glS"], w=[K_("vb")])
                T(lambda e: e.matmul(psl["pu"], lhsT=t["P"][:], rhs=t["vb"][:], start=True, stop=True), r=[K_("P"), K_("vb")], w=[PK("pu")])
                A(lambda e: e.activation(out=uS[hb][:, i, :], in_=psl["pu"], func=AF.Copy), r=[PK("pu")], w=[("uS", hb, i)])
                G(lambda e: e.tensor_scalar(out=t["kbg"][:], in0=kth[hb][:, i, :], scalar1=bgS[:, i, h:h + 1], scalar2=None, op0=ALU.mult),
                  r=[("kth", hb), "bgS"], w=[K_("kbg")])
                T(lambda e: e.matmul(psl["pw"], lhsT=t["kbg"][:], rhs=t["P"][:], start=True, stop=True), r=[K_("P"), K_("kbg")], w=[PK("pw")])
                A(lambda e: e.activation(out=wTS[hb][:, i, :], in_=psl["pw"], func=AF.Copy), r=[PK("pw")], w=[("wTS", hb, i)])
                G(lambda e: e.tensor_scalar(out=kdS[hb][:, i, :], in0=kth[hb][:, i, :], scalar1=ekd[:, i, h:h + 1], scalar2=None, op0=ALU.mult),
                  r=[("kth", hb), "ekd"], w=[("kdS", hb, i)])
                if own:
                    qTc = qTh[hb][:, io * 128:(io + 1) * 128]
                    T(lambda e: e.matmul(psl["pqk"], lhsT=kTc, rhs=qTc, start=True, stop=True), r=[("kTh", hb), ("qTh", hb)], w=[PK("pqk")])
                    V(lambda e: e.tensor_tensor(out=qkS[hb][:, io, :], in0=psl["pqk"], in1=t["decT"][:], op=ALU.mult),
                      r=[PK("pqk"), K_("decT")], w=[("qkS", hb, io)])
                    G(lambda e: e.tensor_tensor(out=qdS[hb][:, io, :], in0=qTc, in1=t["egb"][:], op=ALU.mult),
                      r=[("qTh", hb), K_("egb")], w=[("qdS", hb, io)])

            pW, pSs, pOo, pTr = (pbk[6][:, k, :] for k in range(4))

            def step(h, i):
                hb = h % 2
                own = i >= 8
                io = i - 8
                vb_ = i % 2
                T(lambda e: e.matmul(pW, lhsT=wTS[hb][:, i, :], rhs=Sst[hb][:], start=True, stop=True), r=[("wTS", hb, i), ("Sst", hb)], w=[("bank", 6)])
                V(lambda e: e.tensor_tensor(out=vn5[vb_][:], in0=uS[hb][:, i, :], in1=pW, op=ALU.subtract), r=[("uS", hb, i), ("bank", 6)], w=[("vn5", vb_)])
                if own:
                    T(lambda e: e.matmul(pOo, lhsT=qdS[hb][:, io, :], rhs=Sst[hb][:], start=True, stop=False), r=[("qdS", hb, io), ("Sst", hb)], w=[("bank", 6)])
                    T(lambda e: e.matmul(pOo, lhsT=qkS[hb][:, io, :], rhs=vn5[vb_][:], start=False, stop=True), r=[("qkS", hb, io), ("vn5", vb_)], w=[("bank", 6)])
                T(lambda e: e.matmul(pSs, lhsT=kdS[hb][:, i, :], rhs=vn5[vb_][:], start=True, stop=True), r=[("kdS", hb, i), ("vn5", vb_)], w=[("bank", 6)])
                V(lambda e: e.scalar_tensor_tensor(out=Sst[hb][:], in0=Sst[hb][:], scalar=egl[:, i, h:h + 1], in1=pSs, op0=ALU.mult, op1=ALU.add),
                  r=[("Sst", hb), "egl", ("bank", 6)], w=[("Sst", hb)])
                if own:
                    ob_ = io % 2
                    A(lambda e: e.activation(out=junk5[:], in_=pOo, func=AF.Square, accum_out=ssq[:, io:io + 1]), r=[("bank", 6), "ssq"], w=["junk5", ("ssq", io)])
                    V(lambda e: e.tensor_scalar(out=rs5[:, io:io + 1], in0=ssq[:, io:io + 1], scalar1=1.0 / 128.0, scalar2=EPS, op0=ALU.mult, op1=ALU.add),
                      r=[("ssq", io)], w=[("rs5", io)])
                    A(lambda e: e.activation(out=rs5[:, io:io + 1], in_=rs5[:, io:io + 1], func=AF.Sqrt), r=[("rs5", io)], w=[("rs5", io)])
                    V(lambda e: e.reciprocal(out=rs5[:, io:io + 1], in_=rs5[:, io:io + 1]), r=[("rs5", io)], w=[("rs5", io)])
                    A(lambda e: e.activation(out=on5[ob_][:], in_=pOo, func=AF.Copy, scale=rs5[:, io:io + 1]), r=[("bank", 6), ("rs5", io)], w=[("on5", ob_)])
                    T(lambda e: e.transpose(out=pTr, in_=on5[ob_][:], identity=identF[:]), r=[("on5", ob_), "identF"], w=[("bank", 6)])
                    V(lambda e: e.scalar_tensor_tensor(out=ybst[hb][:, io * 128:(io + 1) * 128], in0=pTr, scalar=ggS[:, 0:1],
                                                       in1=zsS[hb][:, io * 128:(io + 1) * 128], op0=ALU.mult, op1=ALU.mult),
                      r=[("bank", 6), "ggS", ("zsS", hb)], w=[("ybst", hb)])
                    if i == 15:
                        DMA(lambda e: e.dma_start(out=ybT[h], in_=ybst[hb][:]), r=[("ybst", hb)], w=[("ybT", h)], key=("ybo", hb))

            def loads(h):
                hb = h % 2
                DMA(lambda e: e.dma_start(out=kTh[hb][:], in_=bkT[h]), w=[("kTh", hb)])
                DMA(lambda e: e.dma_start(out=qTh[hb][:], in_=bqT[h][:, OWN:S]), w=[("qTh", hb)])
                DMA(lambda e: e.dma_start(out=kth[hb][:], in_=bk[h]), w=[("kth", hb)])
                DMA(lambda e: e.dma_start(out=vth[hb][:], in_=bv[h]), w=[("vth", hb)])
                DMA(lambda e: e.dma_start(out=zsS[hb][:], in_=zsT[h]), w=[("zsS", hb)])

            import os
            from collections import deque
            P5H = int(os.environ.get("P5H", NH))
            chunks = [(h, i) for h in range(P5H) for i in range(16)]
            steps = deque(chunks)
            active = {}
            prep_done = set()
            step_emitted = set()
            nxt_chunk = 0
            cur_step = None
            while nxt_chunk < len(chunks) or active or steps or cur_step:
                for slot in range(NCH):
                    if slot in active or nxt_chunk >= len(chunks):
                        continue
                    h, i = chunks[nxt_chunk]
                    if h >= 2 and (h - 2, i) not in step_emitted:
                        continue
                    if i == 0:
                        loads(h)
                    REC[0] = []
                    prep(h, i, slot)
                    active[slot] = (deque(REC[0]), (h, i))
                    REC[0] = None
                    nxt_chunk += 1
                for slot in sorted(active):
                    ops_, hi_ = active[slot]
                    emit_rec(ops_.popleft())
                    if not ops_:
                        prep_done.add(hi_)
                        del active[slot]
                if cur_step is None and steps and steps[0] in prep_done:
                    h, i = steps.popleft()
                    if i == 0:
                        hb1 = h % 2
                        V(lambda e, hb1=hb1: e.memset(Sst[hb1][:], 0.0), w=[("Sst", hb1)])
                        V(lambda e: e.memset(ssq[:], 0.0), w=["ssq"] + [("ssq", k) for k in range(8)])
                    REC[0] = []
                    step(h, i)
                    cur_step = (deque(REC[0]), (h, i))
                    REC[0] = None
                if cur_step is not None:
                    for _ in range(2):
                        if cur_step[0]:
                            emit_rec(cur_step[0].popleft())
                    if not cur_step[0]:
                        step_emitted.add(cur_step[1])
                        cur_step = None
        P.barrier()
        if stop_after == "p5":
            P.emit()
            return nc

        with ExitStack() as stX:
            ssq2 = sb(stX, "ssq2", [128, 8, 16], F32)
            with ExitStack() as st:
                mgT = sb(st, "mgT", [128, KC, OWN], BF16)
                with ExitStack() as st6a:
                    yaS = sb(st6a, "yaS", [128, NH, OWN], BF16)
                    ybS = sb(st6a, "ybS", [128, NH, OWN], BF16)
                    wa = [sb(st6a, "wa%d" % i, [128, NH, 256], BF16) for i in range(2)]
                    wb_ = [sb(st6a, "wb%d" % i, [128, NH, 256], BF16) for i in range(2)]
                    gaS = [sb(st6a, "gaS%d" % i, [128, OWN], BF16) for i in range(2)]
                    gbS = [sb(st6a, "gbS%d" % i, [128, OWN], BF16) for i in range(2)]
                    t1 = [sb(st6a, "t1%d" % i, [128, 512], F32) for i in range(2)]
                    t2 = [sb(st6a, "t2%d" % i, [128, 512], F32) for i in range(2)]
                    pMa = [ps(st6a, "pMa%d" % i, [128, 2, 512]) for i in range(2)]
                    pMb = [ps(st6a, "pMb%d" % i, [128, 2, 512]) for i in range(2)]
                    DMA(lambda e: e.dma_start(out=yaS[:], in_=yaT.rearrange("h p t -> p h t")), w=["yaS"])
                    DMA(lambda e: e.dma_start(out=ybS[:], in_=ybT.rearrange("h p t -> p h t")), w=["ybS"])
                    wa_v = w_bra.rearrange("(c p) n -> p c n", p=128)
                    wb_v = w_brb.rearrange("(c p) n -> p c n", p=128)
                    k6 = 0
                    for ti in range(16):
                        wbuf = ti % 2
                        for q in range(2):
                            DMA(lambda e, wbuf=wbuf, ti=ti, q=q: e.dma_start(out=wa[wbuf][:, q * 8:(q + 1) * 8, :],
                                                                            in_=wa_v[:, q * 8:(q + 1) * 8, ti * 256:(ti + 1) * 256]),
                                w=[("wa", wbuf, q)], eng="gpsimd")
                            DMA(lambda e, wbuf=wbuf, ti=ti, q=q: e.dma_start(out=wb_[wbuf][:, q * 8:(q + 1) * 8, :],
                                                                            in_=wb_v[:, q * 8:(q + 1) * 8, ti * 256:(ti + 1) * 256]),
                                w=[("wb", wbuf, q)], eng="gpsimd")
                        for sub in range(2):
                            cb = ti * 2 + sub
                            s6 = cb % 2
                            DMA(lambda e, s6=s6, cb=cb: e.dma_start(out=gaS[s6][:], in_=gsa[cb]), w=[("gaS", s6)])
                            DMA(lambda e, s6=s6, cb=cb: e.dma_start(out=gbS[s6][:], in_=gsb[cb]), w=[("gbS", s6)])
                            for (pM, wt_, yS, wk_, yk) in ((pMa, wa, yaS, "wa", "yaS"), (pMb, wb_, ybS, "wb", "ybS")):
                                for c in range(NH):
                                    for tb in range(2):
                                        T(lambda e, pM=pM, wt_=wt_, yS=yS, s6=s6, wbuf=wbuf, c=c, tb=tb, sub=sub: e.matmul(
                                            pM[s6][:, tb, :], lhsT=wt_[wbuf][:, c, sub * 128:(sub + 1) * 128], rhs=yS[:, c, tb * 512:(tb + 1) * 512],
                                            start=(c == 0), stop=(c == NH - 1)),
                                          r=[(wk_, wbuf, 0), (wk_, wbuf, 1), yk], w=[(wk_ + "p", s6, tb)])
                            for tb in range(2):
                                k6 = 1 - k6
                                V(lambda e, s6=s6, tb=tb, k6=k6: e.tensor_tensor(out=t1[k6][:], in0=pMa[s6][:, tb, :], in1=gaS[s6][:, tb * 512:(tb + 1) * 512],
                                                                              op=ALU.mult), r=[("wap", s6, tb), ("gaS", s6)], w=[("t1", k6)])
                                V(lambda e, s6=s6, tb=tb, k6=k6: e.tensor_tensor(out=t2[k6][:], in0=pMb[s6][:, tb, :], in1=gbS[s6][:, tb * 512:(tb + 1) * 512],
                                                                              op=ALU.mult), r=[("wbp", s6, tb), ("gbS", s6)], w=[("t2", k6)])
                                G(lambda e, cb=cb, tb=tb, k6=k6: e.tensor_tensor(out=mgT[:, cb, tb * 512:(tb + 1) * 512], in0=t1[k6][:], in1=t2[k6][:], op=ALU.add),
                                  r=[("t1", k6), ("t2", k6)], w=[("mgT", cb)])
                P.barrier()
                with ExitStack() as st6b:
                    wo = [sb(st6b, "wo%d" % i, [128, KC, 256], BF16) for i in range(2)]
                    xr = [sb(st6b, "xr%d" % i, [128, 256], F32) for i in range(4)]
                    x1t = [sb(st6b, "x1t%d" % i, [128, 256], F32) for i in range(4)]
                    junk6 = sb(st6b, "junk6", [128, 256], F32)
                    pX = [ps(st6b, "pX%d" % i, [128, 512]) for i in range(4)]
                    V(lambda e: e.memset(ssq2[:], 0.0), w=["ssq2"])
                    wo_v = w_o.rearrange("(c p) n -> p c n", p=128)
                    for ct in range(16):
                        wbuf = ct % 2
                        for q in range(4):
                            DMA(lambda e, wbuf=wbuf, ct=ct, q=q: e.dma_start(out=wo[wbuf][:, q * 8:(q + 1) * 8, :],
                                                                            in_=wo_v[:, q * 8:(q + 1) * 8, ct * 256:(ct + 1) * 256]),
                                w=[("wo", wbuf, q)], eng="gpsimd")
                        for tt in range(8):
                            k4 = (ct * 8 + tt) % 4
                            DMA(lambda e, k4=k4, tt=tt, ct=ct: e.dma_start(out=xr[k4][:], in_=xs[OWN + tt * 128:OWN + (tt + 1) * 128, ct * 256:(ct + 1) * 256]),
                                w=[("xr", k4)])
                            for c in range(KC):
                                T(lambda e, k4=k4, c=c, tt=tt, wbuf=wbuf: e.matmul(pX[k4][:, 0:256], lhsT=mgT[:, c, tt * 128:(tt + 1) * 128], rhs=wo[wbuf][:, c, :],
                                                                                start=(c == 0), stop=(c == KC - 1)),
                                  r=[("wo", wbuf, c // 8)], w=[("pX", k4)])
                            V(lambda e, k4=k4: e.tensor_tensor(out=x1t[k4][:], in0=pX[k4][:, 0:256], in1=xr[k4][:], op=ALU.add),
                              r=[("pX", k4), ("xr", k4)], w=[("x1t", k4)])
                            A(lambda e, k4=k4, tt=tt, ct=ct: e.activation(out=junk6[:], in_=x1t[k4][:], func=AF.Square, accum_out=ssq2[:, tt, ct:ct + 1]),
                              r=[("x1t", k4), "ssq2"], w=["junk6", ("ssq2", tt, ct)])
                            DMA(lambda e, k4=k4, tt=tt, ct=ct: e.dma_start(out=x1[tt * 128:(tt + 1) * 128, ct * 256:(ct + 1) * 256], in_=x1t[k4][:]),
                                r=[("x1t", k4)], w=[("x1", tt, ct)], key=("x1o", k4))
                P.barrier()
            s12 = sb(stX, "s12", [128, 8, 16, 128], F32)
            stA = ExitStack()
            xn2T = sb(stA, "xn2T", [128, KC, OWN], BF16)
            with ExitStack() as st1:
                xf6 = [sb(st1, "xf6%d" % i, [128, D], F32) for i in range(2)]
                xb6 = [sb(st1, "xb6%d" % i, [128, D], BF16) for i in range(2)]
                g2s = sb(st1, "g2s", [128, KC], F32)
                ss6v = sb(st1, "ss6", [128, 8], F32)
                pt6 = [ps(st1, "pt6%d" % i, [128, 8, 128], BF16) for i in range(2)]
                DMA(lambda e: e.dma_start(out=g2s[:], in_=g2T), w=["g2s"])
                V(lambda e: e.reduce_sum(out=ss6v[:], in_=ssq2[:], axis=AX.X), w=["ss6"])
                V(lambda e: e.tensor_scalar(out=ss6v[:], in0=ss6v[:], scalar1=1.0 / D, scalar2=EPS, op0=ALU.mult, op1=ALU.add), r=["ss6"], w=["ss6"])
                A(lambda e: e.activation(out=ss6v[:], in_=ss6v[:], func=AF.Sqrt), r=["ss6"], w=["ss6"])
                V(lambda e: e.reciprocal(out=ss6v[:], in_=ss6v[:]), r=["ss6"], w=["ss6"])
                for i in range(8):
                    b = i % 2
                    DMA(lambda e, i=i, b=b: e.dma_start(out=xf6[b][:], in_=x1[i * 128:(i + 1) * 128, :]), w=[("xf6", b)])
                    A(lambda e, i=i, b=b: e.activation(out=xb6[b][:], in_=xf6[b][:], func=AF.Copy, scale=ss6v[:, i:i + 1]),
                      r=[("xf6", b), "ss6"], w=[("xb6", b)])
                    for g4 in range(4):
                        pb = g4 % 2
                        for c in range(8):
                            T(lambda e, b=b, g4=g4, c=c, pb=pb: e.transpose(out=pt6[pb][:, c, :], in_=xb6[b][:, (g4 * 8 + c) * 128:(g4 * 8 + c + 1) * 128],
                                                                         identity=identB[:]), r=[("xb6", b), "identB"], w=[("pt6", pb)])
                        V(lambda e, i=i, g4=g4, pb=pb: e.tensor_tensor(out=xn2T[:, g4 * 8:(g4 + 1) * 8, i * 128:(i + 1) * 128], in0=pt6[pb][:],
                                                                     in1=bc(g2s[:, g4 * 8:(g4 + 1) * 8].unsqueeze(2), [128, 8, 128]), op=ALU.mult),
                          r=[("pt6", pb), "g2s"], w=[("xn2T", i)])
            DMA(lambda e: e.dma_start(out=xn2D, in_=xn2T[:]), r=[("xn2T", i) for i in range(8)], w=["xn2D"])
            P.barrier()
            if stop_after == "p6":
                P.emit()
                return nc

            with ExitStack() as st7:
                with ExitStack() as st:
                    wq = [sb(st, "wq%d" % i, [128, KC, 256], BF16) for i in range(2)]
                    qpS = [sb(st, "qpS%d" % i, [128, OWN], F32) for i in range(2)]
                    kraw = sb(st, "kraw", [128, 2, 128], F32)
                    kT2 = sb(st, "kT2", [128, 2, 128], F32)
                    pQ = [ps(st, "pQ%d" % i, [128, 2, 512]) for i in range(2)]
                    pSc = [ps(st, "pSc%d" % i, [128, 512]) for i in range(2)]
                    pK = ps(st, "pK7", [128, 512])
                    DMA(lambda e: e.dma_start(out=kraw[:, 0, :], in_=pk1), w=[("kraw", 0)])
                    DMA(lambda e: e.dma_start(out=kraw[:, 1, :], in_=pk2), w=[("kraw", 1)])
                    for j in range(2):
                        T(lambda e, j=j: e.transpose(out=pK[:, j * 128:(j + 1) * 128], in_=kraw[:, j, :], identity=identF[:]),
                          r=[("kraw", j), "identF"], w=["pK"])
                    A(lambda e: e.activation(out=kT2[:].rearrange("p a b -> p (a b)"), in_=pK[:, 0:256], func=AF.Copy), r=["pK"], w=["kT2"])
                    wq_v = p_wq.rearrange("(c p) n -> p c n", p=128)
                    for ti in range(8):
                        wbuf = ti % 2
                        for q in range(4):
                            DMA(lambda e, wbuf=wbuf, ti=ti, q=q: e.dma_start(out=wq[wbuf][:, q * 8:(q + 1) * 8, :],
                                                                            in_=wq_v[:, q * 8:(q + 1) * 8, ti * 256:(ti + 1) * 256]),
                                w=[("wq", wbuf, q)], eng="gpsimd")
                        for sub in range(2):
                            blk = ti * 2 + sub
                            s7 = blk % 2
                            for c in range(KC):
                                for tb in range(2):
                                    T(lambda e, s7=s7, wbuf=wbuf, c=c, tb=tb, sub=sub: e.matmul(
                                        pQ[s7][:, tb, :], lhsT=wq[wbuf][:, c, sub * 128:(sub + 1) * 128], rhs=xn2T[:, c, tb * 512:(tb + 1) * 512],
                                        start=(c == 0), stop=(c == KC - 1)), r=[("wq", wbuf, c // 8)], w=[("pQ", s7)])
                            A(lambda e, s7=s7: e.activation(out=qpS[s7][:], in_=pQ[s7][:].rearrange("p a b -> p (a b)"), func=AF.Copy),
                              r=[("pQ", s7)], w=[("qpS", s7)])
                            for tt in range(8):
                                k2 = tt % 2
                                T(lambda e, k2=k2, s7=s7, tt=tt, blk=blk: e.matmul(pSc[k2][:, 0:128], lhsT=qpS[s7][:, tt * 128:(tt + 1) * 128],
                                                                                rhs=kT2[:, blk % 2, :], start=True, stop=True),
                                  r=[("qpS", s7), "kT2"], w=[("pSc", k2)])
                                V(lambda e, k2=k2, tt=tt, blk=blk: e.tensor_copy(out=s12[:, tt, blk, :], in_=pSc[k2][:, 0:128]),
                                  r=[("pSc", k2)], w=[("s12", tt, blk)])
                P.barrier()
                stA.close()
                with ExitStack() as st:
                    thr = sb(st, "thr7", [128, 8, 8], F32)
                    nb7 = sb(st, "nb7", [128, 8, 8], F32)
                    v12 = sb(st, "v12", [128, 2, 16], F32)
                    wk7 = sb(st, "wk7", [128, 128], F32)
                    cand = sb(st, "cand", [128, 16, 16], F32)
                    cwk = sb(st, "cwk", [128, 256], F32)
                    c24 = sb(st, "c24", [128, 24], F32)
                    z7 = sb(st, "z7", [128, 8, 8], F32)
                    j16 = sb(st, "j16", [128, 16], F32)
                    sum7 = [sb(st, "sum7%d" % i, [128, 8, 128], F32) for i in range(4)]
                    E7 = [sb(st, "E7%d" % i, [128, 8, 128], BF16) for i in range(4)]
                    Gh = [[sb(st, "Gh%d_%d" % (i, h), [128, 8, 128], BF16) for h in range(8)] for i in range(2)]
                    GTs = [sb(st, "GTs%d" % i, [128, 8, OWN], BF16) for i in range(2)]
                    pGn = [ps(st, "pGn%d" % i, [128, 2, 512]) for i in range(2)]
                    pGt = [ps(st, "pGt%d" % i, [128, 8, 128], BF16) for i in range(2)]
                    Gn = [sb(st, "Gn%d" % i, [128, 8, 128], BF16) for i in range(2)]
                    V(lambda e: e.memset(z7[:], 0.0), w=["z7"])
                    for tt in range(8):
                        for h in range(8):
                            for half in range(2):
                                sv = s12[:, tt, 2 * h + half, :]
                                V(lambda e, sv=sv, half=half: e.max(out=v12[:, half, 0:8], in_=sv), w=["v12"])
                                V(lambda e, sv=sv, half=half: e.match_replace(out=wk7[:], in_to_replace=v12[:, half, 0:8], in_values=sv, imm_value=-1.0e30),
                                  r=["v12"], w=["wk7"])
                                V(lambda e, half=half: e.max(out=v12[:, half, 8:16], in_=wk7[:]), r=["wk7"], w=["v12"])
                            V(lambda e: e.tensor_tensor(out=cand[:], in0=bc(v12[:, 0, :].unsqueeze(2), [128, 16, 16]),
                                                        in1=bc(v12[:, 1, :].unsqueeze(1), [128, 16, 16]), op=ALU.add), r=["v12"], w=["cand"])
                            cf = cand[:].rearrange("p a b -> p (a b)")
                            V(lambda e, cf=cf: e.max(out=c24[:, 0:8], in_=cf), r=["cand"], w=["c24"])
                            V(lambda e, cf=cf: e.match_replace(out=cwk[:], in_to_replace=c24[:, 0:8], in_values=cf, imm_value=-1.0e30), r=["cand", "c24"], w=["cwk"])
                            V(lambda e: e.max(out=c24[:, 8:16], in_=cwk[:]), r=["cwk"], w=["c24"])
                            V(lambda e: e.match_replace(out=cwk[:], in_to_replace=c24[:, 8:16], in_values=cwk[:], imm_value=-1.0e30), r=["cwk", "c24"], w=["cwk"])
                            V(lambda e: e.max(out=c24[:, 16:24], in_=cwk[:]), r=["cwk"], w=["c24"])
                            tc_ = thr[:, tt, h:h + 1]
                            nc_ = nb7[:, tt, h:h + 1]
                            zc_ = z7[:, tt, h:h + 1]
                            V(lambda e, tc_=tc_: e.tensor_tensor(out=tc_, in0=c24[:, 15:16], in1=c24[:, 16:17], op=ALU.add), r=["c24"], w=["thr"])
                            V(lambda e, tc_=tc_: e.tensor_scalar(out=tc_, in0=tc_, scalar1=0.5, scalar2=None, op0=ALU.mult), r=["thr"], w=["thr"])
                            V(lambda e, tc_=tc_, nc_=nc_: e.tensor_scalar(out=nc_, in0=tc_, scalar1=-1.0, scalar2=None, op0=ALU.mult), r=["thr"], w=["nb7"])
                            A(lambda e, nc_=nc_, zc_=zc_: e.activation(out=j16[:], in_=c24[:, 0:16], func=AF.Exp, bias=nc_, accum_out=zc_),
                              r=["c24", "nb7", "z7"], w=["j16", "z7"])
                            A(lambda e, zc_=zc_: e.activation(out=zc_, in_=zc_, func=AF.Ln), r=["z7"], w=["z7"])
                            V(lambda e, nc_=nc_, zc_=zc_: e.tensor_tensor(out=nc_, in0=nc_, in1=zc_, op=ALU.subtract), r=["nb7", "z7"], w=["nb7"])
                    def g_elem(ic, tt):
                        gs = tt % 2
                        for h in range(8):
                            k2 = h % 4
                            s1c = s12[:, tt, 2 * h, ic * 8:(ic + 1) * 8]
                            s2a = s12[:, tt, 2 * h + 1, :]
                            G(lambda e, k2=k2, s1c=s1c, s2a=s2a: e.tensor_tensor(out=sum7[k2][:], in0=bc(s1c.unsqueeze(2), [128, 8, 128]),
                                                                                 in1=bc(s2a.unsqueeze(1), [128, 8, 128]), op=ALU.add),
                              w=[("sum7", k2)])
                            A(lambda e, k2=k2, tt=tt, h=h: e.activation(out=E7[k2][:], in_=sum7[k2][:], func=AF.Exp, bias=nb7[:, tt, h:h + 1]),
                              r=[("sum7", k2), "nb7"], w=[("E7", k2)])
                            V(lambda e, k2=k2, gs=gs, h=h, tt=tt: e.scalar_tensor_tensor(out=Gh[gs][h][:], in0=sum7[k2][:], scalar=thr[:, tt, h:h + 1],
                                                                                         in1=E7[k2][:], op0=ALU.is_ge, op1=ALU.mult),
                              r=[("sum7", k2), ("E7", k2), "thr"], w=[("Gh", gs, h)])
                        for half in range(2):
                            for h in range(8):
                                T(lambda e, gs=gs, half=half, h=h: e.matmul(pGn[gs][:, half, :], lhsT=identB[:],
                                                                          rhs=Gh[gs][h][:, half * 4:(half + 1) * 4, :].rearrange("p a b -> p (a b)"),
                                                                          start=(h == 0), stop=(h == 7)),
                                  r=[("Gh", gs, h), "identB"], w=[("pGn", gs, half)])

                    def g_tail(ic, tt):
                        gs = tt % 2
                        gb_ = ic % 2
                        A(lambda e, gs=gs: e.activation(out=Gn[gs][:].rearrange("p a b -> p (a b)"), in_=pGn[gs][:].rearrange("p a b -> p (a b)"), func=AF.Copy),
                          r=[("pGn", gs, 0), ("pGn", gs, 1)], w=[("Gn", gs)])
                        for i1l in range(8):
                            T(lambda e, gs=gs, i1l=i1l: e.transpose(out=pGt[gs][:, i1l, :], in_=Gn[gs][:, i1l, :], identity=identB[:]),
                              r=[("Gn", gs), "identB"], w=[("pGt", gs)])
                        V(lambda e, gs=gs, tt=tt, gb_=gb_: e.tensor_copy(out=GTs[gb_][:, :, tt * 128:(tt + 1) * 128], in_=pGt[gs][:]),
                          r=[("pGt", gs)], w=[("GTs", gb_)])
                        if tt == 7:
                            DMA(lambda e, ic=ic, gb_=gb_: e.dma_start(out=GT[ic * 8:(ic + 1) * 8].rearrange("a p t -> p a t"), in_=GTs[gb_][:]),
                                r=[("GTs", gb_)], w=[("GT", ic)], key=("gto", gb_))

                    gtiles = [(ic, tt) for ic in range(16) for tt in range(8)]
                    for n_, (ic, tt) in enumerate(gtiles):
                        g_elem(ic, tt)
                        if n_ >= 1:
                            g_tail(*gtiles[n_ - 1])
                    g_tail(*gtiles[-1])
                P.barrier()
            with ExitStack() as st:
                xn2T2 = sb(st, "xn2T2", [128, KC, OWN], BF16)
                DMA(lambda e: e.dma_start(out=xn2T2[:], in_=xn2D), w=["xn2T2"])
                Ub = [sb(st, "Ub%d" % i, [128, D], BF16) for i in range(2)]
                UT = [sb(st, "UT%d" % i, [128, KC, 128], BF16) for i in range(2)]
                actT = [sb(st, "actT%d" % i, [128, OWN], BF16) for i in range(2)]
                GTt = [sb(st, "GTt%d" % i, [128, OWN], BF16) for i in range(2)]
                Gat = [sb(st, "Gat%d" % i, [128, OWN], BF16) for i in range(2)]
                pUT = [ps(st, "pUT%d" % i, [128, 8, 128], BF16) for i in range(2)]
                pA7 = [ps(st, "pA7%d" % i, [128, 2, 512]) for i in range(2)]
                import os
                NE = int(os.environ.get("P7E", 128))
                for i1 in range(NE):
                    b = i1 % 2
                    for q in range(2):
                        DMA(lambda e, b=b, i1=i1, q=q: e.dma_start(out=Ub[b][:, q * 2048:(q + 1) * 2048], in_=p_u[i1 * 128:(i1 + 1) * 128, q * 2048:(q + 1) * 2048]),
                            w=[("Ub", b, q)], eng="gpsimd")
                    DMA(lambda e, b=b, i1=i1: e.dma_start(out=GTt[b][:], in_=GT[i1]), w=[("GTt", b)])
                    for g4 in range(4):
                        pb = g4 % 2
                        for c in range(8):
                            T(lambda e, b=b, g4=g4, c=c, pb=pb: e.transpose(out=pUT[pb][:, c, :], in_=Ub[b][:, (g4 * 8 + c) * 128:(g4 * 8 + c + 1) * 128],
                                                                         identity=identB[:]), r=[("Ub", b, g4 // 2), "identB"], w=[("pUT", pb)])
                        V(lambda e, b=b, g4=g4, pb=pb: e.tensor_copy(out=UT[b][:, g4 * 8:(g4 + 1) * 8, :], in_=pUT[pb][:]), r=[("pUT", pb)], w=[("UT", b, g4)])
                    for c in range(KC):
                        for tb in range(2):
                            T(lambda e, b=b, c=c, tb=tb: e.matmul(pA7[b][:, tb, :], lhsT=UT[b][:, c, :], rhs=xn2T2[:, c, tb * 512:(tb + 1) * 512],
                                                               start=(c == 0), stop=(c == KC - 1)), r=[("UT", b, c // 8), "xn2T2"], w=[("pA7", b)])
                    A(lambda e, b=b: e.activation(out=actT[b][:], in_=pA7[b][:].rearrange("p a b -> p (a b)"), func=AF.Gelu), r=[("pA7", b)], w=[("actT", b)])
                    V(lambda e, b=b: e.tensor_tensor(out=Gat[b][:], in0=actT[b][:], in1=GTt[b][:], op=ALU.mult), r=[("actT", b), ("GTt", b)], w=[("Gat", b)])
                    DMA(lambda e, b=b, i1=i1: e.dma_start(out=GaT[i1], in_=Gat[b][:]), r=[("Gat", b)], w=[("GaT", i1)], key=("gato", b))
            P.barrier()
        with ExitStack() as st:
            Vb = [sb(st, "Vb%d" % i, [128, 4, 512], BF16) for i in range(3)]
            Gg = [sb(st, "Gg%d" % i, [128, 4, OWN], BF16) for i in range(3)]
            x1r = [sb(st, "x1r%d" % i, [128, 512], F32) for i in range(2)]
            ot = [sb(st, "ot%d" % i, [128, 512], F32) for i in range(2)]
            pO7 = [ps(st, "pO7%d" % i, [128, 512]) for i in range(8)]
            NG = NE // 4
            for db in range(8):
                for ig in range(NG):
                    b3 = (db * NG + ig) % 3
                    DMA(lambda e, b3=b3, ig=ig, db=db: e.dma_start(
                        out=Vb[b3][:], in_=p_v[ig * 512:(ig + 1) * 512, db * 512:(db + 1) * 512].rearrange("(q p) n -> p q n", p=128)),
                        w=[("Vb", b3)], eng="gpsimd")
                    DMA(lambda e, b3=b3, ig=ig: e.dma_start(out=Gg[b3][:], in_=GaT[ig * 4:(ig + 1) * 4].rearrange("q p t -> p q t")), w=[("Gg", b3)])
                    for q in range(4):
                        i1 = ig * 4 + q
                        for tt in range(8):
                            T(lambda e, b3=b3, q=q, tt=tt, i1=i1: e.matmul(pO7[tt][:], lhsT=Gg[b3][:, q, tt * 128:(tt + 1) * 128], rhs=Vb[b3][:, q, :],
                                                                        start=(i1 == 0), stop=(i1 == NE - 1)),
                              r=[("Vb", b3), ("Gg", b3)], w=[("pO7", tt)])
                for tt in range(8):
                    k2 = tt % 2
                    DMA(lambda e, k2=k2, tt=tt, db=db: e.dma_start(out=x1r[k2][:], in_=x1[tt * 128:(tt + 1) * 128, db * 512:(db + 1) * 512]), w=[("x1r", k2)])
                    V(lambda e, k2=k2, tt=tt: e.tensor_tensor(out=ot[k2][:], in0=pO7[tt][:], in1=x1r[k2][:], op=ALU.add),
                      r=[("pO7", tt), ("x1r", k2)], w=[("ot", k2)])
                    DMA(lambda e, k2=k2, tt=tt, db=db: e.dma_start(out=out[tt * 128:(tt + 1) * 128, db * 512:(db + 1) * 512], in_=ot[k2][:]),
                        r=[("ot", k2)], w=[("out", tt, db)], key=("oto", k2))
        P.barrier()

        P.emit()
    return nc


def prep_shared(inp):
    m = {}
    f = lambda k: np.asarray(inp[k], np.float32)
    m["w_in"] = np.ascontiguousarray(f("w_in")[0])
    m["g1T"] = np.ascontiguousarray(f("norm1_g")[0].reshape(KC, 128).T)
    cw = f("conv_w")[0]
    m["convT"] = np.ascontiguousarray(cw.reshape(4, 48, 128).transpose(2, 1, 0))
    m["qg_col"] = np.ascontiguousarray(f("q_norm_g")[0].reshape(128, 1))
    m["kg_col"] = np.ascontiguousarray(f("k_norm_g")[0].reshape(128, 1))
    m["alog_b"] = np.ascontiguousarray(np.broadcast_to(f("a_log")[0][None, :], (128, NH)))
    m["dtb_b"] = np.ascontiguousarray(np.broadcast_to(f("dt_bias")[0][None, :], (128, NH)))
    m["ident_f"] = np.eye(128, dtype=np.float32)
    m["ident_b"] = np.eye(128, dtype=np.float32).astype(ml_dtypes.bfloat16)
    rb = f("rel_bias")
    m["rel_b"] = np.ascontiguousarray(rb)
    m["oh_c"] = _t5_onehot()
    m["b31_b"] = np.ascontiguousarray(np.broadcast_to(rb[31][None, :], (128, NH)))
    m["J_c"] = np.ascontiguousarray(np.eye(128, dtype=np.float32)[::-1])
    m["um_c"] = np.ascontiguousarray(np.triu(np.ones((128, 128), np.float32)))
    m["ls_c"] = np.ascontiguousarray(np.tril(np.ones((128, 128), np.float32), -1))
    m["gg_col"] = np.ascontiguousarray(f("gdn_norm_g")[0].reshape(128, 1))
    m["w_bra"] = np.ascontiguousarray(f("w_br_a")[0])
    m["w_brb"] = np.ascontiguousarray(f("w_br_b")[0])
    m["w_o"] = np.ascontiguousarray(f("w_out")[0])
    m["g2T"] = np.ascontiguousarray(f("norm2_g")[0].reshape(KC, 128).T)
    m["p_wq"] = np.ascontiguousarray(f("peer_wq")[0])
    m["pk1"] = np.ascontiguousarray(f("peer_k1")[0])
    m["pk2"] = np.ascontiguousarray(f("peer_k2")[0])
    m["p_u"] = np.ascontiguousarray(f("peer_u")[0])
    m["p_v"] = np.ascontiguousarray(f("peer_v")[0])
    return m


def prep_core(inp, c, shared=None):
    b, g = c // 2, c % 2
    m = dict(shared if shared is not None else prep_shared(inp))
    x = np.asarray(inp["x"], np.float32)
    xs = np.zeros((S, D), np.float32)
    kb = np.zeros((128, S), np.float32)
    if g == 1:
        xs[:] = x[b]
    else:
        xs[OWN:] = x[b, :OWN]
        kb[:, :OWN] = -3.0e38
    m["xs"] = xs
    m["keybias"] = kb
    return m


def kernel(**inputs):
    nc = build()
    shared = prep_shared(inputs)
    in_maps = [prep_core(inputs, c, shared) for c in range(8)]
    res = run_bass_kernel_spmd(nc, in_maps, core_ids=list(range(8)))
    out = np.zeros((4, S, D), np.float32)
    for c in range(8):
        b, g = c // 2, c % 2
        out[b, g * OWN:(g + 1) * OWN] = np.asarray(res.results[c]["out"], np.float32)
    return out


def _t5_onehot():
    oh = np.zeros((32, 1280), np.float32)
    for d in range(128):
        n = d
        if n < 16:
            bk_ = n
        else:
            bk_ = 16 + int(np.float32(np.log(np.float32(n) / np.float32(16)) / np.float32(math.log(128 / 16)) * np.float32(16)))
            bk_ = min(bk_, 31)
        oh[bk_, d + 640] += 1.0
        oh[31, d + 640] -= 1.0
    return oh
```

```python
import math
import numpy as np
import ml_dtypes
from contextlib import ExitStack
import concourse.bass as bass
import concourse.mybir as mybir
from concourse.bass_utils import run_bass_kernel_spmd

F32 = mybir.dt.float32
BF16 = mybir.dt.bfloat16
AF = mybir.ActivationFunctionType
ALU = mybir.AluOpType
AX = mybir.AxisListType

ENGS = ("tensor", "vector", "scalar", "gpsimd", "sync")

D = 4096
KC = 32
S = 2048
OWN = 1024
NH = 16
EPS = 1e-6
C_AQ, C_AK, C_AV, C_IQ, C_IK, C_IW = 0, 2048, 4096, 6144, 7168, 7232
C_BQ, C_BK, C_BV, C_BA, C_BB, C_BZ = 7248, 9296, 11344, 13392, 13408, 13424
C_GA, C_GB = 15472, 19568
NCOL = 23664


class Prog:
    def __init__(self, nc, es):
        self.nc = nc
        self.es = es
        self.ops = []
        self.cnt = {e: 0 for e in ENGS}
        self.esem = {e: es.enter_context(nc.semaphore("s_" + e)) for e in ENGS}
        self.semobj = {id(s): s for s in self.esem.values()}
        self.known = {e: {} for e in ENGS}
        self.lastw = {}
        self.reads = {}
        self.dsem = {}
        self.free_dsems = []

    def _deps(self, eng, reads, writes):
        deps = []
        for r in reads:
            t = self.lastw.get(r)
            if t is not None:
                deps.append(t)
        for w in writes:
            t = self.lastw.get(w)
            if t is not None:
                deps.append(t)
            deps.extend(self.reads.get(w, ()))
        best = {}
        for (sid, val) in deps:
            if val > best.get(sid, 0):
                best[sid] = val
        waits = []
        kn = self.known[eng]
        for sid, val in best.items():
            if kn.get(sid, 0) >= val:
                continue
            kn[sid] = val
            waits.append((sid, val))
        return waits

    def _commit(self, tok, reads, writes):
        for r in reads:
            self.reads.setdefault(r, []).append(tok)
        for w in writes:
            self.lastw[w] = tok
            self.reads[w] = []

    def op(self, eng, fn, reads=(), writes=(), nosame=None):
        banks = [k for k in reads if isinstance(k, tuple) and k and k[0] == "bank"]
        if banks:
            reads = [k for k in reads if k not in banks]
            writes = list(writes) + [k for k in banks if k not in writes]
        waits = self._deps(eng, reads, writes)
        if nosame is None:
            nosame = (eng == "tensor")
        if nosame:
            waits = [w for w in waits if w[0] != id(self.esem[eng])]
        self.cnt[eng] += 1
        sem = self.esem[eng]
        tok = (id(sem), self.cnt[eng])
        self.ops.append((eng, fn, waits, (sem, 1)))
        self._commit(tok, reads, writes)
        return tok

    def dma(self, eng, fn, reads=(), writes=(), semkey=None):
        assert len(writes) >= 1
        key = semkey if semkey is not None else writes[0]
        if key not in self.dsem:
            if self.free_dsems:
                ent = self.free_dsems.pop()
            else:
                s = self.es.enter_context(self.nc.semaphore("d%d" % len(self.semobj)))
                self.semobj[id(s)] = s
                ent = [s, 0]
            self.dsem[key] = ent
        ent = self.dsem[key]
        waits = self._deps(eng, reads, writes)
        if ent[1] > 0:
            kn = self.known[eng]
            if kn.get(id(ent[0]), 0) < ent[1] * 16:
                kn[id(ent[0])] = ent[1] * 16
                waits.append((id(ent[0]), ent[1] * 16))
        ent[1] += 1
        tok = (id(ent[0]), ent[1] * 16)
        self.ops.append((eng, fn, waits, (ent[0], 16)))
        self._commit(tok, reads, writes)
        return tok

    def barrier(self):
        toks = []
        for e in ENGS:
            if self.cnt[e] > 0:
                toks.append((id(self.esem[e]), self.cnt[e]))
        for key, ent in self.dsem.items():
            if ent[1] > 0:
                toks.append((id(ent[0]), ent[1] * 16))
        for e in ENGS:
            waits = []
            kn = self.known[e]
            for (sid, val) in toks:
                if kn.get(sid, 0) < val:
                    kn[sid] = val
                    waits.append((sid, val))
            if waits:
                self.ops.append((e, None, waits, None))
        self.lastw = {}
        self.reads = {}
        for key, ent in self.dsem.items():
            self.free_dsems.append(ent)
        self.dsem = {}

    def emit(self):
        nc = self.nc
        with nc.Block() as block:
            def mk(ename):
                def body(e):
                    for (eng, fn, waits, inc) in self.ops:
                        if eng != ename:
                            continue
                        for (sid, val) in waits:
                            e.wait_ge(self.semobj[sid], val)
                        if fn is not None:
                            ins = fn(e)
                            ins.then_inc(inc[0], inc[1])
                return body
            block.sync(mk("sync"))
            block.tensor(mk("tensor"))
            block.vector(mk("vector"))
            block.scalar(mk("scalar"))
            block.gpsimd(mk("gpsimd"))


def bc(ap, shape):
    return ap.to_broadcast(list(shape))


def build(stop_after=None, dbg=()):
    nc = bass.Bass("TRN2", target_bir_lowering=False)
    dbg = set(dbg)

    def din(name, shape, dt=F32):
        return nc.dram_tensor(name, list(shape), dt, kind="ExternalInput").ap()

    def dscr(name, shape, dt=F32):
        kind = "ExternalOutput" if name in dbg else "Internal"
        return nc.dram_tensor(name, list(shape), dt, kind=kind).ap()

    xs = din("xs", [S, D])
    w_in = din("w_in", [D, NCOL])
    g1T = din("g1T", [128, KC])
    convT = din("convT", [128, 48, 4])
    qg_col = din("qg_col", [128, 1])
    kg_col = din("kg_col", [128, 1])
    alog_b = din("alog_b", [128, NH])
    dtb_b = din("dtb_b", [128, NH])
    ident_f = din("ident_f", [128, 128])
    ident_b = din("ident_b", [128, 128], BF16)
    keybias = din("keybias", [128, S])
    rel_b = din("rel_b", [32, NH])
    oh_c = din("oh_c", [32, 1280])
    b31_b = din("b31_b", [128, NH])
    J_c = din("J_c", [128, 128])
    um_c = din("um_c", [128, 128])
    ls_c = din("ls_c", [128, 128])
    gg_col = din("gg_col", [128, 1])
    w_bra = din("w_bra", [2048, D])
    w_brb = din("w_brb", [2048, D])
    w_o = din("w_o", [D, D])
    g2T = din("g2T", [128, KC])
    p_wq = din("p_wq", [D, 2048])
    pk1 = din("pk1", [128, 128])
    pk2 = din("pk2", [128, 128])
    p_u = din("p_u", [16384, D])
    p_v = din("p_v", [16384, D])
    out = nc.dram_tensor("out", [OWN, D], F32, kind="ExternalOutput").ap()

    akT = dscr("akT", [NH, 128, S], BF16)
    aqT = dscr("aqT", [NH, 128, OWN], BF16)
    av = dscr("av", [NH, 128, 16, 128], BF16)
    iqT = dscr("iqT", [8, 128, OWN], BF16)
    ikT2 = dscr("ikT2", [128, S], BF16)
    iw = dscr("iw", [OWN, NH])
    bqT = dscr("bqT", [NH, 128, S])
    bkT = dscr("bkT", [NH, 128, S])
    bk = dscr("bk", [NH, 128, 16, 128])
    bv = dscr("bv", [NH, 128, 16, 128])
    glb = dscr("glb", [S, 2 * NH])
    zsT = dscr("zsT", [NH, 128, OWN])
    gsa = dscr("gsa", [KC, 128, OWN], BF16)
    gsb = dscr("gsb", [KC, 128, OWN], BF16)
    mT = dscr("mT", [8, 128, 16, 128], BF16)
    fv = dscr("fv", [NH, 1280])
    yaT = dscr("yaT", [NH, 128, OWN], BF16)
    ybT = dscr("ybT", [NH, 128, OWN], BF16)
    x1 = dscr("x1", [OWN, D])
    xn2D = dscr("xn2D", [128, KC, OWN], BF16)
    GT = dscr("GT", [128, 128, OWN], BF16)
    GaT = dscr("GaT", [128, 128, OWN], BF16)

    with ExitStack() as es:
        P = Prog(nc, es)

        def sb(st, name, shape, dt):
            return st.enter_context(nc.sbuf_tensor(name, list(shape), dt))

        def ps(st, name, shape, dt=F32):
            return st.enter_context(nc.psum_tensor(name, list(shape), dt))

        identF = sb(es, "identF", [128, 128], F32)
        identB = sb(es, "identB", [128, 128], BF16)
        onesF = sb(es, "onesF", [128, 128], F32)
        P.dma("sync", lambda e: e.dma_start(out=identF[:], in_=ident_f), writes=["identF"])
        P.dma("sync", lambda e: e.dma_start(out=identB[:], in_=ident_b), writes=["identB"])
        P.op("vector", lambda e: e.memset(onesF[:], 1.0), writes=["onesF"])

        with ExitStack() as st:
            xnT = sb(st, "xnT", [128, KC, S], BF16)
            g1s = sb(st, "g1s", [128, KC], F32)
            P.dma("sync", lambda e: e.dma_start(out=g1s[:], in_=g1T), writes=["g1s"])
            with ExitStack() as st1:
                xf = [sb(st1, "xf%d" % i, [128, D], F32) for i in range(2)]
                xb = [sb(st1, "xb%d" % i, [128, D], BF16) for i in range(2)]
                junk = sb(st1, "junk", [128, D], BF16)
                ss = sb(st1, "ss", [128, 16], F32)
                rstd = sb(st1, "rstd", [128, 16], F32)
                pt = [ps(st1, "pt%d" % i, [128, 8, 128], BF16) for i in range(2)]
                P.op("vector", lambda e: e.memset(ss[:], 0.0), writes=["ss"])
                for i in range(16):
                    b = i % 2
                    P.dma("sync", lambda e, i=i, b=b: e.dma_start(out=xf[b][:], in_=xs[i * 128:(i + 1) * 128, :]),
                          writes=[("xf", b)])
                    P.op("scalar", lambda e, i=i, b=b: e.activation(out=junk[:], in_=xf[b][:], func=AF.Square,
                                                                   accum_out=ss[:, i:i + 1]),
                         reads=[("xf", b), "ss"], writes=["junk", ("ss", i)])
                    P.op("vector", lambda e, i=i: e.tensor_scalar(out=rstd[:, i:i + 1], in0=ss[:, i:i + 1],
                                                                  scalar1=1.0 / D, scalar2=EPS, op0=ALU.mult, op1=ALU.add),
                         reads=[("ss", i)], writes=[("rstd", i)])
                    P.op("scalar", lambda e, i=i: e.activation(out=rstd[:, i:i + 1], in_=rstd[:, i:i + 1], func=AF.Sqrt),
                         reads=[("rstd", i)], writes=[("rstd", i)])
                    P.op("vector", lambda e, i=i: e.reciprocal(out=rstd[:, i:i + 1], in_=rstd[:, i:i + 1]),
                         reads=[("rstd", i)], writes=[("rstd", i)])
                    P.op("scalar", lambda e, i=i, b=b: e.activation(out=xb[b][:], in_=xf[b][:], func=AF.Copy,
                                                                   scale=rstd[:, i:i + 1]),
                         reads=[("xf", b), ("rstd", i)], writes=[("xb", b)])
                    for g in range(4):
                        pb = g % 2
                        for c in range(8):
                            P.op("tensor", lambda e, b=b, g=g, c=c, pb=pb: e.transpose(
                                out=pt[pb][:, c, :], in_=xb[b][:, (g * 8 + c) * 128:(g * 8 + c + 1) * 128], identity=identB[:]),
                                reads=[("xb", b), "identB"], writes=[("pt", pb)])
                        P.op("vector", lambda e, i=i, g=g, pb=pb: e.tensor_tensor(
                            out=xnT[:, g * 8:(g + 1) * 8, i * 128:(i + 1) * 128], in0=pt[pb][:],
                            in1=bc(g1s[:, g * 8:(g + 1) * 8].unsqueeze(2), [128, 8, 128]), op=ALU.mult),
                            reads=[("pt", pb), "g1s"], writes=[("xnT", i)])
            P.barrier()

            with ExitStack() as st2:
                NWB = 2
                wt = [sb(st2, "wt%d" % i, [128, KC, 256], BF16) for i in range(NWB)]
                raw = sb(st2, "raw", [128, 4 + S], F32)
                cv = [sb(st2, "cv%d" % i, [128, 512], F32) for i in range(2)]
                sl = [sb(st2, "sl%d" % i, [128, 512], F32) for i in range(2)]
                sq = [sb(st2, "sq%d" % i, [128, 512], F32) for i in range(2)]
                rn = [sb(st2, "rn%d" % i, [128, 512], F32) for i in range(2)]
                stf = [sb(st2, "stf%d" % i, [128, 512], F32) for i in range(2)]
                stb = [sb(st2, "stb%d" % i, [128, 512], BF16) for i in range(2)]
                ttm = [sb(st2, "ttm%d" % i, [128, 4, 128], F32) for i in range(2)]
                ttb = [sb(st2, "ttb%d" % i, [128, 4, 128], BF16) for i in range(2)]
                cw = sb(st2, "cw", [128, 48, 4], F32)
                qg = sb(st2, "qg", [128, 1], F32)
                kg = sb(st2, "kg", [128, 1], F32)
                alb = sb(st2, "alb", [128, NH], F32)
                dtb = sb(st2, "dtb", [128, NH], F32)
                sm = [sb(st2, "sm%d" % i, [128, 32], F32) for i in range(6)]
                pA = ps(st2, "pA", [128, 4, 512], F32)
                pB = [ps(st2, "pB%d" % i, [128, 512], F32) for i in range(2)]
                pTf = ps(st2, "pTf", [128, 4, 128], F32)
                pTb = ps(st2, "pTb", [128, 4, 128], BF16)
                P.dma("sync", lambda e: e.dma_start(out=cw[:], in_=convT), writes=["cw"])
                P.dma("sync", lambda e: e.dma_start(out=qg[:], in_=qg_col), writes=["qg"])
                P.dma("sync", lambda e: e.dma_start(out=kg[:], in_=kg_col), writes=["kg"])
                P.dma("sync", lambda e: e.dma_start(out=alb[:], in_=alog_b), writes=["alb"])
                P.dma("sync", lambda e: e.dma_start(out=dtb[:], in_=dtb_b), writes=["dtb"])
                P.op("vector", lambda e: e.memset(raw[:, 0:4], 0.0), writes=["rawpad"])
                P.op("scalar", lambda e: e.activation(out=alb[:], in_=alb[:], func=AF.Exp), reads=["alb"], writes=["alb"])
                P.op("vector", lambda e: e.tensor_scalar(out=alb[:], in0=alb[:], scalar1=-1.0, scalar2=None, op0=ALU.mult),
                     reads=["alb"], writes=["alb"])

                plan = []
                for h in range(NH):
                    plan.append(dict(kind="ak", segs=[(C_AK + h * 128, 128)], own=False, h=h))
                for h in range(NH):
                    plan.append(dict(kind="aq", segs=[(C_AQ + h * 128, 128)], own=True, h=h))
                for h in range(NH):
                    plan.append(dict(kind="av", segs=[(C_AV + h * 128, 128)], own=False, h=h))
                for j in range(8):
                    plan.append(dict(kind="iq", segs=[(C_IQ + j * 128, 128)], own=True, h=j))
                plan.append(dict(kind="ik", segs=[(C_IK, 64), (C_IK, 64)], own=False, h=0))
                plan.append(dict(kind="iw", segs=[(C_IW, 16)], own=True, h=0))
                for wh, nm, c0 in ((0, "bq", C_BQ), (1, "bk", C_BK), (2, "bv", C_BV)):
                    for h in range(NH):
                        plan.append(dict(kind=nm, segs=[(c0 + h * 128, 128)], own=False, h=h, cb=wh * 16 + h))
                plan.append(dict(kind="bab", segs=[(C_BA, 32)], own=False, h=0))
                for h in range(NH):
                    plan.append(dict(kind="bz", segs=[(C_BZ + h * 128, 128)], own=True, h=h))
                for j in range(KC):
                    plan.append(dict(kind="ga", segs=[(C_GA + j * 128, 128)], own=True, h=j))
                for j in range(KC):
                    plan.append(dict(kind="gb", segs=[(C_GB + j * 128, 128)], own=True, h=j))
                if stop_after == "p2small":
                    plan = [p for p in plan if p["h"] == 0 or p["kind"] in ("ik", "iw", "bab")]

                tiles = []
                i = 0
                while i < len(plan):
                    a = plan[i]
                    if (i + 1 < len(plan) and len(a["segs"]) == 1 and a["segs"][0][1] == 128
                            and len(plan[i + 1]["segs"]) == 1 and plan[i + 1]["segs"][0][1] == 128
                            and plan[i + 1]["segs"][0][0] == a["segs"][0][0] + 128):
                        tiles.append([a, plan[i + 1]])
                        i += 2
                    else:
                        tiles.append([a])
                        i += 1

                w_v = w_in.rearrange("(c p) n -> p c n", p=128)

                def load_tile(ti):
                    t = tiles[ti]
                    wb = ti % NWB
                    off = 0
                    parts = []
                    for blk in t:
                        for (c0, n) in blk["segs"]:
                            parts.append((off, c0, n))
                            off += n
                    merged = []
                    for (o, c0, n) in parts:
                        if merged and merged[-1][1] + merged[-1][2] == c0 and merged[-1][0] + merged[-1][2] == o:
                            merged[-1] = (merged[-1][0], merged[-1][1], merged[-1][2] + n)
                        else:
                            merged.append((o, c0, n))
                    for q in range(4):
                        for (o, c0, n) in merged:
                            P.dma("gpsimd", lambda e, wb=wb, q=q, o=o, c0=c0, n=n: e.dma_start(
                                out=wt[wb][:, q * 8:(q + 1) * 8, o:o + n], in_=w_v[:, q * 8:(q + 1) * 8, c0:c0 + n]),
                                writes=[("wt", wb, q, o)], semkey=("wt", wb, q, o))

                def wt_res(wb):
                    return [k for k in list(P.lastw.keys()) if isinstance(k, tuple) and k[0] == "wt" and k[1] == wb]

                cnt2 = [0]

                def nxt():
                    cnt2[0] += 1
                    return cnt2[0] % 2

                for ti in range(min(NWB - 1, len(tiles))):
                    load_tile(ti)
                for ti, t in enumerate(tiles):
                    if ti + NWB - 1 < len(tiles):
                        load_tile(ti + NWB - 1)
                    wb = ti % NWB
                    off = 0
                    for blk in t:
                        M = sum(n for (_, n) in blk["segs"])
                        own = blk["own"]
                        sbl = [2, 3] if own else [0, 1, 2, 3]
                        kind = blk["kind"]
                        h = blk["h"]
                        wres = wt_res(wb)
                        for c in range(KC):
                            for j in sbl:
                                P.op("tensor", lambda e, wb=wb, c=c, j=j, off=off, M=M: e.matmul(
                                    pA[0:M, j, :], lhsT=wt[wb][:, c, off:off + M], rhs=xnT[:, c, j * 512:(j + 1) * 512],
                                    start=(c == 0), stop=(c == KC - 1)),
                                    reads=wres if (c == 0 and j == sbl[0]) or (c == KC - 1 and j == sbl[-1]) else [], writes=[("pA", j)])
                        if kind in ("bq", "bk", "bv"):
                            for j in sbl:
                                P.op("scalar", lambda e, j=j: e.activation(out=raw[:, 4 + j * 512:4 + (j + 1) * 512], in_=pA[:, j, :],
                                                                          func=AF.Copy),
                                     reads=[("pA", j), "rawpad"], writes=[("raw", j)])
                            cb = blk["cb"]
                            for j in sbl:
                                k = nxt()
                                rr = [("raw", j)] + ([("raw", j - 1)] if j > 0 else [])
                                P.op("vector", lambda e, j=j, k=k, cb=cb: e.tensor_scalar(
                                    out=cv[k][:], in0=raw[:, 1 + j * 512:1 + (j + 1) * 512], scalar1=cw[:, cb, 0:1], scalar2=None,
                                    op0=ALU.mult), reads=rr + ["cw"], writes=[("cv", k)])
                                for jj in (1, 2, 3):
                                    P.op("vector", lambda e, j=j, k=k, cb=cb, jj=jj: e.scalar_tensor_tensor(
                                        out=cv[k][:], in0=raw[:, 1 + jj + j * 512:1 + jj + (j + 1) * 512], scalar=cw[:, cb, jj:jj + 1],
                                        in1=cv[k][:], op0=ALU.mult, op1=ALU.add), reads=rr + [("cv", k)], writes=[("cv", k)])
                                P.op("scalar", lambda e, k=k: e.activation(out=sl[k][:], in_=cv[k][:], func=AF.Silu),
                                     reads=[("cv", k)], writes=[("sl", k)])
                                src = sl[k]
                                srckey = ("sl", k)
                                if kind in ("bq", "bk"):
                                    P.op("scalar", lambda e, k=k: e.activation(out=sq[k][:], in_=sl[k][:], func=AF.Square),
                                         reads=[("sl", k)], writes=[("sq", k)])
                                    P.op("tensor", lambda e, k=k: e.matmul(pB[k][:], lhsT=onesF[:], rhs=sq[k][:], start=True, stop=True),
                                         reads=[("sq", k), "onesF"], writes=[("pB", k)])
                                    P.op("scalar", lambda e, k=k: e.activation(out=rn[k][:], in_=pB[k][:], func=AF.Sqrt, bias=EPS,
                                                                              scale=1.0),
                                         reads=[("pB", k)], writes=[("rn", k)])
                                    P.op("vector", lambda e, k=k: e.reciprocal(out=rn[k][:], in_=rn[k][:]),
                                         reads=[("rn", k)], writes=[("rn", k)])
                                    scl = (128.0 ** -0.5) if kind == "bq" else 1.0
                                    P.op("vector", lambda e, k=k, scl=scl: e.scalar_tensor_tensor(
                                        out=stf[k][:], in0=sl[k][:], scalar=scl, in1=rn[k][:], op0=ALU.mult, op1=ALU.mult),
                                        reads=[("sl", k), ("rn", k)], writes=[("stf", k)])
                                    dst = bqT if kind == "bq" else bkT
                                    P.dma("sync", lambda e, k=k, h=h, j=j, dst=dst: e.dma_start(
                                        out=dst[h, :, j * 512:(j + 1) * 512], in_=stf[k][:]),
                                        reads=[("stf", k)], writes=[(kind, h, j)], semkey=("stfo", k))
                                    src = stf[k]
                                    srckey = ("stf", k)
                                if kind in ("bk", "bv"):
                                    for q in range(4):
                                        P.op("tensor", lambda e, q=q, src=src: e.transpose(out=pTf[:, q, :], in_=src[:, q * 128:(q + 1) * 128],
                                                                                           identity=identF[:]),
                                             reads=[srckey, "identF"], writes=["pTf"])
                                    P.op("vector", lambda e, k=k: e.tensor_copy(out=ttm[k][:], in_=pTf[:]),
                                         reads=["pTf"], writes=[("ttm", k)])
                                    dst = bk if kind == "bk" else bv
                                    P.dma("sync", lambda e, k=k, h=h, j=j, dst=dst: e.dma_start(
                                        out=dst[h, :, j * 4:(j + 1) * 4, :], in_=ttm[k][:]),
                                        reads=[("ttm", k)], writes=[(kind + "t", h, j)], semkey=("ttmo", k))
                        elif kind in ("ak", "aq"):
                            gcol = kg if kind == "ak" else qg
                            for j in sbl:
                                k = nxt()
                                P.op("scalar", lambda e, j=j, k=k: e.activation(out=sl[k][:], in_=pA[:, j, :], func=AF.Copy),
                                     reads=[("pA", j)], writes=[("sl", k)])
                                P.op("scalar", lambda e, k=k: e.activation(out=sq[k][:], in_=sl[k][:], func=AF.Square),
                                     reads=[("sl", k)], writes=[("sq", k)])
                                P.op("tensor", lambda e, k=k: e.matmul(pB[k][:], lhsT=onesF[:], rhs=sq[k][:], start=True, stop=True),
                                     reads=[("sq", k), "onesF"], writes=[("pB", k)])
                                P.op("scalar", lambda e, k=k: e.activation(out=rn[k][:], in_=pB[k][:], func=AF.Sqrt, bias=EPS,
                                                                          scale=1.0 / 128.0),
                                     reads=[("pB", k)], writes=[("rn", k)])
                                P.op("vector", lambda e, k=k: e.reciprocal(out=rn[k][:], in_=rn[k][:]),
                                     reads=[("rn", k)], writes=[("rn", k)])
                                P.op("vector", lambda e, k=k, gcol=gcol: e.scalar_tensor_tensor(
                                    out=stb[k][:], in0=sl[k][:], scalar=gcol[:, 0:1], in1=rn[k][:], op0=ALU.mult, op1=ALU.mult),
                                    reads=[("sl", k), ("rn", k), "qg", "kg"], writes=[("stb", k)])
                                if kind == "ak":
                                    P.dma("sync", lambda e, k=k, h=h, j=j: e.dma_start(out=akT[h, :, j * 512:(j + 1) * 512], in_=stb[k][:]),
                                          reads=[("stb", k)], writes=[("akT", h, j)], semkey=("stbo", k))
                                else:
                                    P.dma("sync", lambda e, k=k, h=h, j=j: e.dma_start(out=aqT[h, :, (j - 2) * 512:(j - 1) * 512], in_=stb[k][:]),
                                          reads=[("stb", k)], writes=[("aqT", h, j)], semkey=("stbo", k))
                        elif kind == "av":
                            for j in sbl:
                                k = nxt()
                                P.op("scalar", lambda e, j=j, k=k: e.activation(out=stb[k][:], in_=pA[:, j, :], func=AF.Copy),
                                     reads=[("pA", j)], writes=[("stb", k)])
                                for q in range(4):
                                    P.op("tensor", lambda e, q=q, k=k: e.transpose(out=pTb[:, q, :], in_=stb[k][:, q * 128:(q + 1) * 128],
                                                                                   identity=identB[:]),
                                         reads=[("stb", k), "identB"], writes=["pTb"])
                                P.op("vector", lambda e, k=k: e.tensor_copy(out=ttb[k][:], in_=pTb[:]),
                                     reads=["pTb"], writes=[("ttb", k)])
                                P.dma("sync", lambda e, k=k, h=h, j=j: e.dma_start(
                                    out=av[h, :, j * 4:(j + 1) * 4, :], in_=ttb[k][:]),
                                    reads=[("ttb", k)], writes=[("av", h, j)], semkey=("ttbo", k))
                        elif kind in ("iq", "ik", "ga", "gb"):
                            func = AF.Sigmoid if kind in ("ga", "gb") else AF.Copy
                            for j in sbl:
                                k = nxt()
                                P.op("scalar", lambda e, j=j, k=k, func=func: e.activation(out=stb[k][:], in_=pA[:, j, :], func=func),
                                     reads=[("pA", j)], writes=[("stb", k)])
                                if kind == "ik":
                                    dsto = ikT2[:, j * 512:(j + 1) * 512]
                                elif kind == "iq":
                                    dsto = iqT[h, :, (j - 2) * 512:(j - 1) * 512]
                                elif kind == "ga":
                                    dsto = gsa[h, :, (j - 2) * 512:(j - 1) * 512]
                                else:
                                    dsto = gsb[h, :, (j - 2) * 512:(j - 1) * 512]
                                P.dma("sync", lambda e, k=k, dsto=dsto: e.dma_start(out=dsto, in_=stb[k][:]),
                                      reads=[("stb", k)], writes=[(kind, h, j)], semkey=("stbo", k))
                        elif kind == "bz":
                            for j in sbl:
                                k = nxt()
                                P.op("scalar", lambda e, j=j, k=k: e.activation(out=stf[k][:], in_=pA[:, j, :], func=AF.Silu),
                                     reads=[("pA", j)], writes=[("stf", k)])
                                P.dma("sync", lambda e, k=k, h=h, j=j: e.dma_start(out=zsT[h, :, (j - 2) * 512:(j - 1) * 512], in_=stf[k][:]),
                                      reads=[("stf", k)], writes=[("zsT", h, j)], semkey=("stfo", k))
                        elif kind in ("iw", "bab"):
                            for j in sbl:
                                k = nxt()
                                P.op("scalar", lambda e, j=j, k=k, M=M: e.activation(out=sl[k][0:M, :], in_=pA[0:M, j, :], func=AF.Copy),
                                     reads=[("pA", j)], writes=[("sl", k)])
                                for q in range(4):
                                    P.op("tensor", lambda e, q=q, k=k, M=M: e.transpose(out=pTf[:, q, 0:M], in_=sl[k][0:M, q * 128:(q + 1) * 128],
                                                                                        identity=identF[0:M, 0:M]),
                                         reads=[("sl", k), "identF"], writes=["pTf"])
                                P.op("vector", lambda e, k=k, M=M: e.tensor_copy(out=ttm[k][:, :, 0:M], in_=pTf[:, :, 0:M]),
                                     reads=["pTf"], writes=[("ttm", k)])
                                if kind == "iw":
                                    P.dma("sync", lambda e, k=k, j=j: e.dma_start(
                                        out=iw[(j - 2) * 512:(j - 1) * 512, :].rearrange("(q p) d -> p q d", p=128), in_=ttm[k][:, :, 0:16]),
                                        reads=[("ttm", k)], writes=[("iw", j)], semkey=("ttmo", k))
                                else:
                                    xa, ax_, ee = sm[0], sm[1], sm[2]
                                    for q in range(4):
                                        P.op("vector", lambda e, k=k, q=q: e.tensor_tensor(out=ttm[k][:, q, 0:16], in0=ttm[k][:, q, 0:16], in1=dtb[:],
                                                                                          op=ALU.add),
                                             reads=[("ttm", k), "dtb"], writes=[("ttm", k)])
                                    xv = ttm[k][:, :, 0:16]
                                    tmpa = ttm[k][:, :, 32:48]
                                    tmpb = ttm[k][:, :, 48:64]
                                    P.op("vector", lambda e, xv=xv, tmpa=tmpa: e.tensor_scalar(out=tmpa, in0=xv, scalar1=60.0, scalar2=None, op0=ALU.min),
                                         reads=[("ttm", k)], writes=[("ttm", k)])
                                    P.op("scalar", lambda e, tmpa=tmpa: e.activation(out=tmpa, in_=tmpa, func=AF.Exp),
                                         reads=[("ttm", k)], writes=[("ttm", k)])
                                    P.op("scalar", lambda e, tmpa=tmpa: e.activation(out=tmpa, in_=tmpa, func=AF.Ln, bias=1.0, scale=1.0),
                                         reads=[("ttm", k)], writes=[("ttm", k)])
                                    for q in range(4):
                                        P.op("vector", lambda e, k=k, q=q: e.tensor_tensor(out=ttm[k][:, q, 0:16], in0=ttm[k][:, q, 32:48], in1=alb[:],
                                                                                          op=ALU.mult),
                                             reads=[("ttm", k), "alb"], writes=[("ttm", k)])
                                    P.op("scalar", lambda e, k=k: e.activation(out=ttm[k][:, :, 16:32], in_=ttm[k][:, :, 16:32], func=AF.Sigmoid),
                                         reads=[("ttm", k)], writes=[("ttm", k)])
                                    P.dma("sync", lambda e, k=k, j=j: e.dma_start(
                                        out=glb[j * 512:(j + 1) * 512, :].rearrange("(q p) d -> p q d", p=128), in_=ttm[k][:, :, 0:32]),
                                        reads=[("ttm", k)], writes=[("glb", j)], semkey=("ttmo", k))
                        off += M
            P.barrier()

        if stop_after in ("p2", "p2small"):
            P.emit()
            return nc

        import os as _os
        LIM = [int(_os.environ.get("OPLIM", "1000000000")), 0, False]

        def _lim():
            if LIM[2]:
                LIM[1] += 1
                return LIM[1] > LIM[0]
            return False

        REC = [None]

        def _op(eng, fn, r, w):
            if REC[0] is not None:
                REC[0].append(("op", eng, fn, list(r), list(w), None))
                return None
            return P.op(eng, fn, reads=r, writes=w)

        def V(fn, r=(), w=()):
            return _op("vector", fn, r, w)

        def A(fn, r=(), w=()):
            return _op("scalar", fn, r, w)

        def G(fn, r=(), w=()):
            return _op("gpsimd", fn, r, w)

        def T(fn, r=(), w=()):
            return _op("tensor", fn, r, w)

        def DMA(fn, r=(), w=(), key=None, eng="sync"):
            if REC[0] is not None:
                REC[0].append(("dma", eng, fn, list(r), list(w), key))
                return None
            return P.dma(eng, fn, reads=r, writes=w, semkey=key)

        def emit_rec(o):
            kind_, eng, fn, r, w, key = o
            if kind_ == "op":
                P.op(eng, fn, reads=r, writes=w)
            else:
                P.dma(eng, fn, reads=r, writes=w, semkey=key)

        with ExitStack() as st:
            ikS = sb(st, "ikS", [128, S], BF16)
            kbS = sb(st, "kbS", [128, S], F32)
            iqA = sb(st, "iqA", [128, 8, OWN], BF16)
            iwS = [sb(st, "iwS%d" % i, [128, 16], F32) for i in range(2)]
            rl = [sb(st, "rl%d" % i, [128, 2, 512], F32) for i in range(3)]
            acc = sb(st, "acc", [128, S], F32)
            sc = sb(st, "sc", [128, S], F32)
            wk = sb(st, "wk", [128, S], F32)
            mx = sb(st, "mx", [128, 8], F32)
            mk = sb(st, "mk", [128, S], BF16)
            mts = [sb(st, "mts%d" % i, [128, 16, 128], BF16) for i in range(2)]
            pS = [ps(st, "pS%d" % i, [128, 2, 512]) for i in range(3)]
            pT = [ps(st, "pT%d" % i, [128, 8, 128], BF16) for i in range(2)]
            DMA(lambda e: e.dma_start(out=ikS[:], in_=ikT2), w=["ikS"])
            DMA(lambda e: e.dma_start(out=kbS[:], in_=keybias), w=["kbS"])
            DMA(lambda e: e.dma_start(out=iqA[:], in_=iqT.rearrange("j p t -> p j t")), w=["iqA"])
            V(lambda e: e.memset(mk[:], 0.0), w=["mk"])
            for qt in range(8):
                b = qt % 2
                nk = OWN + (qt + 1) * 128
                DMA(lambda e, b=b, qt=qt: e.dma_start(out=iwS[b][:], in_=iw[qt * 128:(qt + 1) * 128, :]), w=[("iwS", b)])
                for hi in range(16):
                    blk, sub = hi // 2, hi % 2
                    for half in range(2):
                        r = (hi * 2 + half) % 3
                        wd = 1024 if half == 0 else nk - 1024
                        for n in range(2):
                            cn = min(512, wd - n * 512)
                            if cn <= 0:
                                continue
                            c0 = half * 1024 + n * 512
                            T(lambda e, r=r, n=n, qt=qt, sub=sub, blk=blk, c0=c0, cn=cn: e.matmul(
                                pS[r][:, n, 0:cn], lhsT=iqA[sub * 64:(sub + 1) * 64, blk, qt * 128:(qt + 1) * 128], rhs=ikS[sub * 64:(sub + 1) * 64, c0:c0 + cn],
                                start=True, stop=True), r=["iqA", "ikS"], w=[("pS", r)])
                        psf = pS[r][:].rearrange("p a b -> p (a b)")[:, 0:wd]
                        rlf = rl[r][:].rearrange("p a b -> p (a b)")[:, 0:wd]
                        A(lambda e, psf=psf, rlf=rlf: e.activation(out=rlf, in_=psf, func=AF.Relu), r=[("pS", r)], w=[("rl", r)])
                        acch = acc[:, half * 1024:half * 1024 + wd]
                        if hi == 0:
                            V(lambda e, b=b, acch=acch, rlf=rlf: e.tensor_scalar(out=acch, in0=rlf, scalar1=iwS[b][:, 0:1], scalar2=None, op0=ALU.mult),
                              r=[("rl", r), ("iwS", b)], w=[("acc", half)])
                        else:
                            V(lambda e, b=b, acch=acch, rlf=rlf, hi=hi: e.scalar_tensor_tensor(
                                out=acch, in0=rlf, scalar=iwS[b][:, hi:hi + 1], in1=acch, op0=ALU.mult, op1=ALU.add),
                              r=[("rl", r), ("iwS", b), ("acc", half)], w=[("acc", half)])
                V(lambda e, nk=nk: e.tensor_tensor(out=acc[:, 0:nk], in0=acc[:, 0:nk], in1=kbS[:, 0:nk], op=ALU.add),
                  r=[("acc", 0), ("acc", 1), "kbS"], w=[("acc", 0), ("acc", 1)])
                G(lambda e, qt=qt, nk=nk: e.affine_select(out=sc[:, 0:nk], in_=acc[:, 0:nk], pattern=[[-1, nk]], compare_op=ALU.is_ge, fill=-3.0e38,
                                                          base=OWN + qt * 128, channel_multiplier=1),
                  r=[("acc", 0), ("acc", 1)], w=["sc"])
                for rr in range(32):
                    srcb = sc if rr == 0 else wk
                    srck = "sc" if rr == 0 else "wk"
                    V(lambda e, srcb=srcb, nk=nk: e.max(out=mx[:], in_=srcb[:, 0:nk]), r=[srck], w=["mx"])
                    if rr < 31:
                        V(lambda e, srcb=srcb, nk=nk: e.match_replace(out=wk[:, 0:nk], in_to_replace=mx[:], in_values=srcb[:, 0:nk], imm_value=-1.0e30),
                          r=[srck, "mx"], w=["wk"])
                V(lambda e, nk=nk: e.tensor_scalar(out=mk[:, 0:nk], in0=sc[:, 0:nk], scalar1=mx[:, 7:8], scalar2=None, op0=ALU.is_ge), r=["sc", "mx"], w=["mk"])
                for g2 in range(2):
                    for c in range(8):
                        T(lambda e, g2=g2, c=c: e.transpose(out=pT[g2][:, c, :], in_=mk[:, (g2 * 8 + c) * 128:(g2 * 8 + c + 1) * 128], identity=identB[:]),
                          r=["mk", "identB"], w=[("pT", g2)])
                    A(lambda e, g2=g2, b=b: e.activation(out=mts[b][:, g2 * 8:(g2 + 1) * 8, :], in_=pT[g2][:], func=AF.Copy),
                      r=[("pT", g2)], w=[("mts", b)])
                DMA(lambda e, b=b, qt=qt: e.dma_start(out=mT[qt], in_=mts[b][:]),
                    r=[("mts", b)], w=[("mT", qt)], key=("mtso", b))
        P.barrier()
        if stop_after == "p3":
            P.emit()
            return nc

        with ExitStack() as st:
            mTS = sb(st, "mTS", [128, 8, 16, 128], BF16)
            rbS = sb(st, "rbS", [32, NH], F32)
            ohS = sb(st, "ohS", [32, 1280], F32)
            fvS = sb(st, "fvS", [NH, 1280], F32)
            b31S = sb(st, "b31S", [128, NH], F32)
            JS = sb(st, "JS", [128, 128], F32)
            onesB = sb(st, "onesB", [128, 128], BF16)
            hk = [sb(st, "hk%d" % i, [128, 512], F32) for i in range(2)]
            corr = [sb(st, "corr%d" % i, [128, 5, 512], BF16) for i in range(2)]
            kTS = [sb(st, "kTS%d" % i, [128, S], BF16) for i in range(2)]
            qTS = [sb(st, "qTS%d" % i, [128, OWN], BF16) for i in range(2)]
            vS = [sb(st, "vS%d" % i, [128, 16, 128], BF16) for i in range(2)]
            Eb = [sb(st, "Eb%d" % i, [128, 512], BF16) for i in range(3)]
            Pm = [sb(st, "Pm%d" % i, [128, 512], BF16) for i in range(3)]
            rinv = [sb(st, "rinv%d" % i, [128, 512], F32) for i in range(2)]
            yab = [sb(st, "yab%d" % i, [128, 512], BF16) for i in range(2)]
            pS4 = [ps(st, "pS4%d" % i, [128, 512]) for i in range(3)]
            pO = [ps(st, "pO%d" % i, [128, 512]) for i in range(2)]
            pR = [ps(st, "pR%d" % i, [128, 512]) for i in range(2)]
            pC = ps(st, "pC", [128, 512])
            DMA(lambda e: e.dma_start(out=mTS[:], in_=mT.rearrange("q p s t -> p q s t")), w=["mTS"])
            DMA(lambda e: e.dma_start(out=rbS[:], in_=rel_b), w=["rbS"])
            DMA(lambda e: e.dma_start(out=ohS[:], in_=oh_c), w=["ohS"])
            DMA(lambda e: e.dma_start(out=b31S[:], in_=b31_b), w=["b31S"])
            DMA(lambda e: e.dma_start(out=JS[:], in_=J_c), w=["JS"])
            V(lambda e: e.memset(onesB[:], 1.0), w=["onesB"])
            tg = [pO[0], pO[1], pR[0]]
            for n3, (c0, cn) in enumerate(((0, 512), (512, 512), (1024, 256))):
                T(lambda e, n3=n3, c0=c0, cn=cn: e.matmul(tg[n3][0:NH, 0:cn], lhsT=rbS[:], rhs=ohS[:, c0:c0 + cn], start=True, stop=True),
                  r=["rbS", "ohS"], w=[("tg", n3)])
                A(lambda e, n3=n3, c0=c0, cn=cn: e.activation(out=fvS[:, c0:c0 + cn], in_=tg[n3][0:NH, 0:cn], func=AF.Exp),
                  r=[("tg", n3)], w=["fvS"])
            DMA(lambda e: e.dma_start(out=fv, in_=fvS[:]), r=["fvS"], w=["fv"])
            P.barrier()
            OFFS = (128, 0, -128, -256, -384)
            import os
            P4H = int(os.environ.get("P4H", NH))
            LA = 2

            def p4_loads(h):
                b = h % 2
                DMA(lambda e, b=b, h=h: e.dma_start(out=kTS[b][:], in_=akT[h]), w=[("kTS", b)])
                DMA(lambda e, b=b, h=h: e.dma_start(out=qTS[b][:], in_=aqT[h]), w=[("qTS", b)])
                DMA(lambda e, b=b, h=h: e.dma_start(out=vS[b][:], in_=av[h]), w=[("vS", b)])

            def p4_corr(h, oi):
                b = h % 2
                o = OFFS[oi]
                hb = oi % 2
                src_ap = bass.AP(fv.tensor, h * 1280 + o + 513, [[1, 128], [1, 512]])
                DMA(lambda e, hb=hb, src_ap=src_ap: e.dma_start(out=hk[hb][:], in_=src_ap), w=[("hk", hb)])
                T(lambda e, hb=hb: e.matmul(pC[:], lhsT=JS[:], rhs=hk[hb][:], start=True, stop=True), r=[("hk", hb), "JS"], w=["pC"])
                A(lambda e, b=b, oi=oi: e.activation(out=corr[b][:, oi, :], in_=pC[:], func=AF.Copy), r=["pC"], w=[("corr", b)])

            if P4H > 0:
                p4_loads(0)
                for oi in range(5):
                    p4_corr(0, oi)
            for h in range(P4H):
                b = h % 2
                if h + 1 < P4H:
                    p4_loads(h + 1)
                pairs = [(tb, sti) for tb in range(2) for sti in range(12 if tb == 0 else 16)]
                npairs = len(pairs)

                def s_stage(p, h=h, b=b):
                    tb, sti = pairs[p]
                    k2 = p % 3
                    o = OWN + 512 * tb - 128 * sti
                    T(lambda e, k2=k2, b=b, sti=sti, tb=tb: e.matmul(pS4[k2][:], lhsT=kTS[b][:, sti * 128:(sti + 1) * 128],
                                                                  rhs=qTS[b][:, tb * 512:(tb + 1) * 512], start=True, stop=True),
                      r=[("kTS", b), ("qTS", b)], w=[("pS4", k2)])
                    A(lambda e, k2=k2, h=h: e.activation(out=Eb[k2][:], in_=pS4[k2][:], func=AF.Exp, bias=b31S[:, h:h + 1],
                                                        scale=128.0 ** -0.5), r=[("pS4", k2), "b31S"], w=[("Eb", k2)])
                    V(lambda e, k2=k2, sti=sti, tb=tb: e.tensor_tensor(out=Pm[k2][:].rearrange("p (a b) -> p a b", a=4),
                                                                      in0=Eb[k2][:].rearrange("p (a b) -> p a b", a=4),
                                                                      in1=mTS[:, tb * 4:(tb + 1) * 4, sti, :],
                                                                      op=ALU.mult), r=[("Eb", k2), "mTS"], w=[("Pm", k2)])
                    if o in OFFS:
                        oi = OFFS.index(o)
                        G(lambda e, k2=k2, b=b, oi=oi: e.tensor_tensor(out=Pm[k2][:], in0=Pm[k2][:], in1=corr[b][:, oi, :], op=ALU.mult),
                          r=[("Pm", k2), ("corr", b)], w=[("Pm", k2)])

                def pv_stage(p, h=h, b=b):
                    tb, sti = pairs[p]
                    k2 = p % 3
                    ob = tb
                    nst = 12 if tb == 0 else 16
                    T(lambda e, ob=ob, b=b, sti=sti, k2=k2, nst=nst: e.matmul(pO[ob][:], lhsT=vS[b][:, sti, :], rhs=Pm[k2][:],
                                                                          start=(sti == 0), stop=(sti == nst - 1)),
                      r=[("Pm", k2), ("vS", b)], w=[("pO", ob)])
                    T(lambda e, ob=ob, k2=k2, sti=sti, nst=nst: e.matmul(pR[ob][:], lhsT=onesB[:], rhs=Pm[k2][:],
                                                                     start=(sti == 0), stop=(sti == nst - 1)),
                      r=[("Pm", k2), "onesB"], w=[("pR", ob)])
                    if sti == nst - 1:
                        V(lambda e, ob=ob: e.reciprocal(out=rinv[ob][:], in_=pR[ob][:]), r=[("pR", ob)], w=[("rinv", ob)])
                        V(lambda e, ob=ob: e.tensor_tensor(out=yab[ob][:], in0=pO[ob][:], in1=rinv[ob][:], op=ALU.mult),
                          r=[("pO", ob), ("rinv", ob)], w=[("yab", ob)])
                        DMA(lambda e, ob=ob, h=h, tb=tb: e.dma_start(out=yaT[h, :, tb * 512:(tb + 1) * 512], in_=yab[ob][:]),
                            r=[("yab", ob)], w=[("yaT", h, tb)], key=("yabo", ob))

                for p in range(npairs + LA):
                    if p < npairs:
                        s_stage(p)
                    if p - LA >= 0:
                        pv_stage(p - LA)
                    if h + 1 < P4H and p in (4, 8, 12, 16, 20):
                        p4_corr(h + 1, (p - 4) // 4)
        P.barrier()
        if stop_after == "p4":
            P.emit()
            return nc

        with ExitStack() as st:
            UmS = sb(st, "UmS", [128, 128], F32)
            LsS = sb(st, "LsS", [128, 128], F32)
            ggS = sb(st, "ggS", [128, 1], F32)
            glS = sb(st, "glS", [128, 16, 32], F32)
            gcol = sb(st, "gcol", [128, 16, NH], F32)
            glast = sb(st, "glast", [128, 16, NH], F32)
            bgS = sb(st, "bgS", [128, 16, NH], F32)
            ekd = sb(st, "ekd", [128, 16, NH], F32)
            egl = sb(st, "egl", [128, 16, NH], F32)
            kTh = [sb(st, "kTh%d" % i, [128, S], F32) for i in range(2)]
            qTh = [sb(st, "qTh%d" % i, [128, OWN], F32) for i in range(2)]
            kth = [sb(st, "kth%d" % i, [128, 16, 128], F32) for i in range(2)]
            vth = [sb(st, "vth%d" % i, [128, 16, 128], F32) for i in range(2)]
            zsS = [sb(st, "zsS%d" % i, [128, OWN], F32) for i in range(2)]
            uS = [sb(st, "uS%d" % i, [128, 16, 128], F32) for i in range(2)]
            wTS = [sb(st, "wTS%d" % i, [128, 16, 128], F32) for i in range(2)]
            kdS = [sb(st, "kdS%d" % i, [128, 16, 128], F32) for i in range(2)]
            qdS = [sb(st, "qdS%d" % i, [128, 8, 128], F32) for i in range(2)]
            qkS = [sb(st, "qkS%d" % i, [128, 8, 128], F32) for i in range(2)]
            Sst = [sb(st, "Sst%d" % i, [128, 128], F32) for i in range(2)]
            ybst = [sb(st, "ybst%d" % i, [128, OWN], BF16) for i in range(2)]
            ssq = sb(st, "ssq5", [128, 8], F32)
            rs5 = sb(st, "rs5", [128, 8], F32)
            junk5 = sb(st, "junk5", [128, 128], F32)
            on5 = [sb(st, "on5%d" % i, [128, 128], F32) for i in range(2)]
            vn5 = [sb(st, "vn5%d" % i, [128, 128], F32) for i in range(2)]
            TN = ("Ug", "dA", "dec", "dB", "decT", "egb", "N", "M0", "M1", "N0", "N1", "P", "Q", "vb", "kbg")
            NCH = 6
            tmp = {nm: [sb(st, "t5%s%d" % (nm, i), [128, 128], F32) for i in range(NCH)] for nm in TN}
            pbk = [ps(st, "p5b%d" % i, [128, 4, 128]) for i in range(7)]
            def pslot(pb, k):
                return pbk[pb][:, k, :]
            PSN = {"Grow": 0, "KK": 1, "NT": 2, "M2": 0, "N2": 1, "pP": 2, "pQ": 3, "pu": 0, "pw": 1, "pqk": 2}

            DMA(lambda e: e.dma_start(out=UmS[:], in_=um_c), w=["UmS"])
            DMA(lambda e: e.dma_start(out=LsS[:], in_=ls_c), w=["LsS"])
            DMA(lambda e: e.dma_start(out=ggS[:], in_=gg_col), w=["ggS"])
            DMA(lambda e: e.dma_start(out=glS[:], in_=glb.rearrange("(i p) d -> p i d", p=128)), w=["glS"])
            for i in range(16):
                pg = pbk[i % 2][:, 0, 0:NH]
                pl = pbk[2 + i % 2][:, 0, 0:NH]
                T(lambda e, pg=pg, i=i: e.matmul(pg, lhsT=UmS[:], rhs=glS[:, i, 0:NH], start=True, stop=True), r=["UmS", "glS"], w=[("bank", i % 2)])
                T(lambda e, pl=pl, i=i: e.matmul(pl, lhsT=onesF[:], rhs=glS[:, i, 0:NH], start=True, stop=True), r=["onesF", "glS"], w=[("bank", 2 + i % 2)])
                A(lambda e, pg=pg, i=i: e.activation(out=gcol[:, i, :], in_=pg, func=AF.Copy), w=["gcol", ("bank", i % 2)])
                A(lambda e, pl=pl, i=i: e.activation(out=glast[:, i, :], in_=pl, func=AF.Copy), w=["glast", ("bank", 2 + i % 2)])
            A(lambda e: e.activation(out=bgS[:], in_=gcol[:], func=AF.Exp), r=["gcol"], w=["bgS"])
            V(lambda e: e.tensor_tensor(out=bgS[:], in0=bgS[:], in1=glS[:, :, NH:2 * NH], op=ALU.mult), r=["bgS", "glS"], w=["bgS"])
            V(lambda e: e.tensor_tensor(out=ekd[:], in0=glast[:], in1=gcol[:], op=ALU.subtract), r=["glast", "gcol"], w=["ekd"])
            A(lambda e: e.activation(out=ekd[:], in_=ekd[:], func=AF.Exp), r=["ekd"], w=["ekd"])
            A(lambda e: e.activation(out=egl[:], in_=glast[:], func=AF.Exp), r=["glast"], w=["egl"])

            def prep(h, i, pb):
                hb = h % 2
                own = i >= 8
                io = i - 8
                t = {nm: tmp[nm][pb] for nm in TN}
                K_ = lambda nm: ("t5", nm, pb)
                PK = lambda nm: ("bank", pb)
                psl = {nm: pslot(pb, k) for nm, k in PSN.items()}
                glc = glS[:, i, h:h + 1]
                btc = glS[:, i, NH + h:NH + h + 1]
                gcc = gcol[:, i, h:h + 1]
                kTc = kTh[hb][:, i * 128:(i + 1) * 128]
                V(lambda e: e.tensor_scalar(out=t["Ug"][:], in0=UmS[:], scalar1=glc, scalar2=None, op0=ALU.mult), r=["UmS", "glS"], w=[K_("Ug")])
                T(lambda e: e.matmul(psl["Grow"], lhsT=onesF[:], rhs=t["Ug"][:], start=True, stop=True), r=[K_("Ug"), "onesF"], w=[PK("Grow")])
                V(lambda e: e.tensor_scalar(out=t["dA"][:], in0=psl["Grow"], scalar1=gcc, scalar2=0.0, op0=ALU.subtract, op1=ALU.max),
                  r=[PK("Grow"), "gcol"], w=[K_("dA")])
                A(lambda e: e.activation(out=t["dec"][:], in_=t["dA"][:], func=AF.Exp, scale=-1.0), r=[K_("dA")], w=[K_("dec")])
                G(lambda e: e.tensor_tensor(out=t["dec"][:], in0=t["dec"][:], in1=LsS[:], op=ALU.mult), r=[K_("dec"), "LsS"], w=[K_("dec")])
                if own:
                    V(lambda e: e.tensor_scalar(out=t["dB"][:], in0=psl["Grow"], scalar1=gcc, scalar2=0.0, op0=ALU.subtract, op1=ALU.min),
                      r=[PK("Grow"), "gcol"], w=[K_("dB")])
                    A(lambda e: e.activation(out=t["decT"][:], in_=t["dB"][:], func=AF.Exp), r=[K_("dB")], w=[K_("decT")])
                    G(lambda e: e.tensor_tensor(out=t["decT"][:], in0=t["decT"][:], in1=UmS[:], op=ALU.mult), r=[K_("decT"), "UmS"], w=[K_("decT")])
                    A(lambda e: e.activation(out=t["egb"][:], in_=psl["Grow"], func=AF.Exp), r=[PK("Grow")], w=[K_("egb")])
                T(lambda e: e.matmul(psl["KK"], lhsT=kTc, rhs=kTc, start=True, stop=True), r=[("kTh", hb)], w=[PK("KK")])
                V(lambda e: e.scalar_tensor_tensor(out=t["N"][:], in0=psl["KK"], scalar=btc, in1=t["dec"][:], op0=ALU.mult, op1=ALU.mult),
                  r=[PK("KK"), K_("dec"), "glS"], w=[K_("N")])
                T(lambda e: e.transpose(out=psl["NT"], in_=t["N"][:], identity=identF[:]), r=[K_("N"), "identF"], w=[PK("NT")])
                A(lambda e: e.activation(out=t["M0"][:], in_=psl["NT"], func=AF.Copy), r=[PK("NT")], w=[K_("M0")])
                G(lambda e: e.tensor_copy(out=t["N0"][:], in_=t["N"][:]), r=[K_("N")], w=[K_("N0")])
                V(lambda e: e.tensor_tensor(out=t["P"][:], in0=identF[:], in1=t["M0"][:], op=ALU.subtract), r=["identF", K_("M0")], w=[K_("P")])
                G(lambda e: e.tensor_tensor(out=t["Q"][:], in0=identF[:], in1=t["N"][:], op=ALU.subtract), r=["identF", K_("N")], w=[K_("Q")])
                cur = 0
                for lv in range(1, 7):
                    last = lv == 6
                    Mc, Nc = "M%d" % cur, "N%d" % cur
                    Mn, Nn = "M%d" % (1 - cur), "N%d" % (1 - cur)
                    T(lambda e, Mc=Mc, Nc=Nc: e.matmul(psl["M2"], lhsT=t[Nc][:], rhs=t[Mc][:], start=True, stop=True),
                      r=[K_(Mc), K_(Nc)], w=[PK("M2")])
                    A(lambda e, Mn=Mn: e.activation(out=t[Mn][:], in_=psl["M2"], func=AF.Copy), r=[PK("M2")], w=[K_(Mn)])
                    if not last:
                        T(lambda e, Mc=Mc, Nc=Nc: e.matmul(psl["N2"], lhsT=t[Mc][:], rhs=t[Nc][:], start=True, stop=True),
                          r=[K_(Mc), K_(Nc)], w=[PK("N2")])
                        A(lambda e, Nn=Nn: e.activation(out=t[Nn][:], in_=psl["N2"], func=AF.Copy), r=[PK("N2")], w=[K_(Nn)])
                    T(lambda e, Mn=Mn: e.matmul(psl["pP"], lhsT=t["Q"][:], rhs=t[Mn][:], start=True, stop=True), r=[K_("Q"), K_(Mn)], w=[PK("pP")])
                    if not last:
                        T(lambda e, Nn=Nn: e.matmul(psl["pQ"], lhsT=t["P"][:], rhs=t[Nn][:], start=True, stop=True), r=[K_("P"), K_(Nn)], w=[PK("pQ")])
                    V(lambda e: e.tensor_tensor(out=t["P"][:], in0=t["P"][:], in1=psl["pP"], op=ALU.add), r=[K_("P"), PK("pP")], w=[K_("P")])
                    if not last:
                        V(lambda e: e.tensor_tensor(out=t["Q"][:], in0=t["Q"][:], in1=psl["pQ"], op=ALU.add), r=[K_("Q"), PK("pQ")], w=[K_("Q")])
                    cur = 1 - cur
                G(lambda e: e.tensor_scalar(out=t["vb"][:], in0=vth[hb][:, i, :], scalar1=btc, scalar2=None, op0=ALU.mult),
                  r=[("vth", hb), "glS"], w=[K_("vb")])
                T(lambda e: e.matmul(psl["pu"], lhsT=t["P"][:], rhs=t["vb"][:], start=True, stop=True), r=[K_("P"), K_("vb")], w=[PK("pu")])
                A(lambda e: e.activation(out=uS[hb][:, i, :], in_=psl["pu"], func=AF.Copy), r=[PK("pu")], w=[("uS", hb, i)])
                G(lambda e: e.tensor_scalar(out=t["kbg"][:], in0=kth[hb][:, i, :], scalar1=bgS[:, i, h:h + 1], scalar2=None, op0=ALU.mult),
                  r=[("kth", hb), "bgS"], w=[K_("kbg")])
                T(lambda e: e.matmul(psl["pw"], lhsT=t["kbg"][:], rhs=t["P"][:], start=True, stop=True), r=[K_("P"), K_("kbg")], w=[PK("pw")])
                A(lambda e: e.activation(out=wTS[hb][:, i, :], in_=psl["pw"], func=AF.Copy), r=[PK("pw")], w=[("wTS", hb, i)])
                G(lambda e: e.tensor_scalar(out=kdS[hb][:, i, :], in0=kth[hb][:, i, :], scalar1=ekd[:, i, h:h + 1], scalar2=None, op0=ALU.mult),
                  r=[("kth", hb), "ekd"], w=[("kdS", hb, i)])
                if own:
                    qTc = qTh[hb][:, io * 128:(io + 1) * 128]
                    T(lambda e: e.matmul(psl["pqk"], lhsT=kTc, rhs=qTc, start=True, stop=True), r=[("kTh", hb), ("qTh", hb)], w=[PK("pqk")])
                    V(lambda e: e.tensor_tensor(out=qkS[hb][:, io, :], in0=psl["pqk"], in1=t["decT"][:], op=ALU.mult),
                      r=[PK("pqk"), K_("decT")], w=[("qkS", hb, io)])
                    G(lambda e: e.tensor_tensor(out=qdS[hb][:, io, :], in0=qTc, in1=t["egb"][:], op=ALU.mult),
                      r=[("qTh", hb), K_("egb")], w=[("qdS", hb, io)])

            pW, pSs, pOo, pTr = (pbk[6][:, k, :] for k in range(4))

            def step(h, i):
                hb = h % 2
                own = i >= 8
                io = i - 8
                vb_ = i % 2
                T(lambda e: e.matmul(pW, lhsT=wTS[hb][:, i, :], rhs=Sst[hb][:], start=True, stop=True), r=[("wTS", hb, i), ("Sst", hb)], w=[("bank", 6)])
                V(lambda e: e.tensor_tensor(out=vn5[vb_][:], in0=uS[hb][:, i, :], in1=pW, op=ALU.subtract), r=[("uS", hb, i), ("bank", 6)], w=[("vn5", vb_)])
                if own:
                    T(lambda e: e.matmul(pOo, lhsT=qdS[hb][:, io, :], rhs=Sst[hb][:], start=True, stop=False), r=[("qdS", hb, io), ("Sst", hb)], w=[("bank", 6)])
                    T(lambda e: e.matmul(pOo, lhsT=qkS[hb][:, io, :], rhs=vn5[vb_][:], start=False, stop=True), r=[("qkS", hb, io), ("vn5", vb_)], w=[("bank", 6)])
                T(lambda e: e.matmul(pSs, lhsT=kdS[hb][:, i, :], rhs=vn5[vb_][:], start=True, stop=True), r=[("kdS", hb, i), ("vn5", vb_)], w=[("bank", 6)])
                V(lambda e: e.scalar_tensor_tensor(out=Sst[hb][:], in0=Sst[hb][:], scalar=egl[:, i, h:h + 1], in1=pSs, op0=ALU.mult, op1=ALU.add),
                  r=[("Sst", hb), "egl", ("bank", 6)], w=[("Sst", hb)])
                if own:
                    ob_ = io % 2
                    A(lambda e: e.activation(out=junk5[:], in_=pOo, func=AF.Square, accum_out=ssq[:, io:io + 1]), r=[("bank", 6), "ssq"], w=["junk5", ("ssq", io)])
                    V(lambda e: e.tensor_scalar(out=rs5[:, io:io + 1], in0=ssq[:, io:io + 1], scalar1=1.0 / 128.0, scalar2=EPS, op0=ALU.mult, op1=ALU.add),
                      r=[("ssq", io)], w=[("rs5", io)])
                    A(lambda e: e.activation(out=rs5[:, io:io + 1], in_=rs5[:, io:io + 1], func=AF.Sqrt), r=[("rs5", io)], w=[("rs5", io)])
                    V(lambda e: e.reciprocal(out=rs5[:, io:io + 1], in_=rs5[:, io:io + 1]), r=[("rs5", io)], w=[("rs5", io)])
                    A(lambda e: e.activation(out=on5[ob_][:], in_=pOo, func=AF.Copy, scale=rs5[:, io:io + 1]), r=[("bank", 6), ("rs5", io)], w=[("on5", ob_)])
                    T(lambda e: e.transpose(out=pTr, in_=on5[ob_][:], identity=identF[:]), r=[("on5", ob_), "identF"], w=[("bank", 6)])
                    V(lambda e: e.scalar_tensor_tensor(out=ybst[hb][:, io * 128:(io + 1) * 128], in0=pTr, scalar=ggS[:, 0:1],
                                                       in1=zsS[hb][:, io * 128:(io + 1) * 128], op0=ALU.mult, op1=ALU.mult),
                      r=[("bank", 6), "ggS", ("zsS", hb)], w=[("ybst", hb)])
                    if i == 15:
                        DMA(lambda e: e.dma_start(out=ybT[h], in_=ybst[hb][:]), r=[("ybst", hb)], w=[("ybT", h)], key=("ybo", hb))

            def loads(h):
                hb = h % 2
                DMA(lambda e: e.dma_start(out=kTh[hb][:], in_=bkT[h]), w=[("kTh", hb)])
                DMA(lambda e: e.dma_start(out=qTh[hb][:], in_=bqT[h][:, OWN:S]), w=[("qTh", hb)])
                DMA(lambda e: e.dma_start(out=kth[hb][:], in_=bk[h]), w=[("kth", hb)])
                DMA(lambda e: e.dma_start(out=vth[hb][:], in_=bv[h]), w=[("vth", hb)])
                DMA(lambda e: e.dma_start(out=zsS[hb][:], in_=zsT[h]), w=[("zsS", hb)])

            import os
            from collections import deque
            P5H = int(os.environ.get("P5H", NH))
            chunks = [(h, i) for h in range(P5H) for i in range(16)]
            steps = deque(chunks)
            active = {}
            prep_done = set()
            step_emitted = set()
            nxt_chunk = 0
            cur_step = None
            while nxt_chunk < len(chunks) or active or steps or cur_step:
                for slot in range(NCH):
                    if slot in active or nxt_chunk >= len(chunks):
                        continue
                    h, i = chunks[nxt_chunk]
                    if h >= 2 and (h - 2, i) not in step_emitted:
                        continue
                    if i == 0:
                        loads(h)
                    REC[0] = []
                    prep(h, i, slot)
                    active[slot] = (deque(REC[0]), (h, i))
                    REC[0] = None
                    nxt_chunk += 1
                for slot in sorted(active):
                    ops_, hi_ = active[slot]
                    emit_rec(ops_.popleft())
                    if not ops_:
                        prep_done.add(hi_)
                        del active[slot]
                if cur_step is None and steps and steps[0] in prep_done:
                    h, i = steps.popleft()
                    if i == 0:
                        hb1 = h % 2
                        V(lambda e, hb1=hb1: e.memset(Sst[hb1][:], 0.0), w=[("Sst", hb1)])
                        V(lambda e: e.memset(ssq[:], 0.0), w=["ssq"] + [("ssq", k) for k in range(8)])
                    REC[0] = []
                    step(h, i)
                    cur_step = (deque(REC[0]), (h, i))
                    REC[0] = None
                if cur_step is not None:
                    for _ in range(2):
                        if cur_step[0]:
                            emit_rec(cur_step[0].popleft())
                    if not cur_step[0]:
                        step_emitted.add(cur_step[1])
                        cur_step = None
        P.barrier()
        if stop_after == "p5":
            P.emit()
            return nc

        with ExitStack() as stX:
            ssq2 = sb(stX, "ssq2", [128, 8, 16], F32)
            with ExitStack() as st:
                mgT = sb(st, "mgT", [128, KC, OWN], BF16)
                with ExitStack() as st6a:
                    yaS = sb(st6a, "yaS", [128, NH, OWN], BF16)
                    ybS = sb(st6a, "ybS", [128, NH, OWN], BF16)
                    wa = [sb(st6a, "wa%d" % i, [128, NH, 256], BF16) for i in range(2)]
                    wb_ = [sb(st6a, "wb%d" % i, [128, NH, 256], BF16) for i in range(2)]
                    gaS = [sb(st6a, "gaS%d" % i, [128, OWN], BF16) for i in range(2)]
                    gbS = [sb(st6a, "gbS%d" % i, [128, OWN], BF16) for i in range(2)]
                    t1 = [sb(st6a, "t1%d" % i, [128, 512], F32) for i in range(2)]
                    t2 = [sb(st6a, "t2%d" % i, [128, 512], F32) for i in range(2)]
                    pMa = [ps(st6a, "pMa%d" % i, [128, 2, 512]) for i in range(2)]
                    pMb = [ps(st6a, "pMb%d" % i, [128, 2, 512]) for i in range(2)]
                    DMA(lambda e: e.dma_start(out=yaS[:], in_=yaT.rearrange("h p t -> p h t")), w=["yaS"])
                    DMA(lambda e: e.dma_start(out=ybS[:], in_=ybT.rearrange("h p t -> p h t")), w=["ybS"])
                    wa_v = w_bra.rearrange("(c p) n -> p c n", p=128)
                    wb_v = w_brb.rearrange("(c p) n -> p c n", p=128)
                    k6 = 0
                    for ti in range(16):
                        wbuf = ti % 2
                        for q in range(2):
                            DMA(lambda e, wbuf=wbuf, ti=ti, q=q: e.dma_start(out=wa[wbuf][:, q * 8:(q + 1) * 8, :],
                                                                            in_=wa_v[:, q * 8:(q + 1) * 8, ti * 256:(ti + 1) * 256]),
                                w=[("wa", wbuf, q)], eng="gpsimd")
                            DMA(lambda e, wbuf=wbuf, ti=ti, q=q: e.dma_start(out=wb_[wbuf][:, q * 8:(q + 1) * 8, :],
                                                                            in_=wb_v[:, q * 8:(q + 1) * 8, ti * 256:(ti + 1) * 256]),
                                w=[("wb", wbuf, q)], eng="gpsimd")
                        for sub in range(2):
                            cb = ti * 2 + sub
                            s6 = cb % 2
                            DMA(lambda e, s6=s6, cb=cb: e.dma_start(out=gaS[s6][:], in_=gsa[cb]), w=[("gaS", s6)])
                            DMA(lambda e, s6=s6, cb=cb: e.dma_start(out=gbS[s6][:], in_=gsb[cb]), w=[("gbS", s6)])
                            for (pM, wt_, yS, wk_, yk) in ((pMa, wa, yaS, "wa", "yaS"), (pMb, wb_, ybS, "wb", "ybS")):
                                for c in range(NH):
                                    for tb in range(2):
                                        T(lambda e, pM=pM, wt_=wt_, yS=yS, s6=s6, wbuf=wbuf, c=c, tb=tb, sub=sub: e.matmul(
                                            pM[s6][:, tb, :], lhsT=wt_[wbuf][:, c, sub * 128:(sub + 1) * 128], rhs=yS[:, c, tb * 512:(tb + 1) * 512],
                                            start=(c == 0), stop=(c == NH - 1)),
                                          r=[(wk_, wbuf, 0), (wk_, wbuf, 1), yk], w=[(wk_ + "p", s6, tb)])
                            for tb in range(2):
                                k6 = 1 - k6
                                V(lambda e, s6=s6, tb=tb, k6=k6: e.tensor_tensor(out=t1[k6][:], in0=pMa[s6][:, tb, :], in1=gaS[s6][:, tb * 512:(tb + 1) * 512],
                                                                              op=ALU.mult), r=[("wap", s6, tb), ("gaS", s6)], w=[("t1", k6)])
                                V(lambda e, s6=s6, tb=tb, k6=k6: e.tensor_tensor(out=t2[k6][:], in0=pMb[s6][:, tb, :], in1=gbS[s6][:, tb * 512:(tb + 1) * 512],
                                                                              op=ALU.mult), r=[("wbp", s6, tb), ("gbS", s6)], w=[("t2", k6)])
                                G(lambda e, cb=cb, tb=tb, k6=k6: e.tensor_tensor(out=mgT[:, cb, tb * 512:(tb + 1) * 512], in0=t1[k6][:], in1=t2[k6][:], op=ALU.add),
                                  r=[("t1", k6), ("t2", k6)], w=[("mgT", cb)])
                P.barrier()
                with ExitStack() as st6b:
                    wo = [sb(st6b, "wo%d" % i, [128, KC, 256], BF16) for i in range(2)]
                    xr = [sb(st6b, "xr%d" % i, [128, 256], F32) for i in range(4)]
                    x1t = [sb(st6b, "x1t%d" % i, [128, 256], F32) for i in range(4)]
                    junk6 = sb(st6b, "junk6", [128, 256], F32)
                    pX = [ps(st6b, "pX%d" % i, [128, 512]) for i in range(4)]
                    V(lambda e: e.memset(ssq2[:], 0.0), w=["ssq2"])
                    wo_v = w_o.rearrange("(c p) n -> p c n", p=128)
                    for ct in range(16):
                        wbuf = ct % 2
                        for q in range(4):
                            DMA(lambda e, wbuf=wbuf, ct=ct, q=q: e.dma_start(out=wo[wbuf][:, q * 8:(q + 1) * 8, :],
                                                                            in_=wo_v[:, q * 8:(q + 1) * 8, ct * 256:(ct + 1) * 256]),
                                w=[("wo", wbuf, q)], eng="gpsimd")
                        for tt in range(8):
                            k4 = (ct * 8 + tt) % 4
                            DMA(lambda e, k4=k4, tt=tt, ct=ct: e.dma_start(out=xr[k4][:], in_=xs[OWN + tt * 128:OWN + (tt + 1) * 128, ct * 256:(ct + 1) * 256]),
                                w=[("xr", k4)])
                            for c in range(KC):
                                T(lambda e, k4=k4, c=c, tt=tt, wbuf=wbuf: e.matmul(pX[k4][:, 0:256], lhsT=mgT[:, c, tt * 128:(tt + 1) * 128], rhs=wo[wbuf][:, c, :],
                                                                                start=(c == 0), stop=(c == KC - 1)),
                                  r=[("wo", wbuf, c // 8)], w=[("pX", k4)])
                            V(lambda e, k4=k4: e.tensor_tensor(out=x1t[k4][:], in0=pX[k4][:, 0:256], in1=xr[k4][:], op=ALU.add),
                              r=[("pX", k4), ("xr", k4)], w=[("x1t", k4)])
                            A(lambda e, k4=k4, tt=tt, ct=ct: e.activation(out=junk6[:], in_=x1t[k4][:], func=AF.Square, accum_out=ssq2[:, tt, ct:ct + 1]),
                              r=[("x1t", k4), "ssq2"], w=["junk6", ("ssq2", tt, ct)])
                            DMA(lambda e, k4=k4, tt=tt, ct=ct: e.dma_start(out=x1[tt * 128:(tt + 1) * 128, ct * 256:(ct + 1) * 256], in_=x1t[k4][:]),
                                r=[("x1t", k4)], w=[("x1", tt, ct)], key=("x1o", k4))
                P.barrier()
            s12 = sb(stX, "s12", [128, 8, 16, 128], F32)
            stA = ExitStack()
            xn2T = sb(stA, "xn2T", [128, KC, OWN], BF16)
            with ExitStack() as st1:
                xf6 = [sb(st1, "xf6%d" % i, [128, D], F32) for i in range(2)]
                xb6 = [sb(st1, "xb6%d" % i, [128, D], BF16) for i in range(2)]
                g2s = sb(st1, "g2s", [128, KC], F32)
                ss6v = sb(st1, "ss6", [128, 8], F32)
                pt6 = [ps(st1, "pt6%d" % i, [128, 8, 128], BF16) for i in range(2)]
                DMA(lambda e: e.dma_start(out=g2s[:], in_=g2T), w=["g2s"])
                V(lambda e: e.reduce_sum(out=ss6v[:], in_=ssq2[:], axis=AX.X), w=["ss6"])
                V(lambda e: e.tensor_scalar(out=ss6v[:], in0=ss6v[:], scalar1=1.0 / D, scalar2=EPS, op0=ALU.mult, op1=ALU.add), r=["ss6"], w=["ss6"])
                A(lambda e: e.activation(out=ss6v[:], in_=ss6v[:], func=AF.Sqrt), r=["ss6"], w=["ss6"])
                V(lambda e: e.reciprocal(out=ss6v[:], in_=ss6v[:]), r=["ss6"], w=["ss6"])
                for i in range(8):
                    b = i % 2
                    DMA(lambda e, i=i, b=b: e.dma_start(out=xf6[b][:], in_=x1[i * 128:(i + 1) * 128, :]), w=[("xf6", b)])
                    A(lambda e, i=i, b=b: e.activation(out=xb6[b][:], in_=xf6[b][:], func=AF.Copy, scale=ss6v[:, i:i + 1]),
                      r=[("xf6", b), "ss6"], w=[("xb6", b)])
                    for g4 in range(4):
                        pb = g4 % 2
                        for c in range(8):
                            T(lambda e, b=b, g4=g4, c=c, pb=pb: e.transpose(out=pt6[pb][:, c, :], in_=xb6[b][:, (g4 * 8 + c) * 128:(g4 * 8 + c + 1) * 128],
                                                                         identity=identB[:]), r=[("xb6", b), "identB"], w=[("pt6", pb)])
                        V(lambda e, i=i, g4=g4, pb=pb: e.tensor_tensor(out=xn2T[:, g4 * 8:(g4 + 1) * 8, i * 128:(i + 1) * 128], in0=pt6[pb][:],
                                                                     in1=bc(g2s[:, g4 * 8:(g4 + 1) * 8].unsqueeze(2), [128, 8, 128]), op=ALU.mult),
                          r=[("pt6", pb), "g2s"], w=[("xn2T", i)])
            DMA(lambda e: e.dma_start(out=xn2D, in_=xn2T[:]), r=[("xn2T", i) for i in range(8)], w=["xn2D"])
            P.barrier()
            if stop_after == "p6":
                P.emit()
                return nc

            with ExitStack() as st7:
                with ExitStack() as st:
                    wq = [sb(st, "wq%d" % i, [128, KC, 256], BF16) for i in range(2)]
                    qpS = [sb(st, "qpS%d" % i, [128, OWN], F32) for i in range(2)]
                    kraw = sb(st, "kraw", [128, 2, 128], F32)
                    kT2 = sb(st, "kT2", [128, 2, 128], F32)
                    pQ = [ps(st, "pQ%d" % i, [128, 2, 512]) for i in range(2)]
                    pSc = [ps(st, "pSc%d" % i, [128, 512]) for i in range(2)]
                    pK = ps(st, "pK7", [128, 512])
                    DMA(lambda e: e.dma_start(out=kraw[:, 0, :], in_=pk1), w=[("kraw", 0)])
                    DMA(lambda e: e.dma_start(out=kraw[:, 1, :], in_=pk2), w=[("kraw", 1)])
                    for j in range(2):
                        T(lambda e, j=j: e.transpose(out=pK[:, j * 128:(j + 1) * 128], in_=kraw[:, j, :], identity=identF[:]),
                          r=[("kraw", j), "identF"], w=["pK"])
                    A(lambda e: e.activation(out=kT2[:].rearrange("p a b -> p (a b)"), in_=pK[:, 0:256], func=AF.Copy), r=["pK"], w=["kT2"])
                    wq_v = p_wq.rearrange("(c p) n -> p c n", p=128)
                    for ti in range(8):
                        wbuf = ti % 2
                        for q in range(4):
                            DMA(lambda e, wbuf=wbuf, ti=ti, q=q: e.dma_start(out=wq[wbuf][:, q * 8:(q + 1) * 8, :],
                                                                            in_=wq_v[:, q * 8:(q + 1) * 8, ti * 256:(ti + 1) * 256]),
                                w=[("wq", wbuf, q)], eng="gpsimd")
                        for sub in range(2):
                            blk = ti * 2 + sub
                            s7 = blk % 2
                            for c in range(KC):
                                for tb in range(2):
                                    T(lambda e, s7=s7, wbuf=wbuf, c=c, tb=tb, sub=sub: e.matmul(
                                        pQ[s7][:, tb, :], lhsT=wq[wbuf][:, c, sub * 128:(sub + 1) * 128], rhs=xn2T[:, c, tb * 512:(tb + 1) * 512],
                                        start=(c == 0), stop=(c == KC - 1)), r=[("wq", wbuf, c // 8)], w=[("pQ", s7)])
                            A(lambda e, s7=s7: e.activation(out=qpS[s7][:], in_=pQ[s7][:].rearrange("p a b -> p (a b)"), func=AF.Copy),
                              r=[("pQ", s7)], w=[("qpS", s7)])
                            for tt in range(8):
                                k2 = tt % 2
                                T(lambda e, k2=k2, s7=s7, tt=tt, blk=blk: e.matmul(pSc[k2][:, 0:128], lhsT=qpS[s7][:, tt * 128:(tt + 1) * 128],
                                                                                rhs=kT2[:, blk % 2, :], start=True, stop=True),
                                  r=[("qpS", s7), "kT2"], w=[("pSc", k2)])
                                V(lambda e, k2=k2, tt=tt, blk=blk: e.tensor_copy(out=s12[:, tt, blk, :], in_=pSc[k2][:, 0:128]),
                                  r=[("pSc", k2)], w=[("s12", tt, blk)])
                P.barrier()
                stA.close()
                with ExitStack() as st:
                    thr = sb(st, "thr7", [128, 8, 8], F32)
                    nb7 = sb(st, "nb7", [128, 8, 8], F32)
                    v12 = sb(st, "v12", [128, 2, 16], F32)
                    wk7 = sb(st, "wk7", [128, 128], F32)
                    cand = sb(st, "cand", [128, 16, 16], F32)
                    cwk = sb(st, "cwk", [128, 256], F32)
                    c24 = sb(st, "c24", [128, 24], F32)
                    z7 = sb(st, "z7", [128, 8, 8], F32)
                    j16 = sb(st, "j16", [128, 16], F32)
                    sum7 = [sb(st, "sum7%d" % i, [128, 8, 128], F32) for i in range(4)]
                    E7 = [sb(st, "E7%d" % i, [128, 8, 128], BF16) for i in range(4)]
                    Gh = [[sb(st, "Gh%d_%d" % (i, h), [128, 8, 128], BF16) for h in range(8)] for i in range(2)]
                    GTs = [sb(st, "GTs%d" % i, [128, 8, OWN], BF16) for i in range(2)]
                    pGn = [ps(st, "pGn%d" % i, [128, 2, 512]) for i in range(2)]
                    pGt = [ps(st, "pGt%d" % i, [128, 8, 128], BF16) for i in range(2)]
                    Gn = [sb(st, "Gn%d" % i, [128, 8, 128], BF16) for i in range(2)]
                    V(lambda e: e.memset(z7[:], 0.0), w=["z7"])
                    for tt in range(8):
                        for h in range(8):
                            for half in range(2):
                                sv = s12[:, tt, 2 * h + half, :]
                                V(lambda e, sv=sv, half=half: e.max(out=v12[:, half, 0:8], in_=sv), w=["v12"])
                                V(lambda e, sv=sv, half=half: e.match_replace(out=wk7[:], in_to_replace=v12[:, half, 0:8], in_values=sv, imm_value=-1.0e30),
                                  r=["v12"], w=["wk7"])
                                V(lambda e, half=half: e.max(out=v12[:, half, 8:16], in_=wk7[:]), r=["wk7"], w=["v12"])
                            V(lambda e: e.tensor_tensor(out=cand[:], in0=bc(v12[:, 0, :].unsqueeze(2), [128, 16, 16]),
                                                        in1=bc(v12[:, 1, :].unsqueeze(1), [128, 16, 16]), op=ALU.add), r=["v12"], w=["cand"])
                            cf = cand[:].rearrange("p a b -> p (a b)")
                            V(lambda e, cf=cf: e.max(out=c24[:, 0:8], in_=cf), r=["cand"], w=["c24"])
                            V(lambda e, cf=cf: e.match_replace(out=cwk[:], in_to_replace=c24[:, 0:8], in_values=cf, imm_value=-1.0e30), r=["cand", "c24"], w=["cwk"])
                            V(lambda e: e.max(out=c24[:, 8:16], in_=cwk[:]), r=["cwk"], w=["c24"])
                            V(lambda e: e.match_replace(out=cwk[:], in_to_replace=c24[:, 8:16], in_values=cwk[:], imm_value=-1.0e30), r=["cwk", "c24"], w=["cwk"])
                            V(lambda e: e.max(out=c24[:, 16:24], in_=cwk[:]), r=["cwk"], w=["c24"])
                            tc_ = thr[:, tt, h:h + 1]
                            nc_ = nb7[:, tt, h:h + 1]
                            zc_ = z7[:, tt, h:h + 1]
                            V(lambda e, tc_=tc_: e.tensor_tensor(out=tc_, in0=c24[:, 15:16], in1=c24[:, 16:17], op=ALU.add), r=["c24"], w=["thr"])
                            V(lambda e, tc_=tc_: e.tensor_scalar(out=tc_, in0=tc_, scalar1=0.5, scalar2=None, op0=ALU.mult), r=["thr"], w=["thr"])
                            V(lambda e, tc_=tc_, nc_=nc_: e.tensor_scalar(out=nc_, in0=tc_, scalar1=-1.0, scalar2=None, op0=ALU.mult), r=["thr"], w=["nb7"])
                            A(lambda e, nc_=nc_, zc_=zc_: e.activation(out=j16[:], in_=c24[:, 0:16], func=AF.Exp, bias=nc_, accum_out=zc_),
                              r=["c24", "nb7", "z7"], w=["j16", "z7"])
                            A(lambda e, zc_=zc_: e.activation(out=zc_, in_=zc_, func=AF.Ln), r=["z7"], w=["z7"])
                            V(lambda e, nc_=nc_, zc_=zc_: e.tensor_tensor(out=nc_, in0=nc_, in1=zc_, op=ALU.subtract), r=["nb7", "z7"], w=["nb7"])
                    def g_elem(ic, tt):
                        gs = tt % 2
                        for h in range(8):
                            k2 = h % 4
                            s1c = s12[:, tt, 2 * h, ic * 8:(ic + 1) * 8]
                            s2a = s12[:, tt, 2 * h + 1, :]
                            G(lambda e, k2=k2, s1c=s1c, s2a=s2a: e.tensor_tensor(out=sum7[k2][:], in0=bc(s1c.unsqueeze(2), [128, 8, 128]),
                                                                                 in1=bc(s2a.unsqueeze(1), [128, 8, 128]), op=ALU.add),
                              w=[("sum7", k2)])
                            A(lambda e, k2=k2, tt=tt, h=h: e.activation(out=E7[k2][:], in_=sum7[k2][:], func=AF.Exp, bias=nb7[:, tt, h:h + 1]),
                              r=[("sum7", k2), "nb7"], w=[("E7", k2)])
                            V(lambda e, k2=k2, gs=gs, h=h, tt=tt: e.scalar_tensor_tensor(out=Gh[gs][h][:], in0=sum7[k2][:], scalar=thr[:, tt, h:h + 1],
                                                                                         in1=E7[k2][:], op0=ALU.is_ge, op1=ALU.mult),
                              r=[("sum7", k2), ("E7", k2), "thr"], w=[("Gh", gs, h)])
                        for half in range(2):
                            for h in range(8):
                                T(lambda e, gs=gs, half=half, h=h: e.matmul(pGn[gs][:, half, :], lhsT=identB[:],
                                                                          rhs=Gh[gs][h][:, half * 4:(half + 1) * 4, :].rearrange("p a b -> p (a b)"),
                                                                          start=(h == 0), stop=(h == 7)),
                                  r=[("Gh", gs, h), "identB"], w=[("pGn", gs, half)])

                    def g_tail(ic, tt):
                        gs = tt % 2
                        gb_ = ic % 2
                        A(lambda e, gs=gs: e.activation(out=Gn[gs][:].rearrange("p a b -> p (a b)"), in_=pGn[gs][:].rearrange("p a b -> p (a b)"), func=AF.Copy),
                          r=[("pGn", gs, 0), ("pGn", gs, 1)], w=[("Gn", gs)])
                        for i1l in range(8):
                            T(lambda e, gs=gs, i1l=i1l: e.transpose(out=pGt[gs][:, i1l, :], in_=Gn[gs][:, i1l, :], identity=identB[:]),
                              r=[("Gn", gs), "identB"], w=[("pGt", gs)])
                        V(lambda e, gs=gs, tt=tt, gb_=gb_: e.tensor_copy(out=GTs[gb_][:, :, tt * 128:(tt + 1) * 128], in_=pGt[gs][:]),
                          r=[("pGt", gs)], w=[("GTs", gb_)])
                        if tt == 7:
                            DMA(lambda e, ic=ic, gb_=gb_: e.dma_start(out=GT[ic * 8:(ic + 1) * 8].rearrange("a p t -> p a t"), in_=GTs[gb_][:]),
                                r=[("GTs", gb_)], w=[("GT", ic)], key=("gto", gb_))

                    gtiles = [(ic, tt) for ic in range(16) for tt in range(8)]
                    for n_, (ic, tt) in enumerate(gtiles):
                        g_elem(ic, tt)
                        if n_ >= 1:
                            g_tail(*gtiles[n_ - 1])
                    g_tail(*gtiles[-1])
                P.barrier()
            with ExitStack() as st:
                xn2T2 = sb(st, "xn2T2", [128, KC, OWN], BF16)
                DMA(lambda e: e.dma_start(out=xn2T2[:], in_=xn2D), w=["xn2T2"])
                Ub = [sb(st, "Ub%d" % i, [128, D], BF16) for i in range(2)]
                UT = [sb(st, "UT%d" % i, [128, KC, 128], BF16) for i in range(2)]
                actT = [sb(st, "actT%d" % i, [128, OWN], BF16) for i in range(2)]
                GTt = [sb(st, "GTt%d" % i, [128, OWN], BF16) for i in range(2)]
                Gat = [sb(st, "Gat%d" % i, [128, OWN], BF16) for i in range(2)]
                pUT = [ps(st, "pUT%d" % i, [128, 8, 128], BF16) for i in range(2)]
                pA7 = [ps(st, "pA7%d" % i, [128, 2, 512]) for i in range(2)]
                import os
                NE = int(os.environ.get("P7E", 128))
                for i1 in range(NE):
                    b = i1 % 2
                    for q in range(2):
                        DMA(lambda e, b=b, i1=i1, q=q: e.dma_start(out=Ub[b][:, q * 2048:(q + 1) * 2048], in_=p_u[i1 * 128:(i1 + 1) * 128, q * 2048:(q + 1) * 2048]),
                            w=[("Ub", b, q)], eng="gpsimd")
                    DMA(lambda e, b=b, i1=i1: e.dma_start(out=GTt[b][:], in_=GT[i1]), w=[("GTt", b)])
                    for g4 in range(4):
                        pb = g4 % 2
                        for c in range(8):
                            T(lambda e, b=b, g4=g4, c=c, pb=pb: e.transpose(out=pUT[pb][:, c, :], in_=Ub[b][:, (g4 * 8 + c) * 128:(g4 * 8 + c + 1) * 128],
                                                                         identity=identB[:]), r=[("Ub", b, g4 // 2), "identB"], w=[("pUT", pb)])
                        V(lambda e, b=b, g4=g4, pb=pb: e.tensor_copy(out=UT[b][:, g4 * 8:(g4 + 1) * 8, :], in_=pUT[pb][:]), r=[("pUT", pb)], w=[("UT", b, g4)])
                    for c in range(KC):
                        for tb in range(2):
                            T(lambda e, b=b, c=c, tb=tb: e.matmul(pA7[b][:, tb, :], lhsT=UT[b][:, c, :], rhs=xn2T2[:, c, tb * 512:(tb + 1) * 512],
                                                               start=(c == 0), stop=(c == KC - 1)), r=[("UT", b, c // 8), "xn2T2"], w=[("pA7", b)])
                    A(lambda e, b=b: e.activation(out=actT[b][:], in_=pA7[b][:].rearrange("p a b -> p (a b)"), func=AF.Gelu), r=[("pA7", b)], w=[("actT", b)])
                    V(lambda e, b=b: e.tensor_tensor(out=Gat[b][:], in0=actT[b][:], in1=GTt[b][:], op=ALU.mult), r=[("actT", b), ("GTt", b)], w=[("Gat", b)])
                    DMA(lambda e, b=b, i1=i1: e.dma_start(out=GaT[i1], in_=Gat[b][:]), r=[("Gat", b)], w=[("GaT", i1)], key=("gato", b))
            P.barrier()
        with ExitStack() as st:
            Vb = [sb(st, "Vb%d" % i, [128, 4, 512], BF16) for i in range(3)]
            Gg = [sb(st, "Gg%d" % i, [128, 4, OWN], BF16) for i in range(3)]
            x1r = [sb(st, "x1r%d" % i, [128, 512], F32) for i in range(2)]
            ot = [sb(st, "ot%d" % i, [128, 512], F32) for i in range(2)]
            pO7 = [ps(st, "pO7%d" % i, [128, 512]) for i in range(8)]
            NG = NE // 4
            for db in range(8):
                for ig in range(NG):
                    b3 = (db * NG + ig) % 3
                    DMA(lambda e, b3=b3, ig=ig, db=db: e.dma_start(
                        out=Vb[b3][:], in_=p_v[ig * 512:(ig + 1) * 512, db * 512:(db + 1) * 512].rearrange("(q p) n -> p q n", p=128)),
                        w=[("Vb", b3)], eng="gpsimd")
                    DMA(lambda e, b3=b3, ig=ig: e.dma_start(out=Gg[b3][:], in_=GaT[ig * 4:(ig + 1) * 4].rearrange("q p t -> p q t")), w=[("Gg", b3)])
                    for q in range(4):
                        i1 = ig * 4 + q
                        for tt in range(8):
                            T(lambda e, b3=b3, q=q, tt=tt, i1=i1: e.matmul(pO7[tt][:], lhsT=Gg[b3][:, q, tt * 128:(tt + 1) * 128], rhs=Vb[b3][:, q, :],
                                                                        start=(i1 == 0), stop=(i1 == NE - 1)),
                              r=[("Vb", b3), ("Gg", b3)], w=[("pO7", tt)])
                for tt in range(8):
                    k2 = tt % 2
                    DMA(lambda e, k2=k2, tt=tt, db=db: e.dma_start(out=x1r[k2][:], in_=x1[tt * 128:(tt + 1) * 128, db * 512:(db + 1) * 512]), w=[("x1r", k2)])
                    V(lambda e, k2=k2, tt=tt: e.tensor_tensor(out=ot[k2][:], in0=pO7[tt][:], in1=x1r[k2][:], op=ALU.add),
                      r=[("pO7", tt), ("x1r", k2)], w=[("ot", k2)])
                    DMA(lambda e, k2=k2, tt=tt, db=db: e.dma_start(out=out[tt * 128:(tt + 1) * 128, db * 512:(db + 1) * 512], in_=ot[k2][:]),
                        r=[("ot", k2)], w=[("out", tt, db)], key=("oto", k2))
        P.barrier()

        P.emit()
    return nc


def prep_shared(inp):
    m = {}
    f = lambda k: np.asarray(inp[k], np.float32)
    m["w_in"] = np.ascontiguousarray(f("w_in")[0])
    m["g1T"] = np.ascontiguousarray(f("norm1_g")[0].reshape(KC, 128).T)
    cw = f("conv_w")[0]
    m["convT"] = np.ascontiguousarray(cw.reshape(4, 48, 128).transpose(2, 1, 0))
    m["qg_col"] = np.ascontiguousarray(f("q_norm_g")[0].reshape(128, 1))
    m["kg_col"] = np.ascontiguousarray(f("k_norm_g")[0].reshape(128, 1))
    m["alog_b"] = np.ascontiguousarray(np.broadcast_to(f("a_log")[0][None, :], (128, NH)))
    m["dtb_b"] = np.ascontiguousarray(np.broadcast_to(f("dt_bias")[0][None, :], (128, NH)))
    m["ident_f"] = np.eye(128, dtype=np.float32)
    m["ident_b"] = np.eye(128, dtype=np.float32).astype(ml_dtypes.bfloat16)
    rb = f("rel_bias")
    m["rel_b"] = np.ascontiguousarray(rb)
    m["oh_c"] = _t5_onehot()
    m["b31_b"] = np.ascontiguousarray(np.broadcast_to(rb[31][None, :], (128, NH)))
    m["J_c"] = np.ascontiguousarray(np.eye(128, dtype=np.float32)[::-1])
    m["um_c"] = np.ascontiguousarray(np.triu(np.ones((128, 128), np.float32)))
    m["ls_c"] = np.ascontiguousarray(np.tril(np.ones((128, 128), np.float32), -1))
    m["gg_col"] = np.ascontiguousarray(f("gdn_norm_g")[0].reshape(128, 1))
    m["w_bra"] = np.ascontiguousarray(f("w_br_a")[0])
    m["w_brb"] = np.ascontiguousarray(f("w_br_b")[0])
    m["w_o"] = np.ascontiguousarray(f("w_out")[0])
    m["g2T"] = np.ascontiguousarray(f("norm2_g")[0].reshape(KC, 128).T)
    m["p_wq"] = np.ascontiguousarray(f("peer_wq")[0])
    m["pk1"] = np.ascontiguousarray(f("peer_k1")[0])
    m["pk2"] = np.ascontiguousarray(f("peer_k2")[0])
    m["p_u"] = np.ascontiguousarray(f("peer_u")[0])
    m["p_v"] = np.ascontiguousarray(f("peer_v")[0])
    return m


def prep_core(inp, c, shared=None):
    b, g = c // 2, c % 2
    m = dict(shared if shared is not None else prep_shared(inp))
    x = np.asarray(inp["x"], np.float32)
    xs = np.zeros((S, D), np.float32)
    kb = np.zeros((128, S), np.float32)
    if g == 1:
        xs[:] = x[b]
    else:
        xs[OWN:] = x[b, :OWN]
        kb[:, :OWN] = -3.0e38
    m["xs"] = xs
    m["keybias"] = kb
    return m


def kernel(**inputs):
    nc = build()
    shared = prep_shared(inputs)
    in_maps = [prep_core(inputs, c, shared) for c in range(8)]
    res = run_bass_kernel_spmd(nc, in_maps, core_ids=list(range(8)))
    out = np.zeros((4, S, D), np.float32)
    for c in range(8):
        b, g = c // 2, c % 2
        out[b, g * OWN:(g + 1) * OWN] = np.asarray(res.results[c]["out"], np.float32)
    return out


def _t5_onehot():
    oh = np.zeros((32, 1280), np.float32)
    for d in range(128):
        n = d
        if n < 16:
            bk_ = n
        else:
            bk_ = 16 + int(np.float32(np.log(np.float32(n) / np.float32(16)) / np.float32(math.log(128 / 16)) * np.float32(16)))
            bk_ = min(bk_, 31)
        oh[bk_, d + 640] += 1.0
        oh[31, d + 640] -= 1.0
    return oh
```
